# Optimizing a Trainium2 kernel written in Bass

```python
import jax, jax.numpy as jnp
from jax import lax
import numpy as np

D_MODEL = 1024
BATCH = 8
SEQ = 4096
DEPTH = 4

N_MIXERS = 2
N_LAYERS_A = (DEPTH + 1) // 2
N_LAYERS_B = DEPTH // 2

DEEPNORM_ALPHA = (2.0 * DEPTH) ** 0.25
DEEPNORM_BETA = (8.0 * DEPTH) ** -0.25
LN_EPS = 1e-5

GLA_HEADS = 4
GLA_DK = D_MODEL // 2 // GLA_HEADS
GLA_DV = D_MODEL // GLA_HEADS
GLA_QK = GLA_HEADS * GLA_DK
GLA_VO = GLA_HEADS * GLA_DV
GLA_GATE_RANK = 16
GLA_GATE_TAU = 16.0
GLA_CHUNK = 64
GLA_IN = 2 * GLA_QK + 2 * GLA_VO + 2 * GLA_GATE_RANK
GLA_SPLITS = [GLA_QK, 2 * GLA_QK, 2 * GLA_QK + GLA_VO, 2 * GLA_QK + 2 * GLA_VO,
              2 * GLA_QK + 2 * GLA_VO + GLA_GATE_RANK]

SGU_CHUNK = 128
SGU_GROUPS = 8
SGU_WIDTH = D_MODEL
SGU_GROUP_DIM = SGU_WIDTH // SGU_GROUPS

N_EXPERTS = 32
TOP_K = 4
D_EXPERT = D_MODEL
SWIGLU_LIMIT = 7.0
SWIGLU_ALPHA = 1.702
MOE_BLOCK = 256

kernel_name = 'hybrid_gla_sgu_moe_deepnorm'


def layer_norm(x, g, b):
    xf = x.astype(jnp.float32)
    mu = jnp.mean(xf, axis=-1, keepdims=True)
    var = jnp.mean(jnp.square(xf - mu), axis=-1, keepdims=True)
    y = (xf - mu) * lax.rsqrt(var + LN_EPS)
    return (y * g.astype(jnp.float32) + b.astype(jnp.float32)).astype(x.dtype)


def gla_scan(q, k, v, logg):
    bsz, nh, s, dk = q.shape
    dv = v.shape[-1]
    nc = s // GLA_CHUNK

    def to_chunks(t):
        t = t.reshape(bsz, nh, nc, GLA_CHUNK, t.shape[-1])
        return jnp.moveaxis(t, 2, 0)

    mask = jnp.tril(jnp.ones((GLA_CHUNK, GLA_CHUNK), dtype=bool))[:, :, None]

    def step(state, inp):
        qc, kc, vc, gc = inp
        b = jnp.cumsum(gc, axis=-2)
        o_inter = jnp.einsum('bhid,bhde->bhie', qc * jnp.exp(b), state)
        diff = b[:, :, :, None, :] - b[:, :, None, :, :]
        decay = jnp.exp(jnp.where(mask, diff, -jnp.inf))
        scores = jnp.einsum('bhid,bhjd,bhijd->bhij', qc, kc, decay)
        o_intra = jnp.einsum('bhij,bhje->bhie', scores, vc)
        b_last = b[:, :, -1:, :]
        new_state = (jnp.exp(b_last[:, :, 0, :])[..., None] * state
                     + jnp.einsum('bhjd,bhje->bhde', kc * jnp.exp(b_last - b), vc))
        return new_state, o_inter + o_intra

    s0 = jnp.zeros((bsz, nh, dk, dv), jnp.float32)
    _, o = lax.scan(step, s0, (to_chunks(q), to_chunks(k), to_chunks(v), to_chunks(logg)))
    return jnp.moveaxis(o, 0, 2).reshape(bsz, nh, s, dv)


def gla_mixer(h, w_in, wg2_f, bg_f, wg2_b, bg_b, norm_g, w_out):
    bsz, s, _ = h.shape
    proj = h @ w_in
    q, k, v, r, lr_f, lr_b = jnp.split(proj, GLA_SPLITS, axis=-1)

    def heads(t, d):
        return t.reshape(bsz, s, GLA_HEADS, d).transpose(0, 2, 1, 3).astype(jnp.float32)

    qh = heads(q, GLA_DK) * (GLA_DK ** -0.5)
    kh = heads(k, GLA_DK)
    vh = heads(v, GLA_DV)
    logg_f = heads(jax.nn.log_sigmoid((lr_f @ wg2_f + bg_f).astype(jnp.float32)) / GLA_GATE_TAU, GLA_DK)
    logg_b = heads(jax.nn.log_sigmoid((lr_b @ wg2_b + bg_b).astype(jnp.float32)) / GLA_GATE_TAU, GLA_DK)

    o_f = gla_scan(qh, kh, vh, logg_f)
    flip = lambda t: jnp.flip(t, axis=2)
    o_b = flip(gla_scan(flip(qh), flip(kh), flip(vh), flip(logg_b)))
    o = o_f + o_b
    o = o * lax.rsqrt(jnp.mean(o * o, axis=-1, keepdims=True) + LN_EPS)
    o = o.astype(h.dtype) * norm_g
    o = o.transpose(0, 2, 1, 3).reshape(bsz, s, GLA_VO)
    return (o * jax.nn.silu(r)) @ w_out


def sgu_mixer(h, w_in, b_in, ln_g, ln_b, w_s, b_s, w_out, b_out):
    bsz, s, _ = h.shape
    z = jax.nn.gelu(h @ w_in + b_in, approximate=False)
    u, v = z[..., :SGU_WIDTH], z[..., SGU_WIDTH:]
    v = layer_norm(v, ln_g, ln_b)
    nch = s // SGU_CHUNK
    v = v.reshape(bsz, nch, SGU_CHUNK, SGU_GROUPS, SGU_GROUP_DIM)
    sv = jnp.einsum('gpq,bnqgc->bnpgc', w_s, v) + b_s.T[None, None, :, :, None]
    sv = sv.reshape(bsz, s, SGU_WIDTH)
    return (u * sv) @ w_out + b_out


def clamped_swiglu(hu):
    gate, lin = hu[..., ::2], hu[..., 1::2]
    gate = jnp.minimum(gate, SWIGLU_LIMIT)
    lin = jnp.clip(lin, -SWIGLU_LIMIT, SWIGLU_LIMIT)
    return gate * jax.nn.sigmoid(SWIGLU_ALPHA * gate) * (lin + 1.0)


def moe_ffn(h, w_router, b_router, w_up, b_up, w_down, b_down):
    bsz, s, d = h.shape
    n_tok = bsz * s
    xt = h.reshape(n_tok, d)
    logits = (xt @ w_router + b_router).astype(jnp.float32)
    top_vals, top_idx = lax.top_k(logits, TOP_K)
    top_w = jax.nn.softmax(top_vals, axis=-1).astype(h.dtype)

    n_assign = n_tok * TOP_K
    flat_e = top_idx.reshape(-1).astype(jnp.int32)
    flat_w = top_w.reshape(-1)
    flat_tok = jnp.arange(n_assign, dtype=jnp.int32) // TOP_K
    order = jnp.argsort(flat_e)
    se, stok, sw = flat_e[order], flat_tok[order], flat_w[order]

    counts = jnp.zeros((N_EXPERTS,), jnp.int32).at[flat_e].add(1)
    padded = (counts + MOE_BLOCK - 1) // MOE_BLOCK * MOE_BLOCK
    grp_start = jnp.cumsum(counts) - counts
    pad_end = jnp.cumsum(padded)
    pad_start = pad_end - padded
    dest = pad_start[se] + (jnp.arange(n_assign, dtype=jnp.int32) - grp_start[se])
    n_blocks = -(-(n_assign + N_EXPERTS * MOE_BLOCK) // MOE_BLOCK)
    cap = n_blocks * MOE_BLOCK
    buf_tok = jnp.zeros((cap,), jnp.int32).at[dest].set(stok)
    buf_w = jnp.zeros((cap,), h.dtype).at[dest].set(sw)
    blk_start = jnp.arange(n_blocks, dtype=jnp.int32) * MOE_BLOCK
    blk_e = jnp.minimum(jnp.searchsorted(pad_end, blk_start, side='right'), N_EXPERTS - 1).astype(jnp.int32)

    xb = xt[buf_tok].reshape(n_blocks, MOE_BLOCK, d)

    def expert_block(args):
        xblk, e = args
        hu = xblk @ w_up[e] + b_up[e]
        return clamped_swiglu(hu) @ w_down[e] + b_down[e]

    yb = lax.map(expert_block, (xb, blk_e)).reshape(cap, d) * buf_w[:, None]
    return jnp.zeros((n_tok, d), h.dtype).at[buf_tok].add(yb).reshape(bsz, s, d)


def setup_inputs(seed: int = 0) -> dict:
    key = jax.random.key(seed)
    ks = jax.random.split(key, 32)
    nrm = lambda k, shape, sc: jax.random.normal(k, shape, jnp.float32) * sc
    d = D_MODEL
    return {
        'x': nrm(ks[0], (BATCH, SEQ, d), 1.0),
        'c': nrm(ks[1], (BATCH, d), 1.0),
        'ada_w': nrm(ks[2], (DEPTH, d, 6 * d), 0.1 * d ** -0.5),
        'ada_b': nrm(ks[3], (DEPTH, 6 * d), 0.01),
        'ln_g': 1.0 + nrm(ks[4], (DEPTH, 2, d), 0.02),
        'ln_b': nrm(ks[5], (DEPTH, 2, d), 0.02),
        'gla_w_in': nrm(ks[6], (N_LAYERS_A, d, GLA_IN), d ** -0.5),
        'gla_wg2_f': nrm(ks[7], (N_LAYERS_A, GLA_GATE_RANK, GLA_QK), GLA_GATE_RANK ** -0.5),
        'gla_bg_f': nrm(ks[8], (N_LAYERS_A, GLA_QK), 0.1),
        'gla_wg2_b': nrm(ks[9], (N_LAYERS_A, GLA_GATE_RANK, GLA_QK), GLA_GATE_RANK ** -0.5),
        'gla_bg_b': nrm(ks[10], (N_LAYERS_A, GLA_QK), 0.1),
        'gla_norm_g': 1.0 + nrm(ks[11], (N_LAYERS_A, GLA_DV), 0.02),
        'gla_w_out': nrm(ks[12], (N_LAYERS_A, GLA_VO, d), DEEPNORM_BETA * GLA_VO ** -0.5),
        'sgu_w_in': nrm(ks[13], (N_LAYERS_B, d, 2 * SGU_WIDTH), d ** -0.5),
        'sgu_b_in': nrm(ks[14], (N_LAYERS_B, 2 * SGU_WIDTH), 0.02),
        'sgu_ln_g': 1.0 + nrm(ks[15], (N_LAYERS_B, SGU_WIDTH), 0.02),
        'sgu_ln_b': nrm(ks[16], (N_LAYERS_B, SGU_WIDTH), 0.02),
        'sgu_w_s': nrm(ks[17], (N_LAYERS_B, SGU_GROUPS, SGU_CHUNK, SGU_CHUNK), 0.5 * SGU_CHUNK ** -0.5),
        'sgu_b_s': 1.0 + nrm(ks[18], (N_LAYERS_B, SGU_GROUPS, SGU_CHUNK), 0.02),
        'sgu_w_out': nrm(ks[19], (N_LAYERS_B, SGU_WIDTH, d), DEEPNORM_BETA * SGU_WIDTH ** -0.5),
        'sgu_b_out': nrm(ks[20], (N_LAYERS_B, d), 0.02),
        'moe_w_router': nrm(ks[21], (DEPTH, d, N_EXPERTS), d ** -0.5),
        'moe_b_router': nrm(ks[22], (DEPTH, N_EXPERTS), 0.01),
        'moe_w_up': nrm(ks[23], (DEPTH, N_EXPERTS, d, 2 * D_EXPERT), d ** -0.5),
        'moe_b_up': nrm(ks[24], (DEPTH, N_EXPERTS, 2 * D_EXPERT), 0.02),
        'moe_w_down': nrm(ks[25], (DEPTH, N_EXPERTS, D_EXPERT, d), DEEPNORM_BETA * D_EXPERT ** -0.5),
        'moe_b_down': nrm(ks[26], (DEPTH, N_EXPERTS, d), 0.02),
    }


def reference(x, c, ada_w, ada_b, ln_g, ln_b,
              gla_w_in, gla_wg2_f, gla_bg_f, gla_wg2_b, gla_bg_b, gla_norm_g, gla_w_out,
              sgu_w_in, sgu_b_in, sgu_ln_g, sgu_ln_b, sgu_w_s, sgu_b_s, sgu_w_out, sgu_b_out,
              moe_w_router, moe_b_router, moe_w_up, moe_b_up, moe_w_down, moe_b_down):
    mod_all = jnp.einsum('bd,lde->lbe', jax.nn.silu(c), ada_w) + ada_b[:, None, :]
    for layer in range(DEPTH):
        shift1, scale1, gate1, shift2, scale2, gate2 = jnp.split(mod_all[layer], 6, axis=-1)
        h = x * (1.0 + scale1[:, None, :]) + shift1[:, None, :]
        i = layer // N_MIXERS
        if layer % N_MIXERS == 0:
            y = gla_mixer(h, gla_w_in[i], gla_wg2_f[i], gla_bg_f[i], gla_wg2_b[i], gla_bg_b[i],
                          gla_norm_g[i], gla_w_out[i])
        else:
            y = sgu_mixer(h, sgu_w_in[i], sgu_b_in[i], sgu_ln_g[i], sgu_ln_b[i], sgu_w_s[i],
                          sgu_b_s[i], sgu_w_out[i], sgu_b_out[i])
        x = layer_norm(DEEPNORM_ALPHA * x + (1.0 + gate1[:, None, :]) * y, ln_g[layer, 0], ln_b[layer, 0])
        h = x * (1.0 + scale2[:, None, :]) + shift2[:, None, :]
        y = moe_ffn(h, moe_w_router[layer], moe_b_router[layer], moe_w_up[layer], moe_b_up[layer],
                    moe_w_down[layer], moe_b_down[layer])
        x = layer_norm(DEEPNORM_ALPHA * x + (1.0 + gate2[:, None, :]) * y, ln_g[layer, 1], ln_b[layer, 1])
    return x
```

```python
import numpy as np
from contextlib import ExitStack
import concourse.bass as bass
import concourse.mybir as mybir
from concourse.bass_utils import run_bass_kernel_spmd

F32 = mybir.dt.float32
BF16 = mybir.dt.bfloat16
I32 = mybir.dt.int32
AF = mybir.ActivationFunctionType
ALU = mybir.AluOpType
AX = mybir.AxisListType

D = 1024
DEPTH = 4
ALPHA = (2.0 * DEPTH) ** 0.25
EPS = 1e-5
NE = 32
GIN = 3104
NDUMP = 512
DEBUG = False


class _Op:
    __slots__ = ("eng", "fn", "deps", "is_dma", "tag", "val", "signal", "idx")


class Sched:
    ENGS = ("pe", "dve", "act", "pool", "sp")

    def __init__(self, nc, same_engine_sync=True):
        self.nc = nc
        self.ops = []
        self.last_w = {}
        self.readers = {}
        self.same_engine_sync = same_engine_sync
        self.dma_count = {}
        self.last_dma = {}

    def op(self, eng, fn, reads=(), writes=(), dma_tag=None):
        o = _Op()
        o.eng = eng
        o.fn = fn
        o.is_dma = dma_tag is not None
        o.tag = dma_tag
        o.signal = False
        o.idx = len(self.ops)
        deps = {}
        for t in reads:
            w = self.last_w.get(t)
            if w is not None:
                deps[w.idx] = w
        for t in writes:
            w = self.last_w.get(t)
            if w is not None:
                deps[w.idx] = w
            for r in self.readers.get(t, ()):
                deps[r.idx] = r
        if o.is_dma:
            p = self.last_dma.get(dma_tag)
            if p is not None:
                deps[p.idx] = p
            self.last_dma[dma_tag] = o
        o.deps = list(deps.values())
        for t in writes:
            self.last_w[t] = o
            self.readers[t] = []
        for t in reads:
            lst = self.readers.setdefault(t, [])
            key = (o.eng, o.tag)
            lst[:] = [r for r in lst if (r.eng, r.tag) != key]
            lst.append(o)
        if o.is_dma:
            c = self.dma_count.get(dma_tag, 0) + 1
            self.dma_count[dma_tag] = c
            o.val = 16 * c
        self.ops.append(o)
        return o

    def fence(self):
        last = {}
        for o in self.ops:
            if o.fn is None:
                continue
            last[(o.eng, o.tag)] = o
        deps = list(last.values())
        for e in self.ENGS:
            o = _Op()
            o.eng = e
            o.fn = None
            o.is_dma = False
            o.tag = None
            o.signal = False
            o.idx = len(self.ops)
            o.deps = list(deps)
            self.ops.append(o)
        self.last_w = {}
        self.readers = {}

    def dma(self, eng, out, in_, reads, writes, tag, **kw):
        return self.op(eng, lambda e: e.dma_start(out=out, in_=in_, **kw), reads, writes, dma_tag=tag)

    def _needs_wait(self, o, d):
        if d.is_dma:
            return True
        if d.eng == o.eng and not o.is_dma:
            if o.eng == "pe":
                return False
            return self.same_engine_sync
        return True

    def emit(self):
        nc = self.nc
        for o in self.ops:
            for d in o.deps:
                if self._needs_wait(o, d) and not d.is_dma:
                    d.signal = True
        cnt = {e: 0 for e in self.ENGS}
        for o in self.ops:
            if not o.is_dma and o.signal:
                cnt[o.eng] += 1
                o.val = cnt[o.eng]
        streams = {e: [] for e in self.ENGS}
        for o in self.ops:
            streams[o.eng].append(o)
        with ExitStack() as es:
            esem = {e: es.enter_context(nc.semaphore("s_" + e)) for e in self.ENGS}
            dsem = {t: es.enter_context(nc.semaphore("d_" + str(t))) for t in self.dma_count}
            block = es.enter_context(nc.Block())

            def run(ename, eng):
                known = {}
                for o in streams[ename]:
                    for d in o.deps:
                        if not self._needs_wait(o, d):
                            continue
                        if d.is_dma:
                            s, v, k = dsem[d.tag], d.val, ("d", d.tag)
                        else:
                            s, v, k = esem[d.eng], d.val, ("e", d.eng)
                        if known.get(k, 0) >= v:
                            continue
                        known[k] = v
                        eng.wait_ge(s, v)
                    if o.fn is None:
                        continue
                    ins = o.fn(eng)
                    if o.is_dma:
                        ins.then_inc(dsem[o.tag], 16)
                    elif o.signal:
                        ins.then_inc(esem[ename], 1)
                if ename == "sp":
                    for t, c in self.dma_count.items():
                        eng.wait_ge(dsem[t], 16 * c)

            @block.tensor
            def _(e):
                run("pe", e)

            @block.vector
            def _(e):
                run("dve", e)

            @block.scalar
            def _(e):
                run("act", e)

            @block.gpsimd
            def _(e):
                run("pool", e)

            @block.sync
            def _(e):
                run("sp", e)
        return cnt


SB_BYTES = 211968


class KB:
    def __init__(self, nc):
        self.nc = nc
        self.S = Sched(nc)
        self.SB = nc.alloc_sbuf_tensor("SB", [128, SB_BYTES // 4], F32).ap()
        self.PS = nc.alloc_psum_tensor("PS", [128, 4096], F32).ap()
        self.off = 0
        self.base = 0
        self.pso = 0
        self.uid = 0

    def persist(self):
        self.base = self.off

    def phase(self):
        self.S.fence()
        self.off = self.base
        self.pso = 0

    def sb(self, shape, dt, np_=128):
        n = int(np.prod(shape[1:]))
        bpe = 2 if dt == BF16 else 4
        nb = (n * bpe + 63) // 64 * 64
        assert self.off + nb <= SB_BYTES, ("SBUF overflow", self.off, nb)
        a = self.SB[0:shape[0], self.off // 4:(self.off + nb) // 4]
        self.off += nb
        if dt != F32:
            a = a.bitcast(dt)
        a = a[:, 0:n]
        if len(shape) == 3:
            a = a.rearrange("p (a b) -> p a b", a=shape[1])
        return a

    def ps(self, nbanks=1):
        assert self.pso + nbanks <= 8
        a = self.PS[:, self.pso * 512:(self.pso + nbanks) * 512]
        self.pso += nbanks
        return a

    def mm(self, out, lhsT, rhs, start, stop, r, w):
        self.S.op("pe", lambda e: e.matmul(out, lhsT=lhsT, rhs=rhs, start=start, stop=stop), r, w)

    def tr(self, out, in_, ident, r, w):
        self.S.op("pe", lambda e: e.transpose(out=out, in_=in_, identity=ident), r, w)

    def tt(self, eng, out, in0, in1, op, r, w):
        self.S.op(eng, lambda e: e.tensor_tensor(out=out, in0=in0, in1=in1, op=op), r, w)

    def ts(self, eng, out, in0, s1, s2, op0, op1, r, w):
        if op1 is None:
            self.S.op(eng, lambda e: e.tensor_scalar(out=out, in0=in0, scalar1=s1, scalar2=None, op0=op0), r, w)
        else:
            self.S.op(eng, lambda e: e.tensor_scalar(out=out, in0=in0, scalar1=s1, scalar2=s2, op0=op0, op1=op1), r, w)

    def stt(self, eng, out, in0, sc, in1, op0, op1, r, w):
        self.S.op(eng, lambda e: e.scalar_tensor_tensor(out=out, in0=in0, scalar=sc, in1=in1, op0=op0, op1=op1), r, w)

    def cp(self, eng, out, in_, r, w):
        if eng == "act":
            self.S.op(eng, lambda e: e.copy(out=out, in_=in_), r, w)
        else:
            self.S.op(eng, lambda e: e.tensor_copy(out=out, in_=in_), r, w)

    def act(self, out, in_, func, r, w, bias=None, scale=None, accum=None):
        kw = {}
        if bias is not None:
            kw["bias"] = bias
        if scale is not None:
            kw["scale"] = scale
        if accum is not None:
            kw["accum_out"] = accum
        self.S.op("act", lambda e: e.activation(out=out, in_=in_, func=func, **kw), r, w)

    def memset(self, eng, ap, v, w):
        self.S.op(eng, lambda e: e.memset(ap, v), [], w)

    def dma(self, eng, out, in_, r, w, tag, **kw):
        self.S.dma(eng, out, in_, r, w, tag, **kw)


def make_consts():
    j = np.arange(128)[:, None]
    i = np.arange(128)[None, :]
    c = -1.0 / 16.0
    mats = [
        np.eye(128),
        (j <= i) * c,
        (j >= i) * c,
        (j > i) * c,
        (j < i) * c,
        (j <= i) * 1.0,
        (j >= i) * 1.0,
        np.ones((128, 128)),
        (j < i) * 1.0,
    ]
    return np.concatenate(mats, axis=1).astype(np.float32)


NCONST = 9


def build_program(T, C, layer_list=(0, 1, 2, 3), do_mixer=True, do_moe=True):
    NT = T // 128
    NSLOT = NE * C
    nc = bass.Bass("TRN2", target_bir_lowering=False)
    dt_in = lambda name, shape, dt=F32: nc.dram_tensor(name, list(shape), dt, kind="ExternalInput").ap()
    dt_int = lambda name, shape, dt=F32: nc.dram_tensor(name, list(shape), dt, kind="Internal").ap()
    x_in = dt_in("x", [T, D])
    cT_in = dt_in("cT", [128, 8])
    ada_w = dt_in("ada_w", [4, D, 6 * D])
    ada_b = dt_in("ada_b", [4, 6 * D])
    ln_g = dt_in("ln_g", [4, 2, D])
    ln_b = dt_in("ln_b", [4, 2, D])
    gla_w_in = dt_in("gla_w_in", [2, D, GIN])
    gla_wg2_f = dt_in("gla_wg2_f", [2, 16, 512])
    gla_bg_f = dt_in("gla_bg_f", [2, 512])
    gla_wg2_b = dt_in("gla_wg2_b", [2, 16, 512])
    gla_bg_b = dt_in("gla_bg_b", [2, 512])
    gla_norm_g = dt_in("gla_norm_g", [2, 256])
    gla_w_out = dt_in("gla_w_out", [2, D, D])
    sgu_w_in = dt_in("sgu_w_in", [2, D, 2 * D])
    sgu_b_in = dt_in("sgu_b_in", [2, 2 * D])
    sgu_ln_g = dt_in("sgu_ln_g", [2, D])
    sgu_ln_b = dt_in("sgu_ln_b", [2, D])
    sgu_w_sT = dt_in("sgu_w_sT", [2, 128, 8, 128])
    sgu_b_sT = dt_in("sgu_b_sT", [2, 128, 8])
    sgu_w_out = dt_in("sgu_w_out", [2, D, D])
    sgu_b_out = dt_in("sgu_b_out", [2, D])
    moe_w_router = dt_in("moe_w_router", [4, D, NE])
    moe_b_router = dt_in("moe_b_router", [4, NE])
    moe_w_up = dt_in("moe_w_up", [4, NE, D, 2 * D])
    moe_b_upT = dt_in("moe_b_upT", [4, 128, NE, 16])
    moe_w_down = dt_in("moe_w_down", [4, NE, D, D])
    moe_b_down = dt_in("moe_b_down", [4, NE, D])
    consts_in = dt_in("consts", [128, NCONST * 128])
    dumpidx_in = dt_in("dumpidx", [128, 4])
    ecrow_in = dt_in("ecrow", [1, NE])
    out = nc.dram_tensor("out", [T, D], F32, kind="ExternalOutput").ap()

    xa = dt_int("xa", [T, D])
    xb_ = dt_int("xb", [T, D])
    modd = dt_int("modd", [4, 6 * D])
    sbn = dt_int("sbn", [NT, 128, 1024], BF16)
    Xs = dt_int("Xs", [NSLOT + NDUMP, D], BF16)
    Ws = dt_int("Ws", [NSLOT + NDUMP, 8])
    Ys = dt_int("Ys", [NSLOT + NDUMP, D])

    K = KB(nc)
    S = K.S
    dbg_outs = {}

    def dbg(name, ap, tok, dt=F32):
        if not DEBUG:
            return
        shape = [ap.shape[0], int(np.prod(ap.shape[1:]))]
        d_ = nc.dram_tensor("dbg_" + name, shape, dt, kind="ExternalOutput").ap()
        src = ap if len(ap.shape) == 2 else ap.rearrange("p a b -> p (a b)")
        K.dma("sp", d_, src, [tok], [], "dbg_" + name)
        dbg_outs[name] = d_

    c32 = K.sb([128, NCONST * 128], F32)
    cb = K.sb([128, NCONST * 128], BF16)
    ones_r32 = K.sb([1, 512], F32)
    ones_rb = K.sb([1, 512], BF16)
    nhalf = K.sb([128, 4], F32)
    K.dma("sp", c32, consts_in, [], ["c32"], "c32")
    K.cp("dve", cb, c32, ["c32"], ["cb"])
    K.memset("pool", ones_r32, 1.0, ["ones_r32"])
    K.cp("dve", ones_rb, ones_r32, ["ones_r32"], ["ones_rb"])
    K.memset("pool", nhalf, -0.5, ["nhalf"])
    cm = lambda t, i: t[:, i * 128:(i + 1) * 128]
    id32, idb = cm(c32, 0), cm(cb, 0)
    CT = ["c32", "cb", "ones_r32", "ones_rb", "nhalf"]
    K.persist()

    def phase_mod():
        K.phase()
        cT = K.sb([128, 8], F32)
        sc = K.sb([128, 8], F32)
        wt = [K.sb([128, 8, 512], F32) for _ in range(2)]
        ab = K.sb([1, 6 * D], F32)
        mrow = K.sb([1, 6 * D], F32)
        pm = K.ps(1)
        K.dma("sp", cT, cT_in, [], ["cT"], "cT")
        K.act(sc, cT, AF.Silu, ["cT"], ["sc"])
        n = 0
        for l in layer_list:
            K.dma("sp", ab, ada_b[l:l + 1, :], [], ["ab"], "ab")
            for j in range(12):
                b = n % 2
                n += 1
                K.dma("sp", wt[b], ada_w[l][:, j * 512:(j + 1) * 512].rearrange("(k p) n -> p k n", p=128), [], [("wt", b)], "wt%d" % b)
                for k in range(8):
                    K.mm(pm[0:1, :], sc[:, k:k + 1], wt[b][:, k, :], k == 0, k == 7, ["sc", ("wt", b)], ["pm"])
                plus = 1.0 if (j // 2) in (1, 2, 4, 5) else 0.0
                K.stt("dve", mrow[:, j * 512:(j + 1) * 512], pm[0:1, :], plus, ab[:, j * 512:(j + 1) * 512], ALU.add, ALU.add, ["pm", "ab"], ["mrow"])
            K.dma("sp", modd[l:l + 1, :], mrow, ["mrow"], ["modd"], "mrow")

    def load_bc(dst, row, tok):
        K.dma("sp", dst, row.partition_broadcast(128), ["modd"], [tok], "bc")

    def alloc_bc(sub, l, names=("scale", "shift", "gate", "lng", "lnb"), t=None):
        t = {} if t is None else t
        o = sub * 3 * D
        srcs = {"shift": modd[l:l + 1, o:o + D], "scale": modd[l:l + 1, o + D:o + 2 * D], "gate": modd[l:l + 1, o + 2 * D:o + 3 * D],
                "lng": ln_g[l, sub:sub + 1, :], "lnb": ln_b[l, sub:sub + 1, :]}
        for nm in names:
            t[nm] = K.sb([128, D], F32)
            load_bc(t[nm], srcs[nm], "bc_" + nm)
        return t

    def alloc_work(t):
        t["xt"] = [K.sb([128, D], F32) for _ in range(2)]
        t["h32"] = K.sb([128, D], F32)
        t["z"] = K.sb([128, D], F32)
        t["xo"] = [K.sb([128, D], F32) for _ in range(2)]
        t["st"] = K.sb([128, 2, 6], F32)
        t["mv"] = K.sb([128, 8], F32)
        return t

    def alloc_common(sub, l):
        return alloc_work(alloc_bc(sub, l))

    BCT = ["bc_shift", "bc_scale", "bc_gate", "bc_lng", "bc_lnb"]

    def front(cm_, xsrc, t, hb):
        b = t % 2
        K.dma("sp", cm_["xt"][b], xsrc[t * 128:(t + 1) * 128, :], ["xsrc"], [("xt", b)], "xt%d" % b)
        K.tt("dve", cm_["h32"], cm_["xt"][b], cm_["scale"], ALU.mult, [("xt", b), "bc_scale"], ["h32"])
        if hb is not None:
            K.tt("dve", hb, cm_["h32"], cm_["shift"], ALU.add, ["h32", "bc_shift"], ["hb"])
        else:
            K.tt("dve", cm_["h32"], cm_["h32"], cm_["shift"], ALU.add, ["h32", "bc_shift"], ["h32"])

    def back(cm_, xdst, t, y_ap, ytok):
        b = t % 2
        z, mv, st = cm_["z"], cm_["mv"], cm_["st"]
        K.tt("dve", z, y_ap, cm_["gate"], ALU.mult, [ytok, "bc_gate"], ["z"])
        K.stt("dve", z, cm_["xt"][b], ALPHA, z, ALU.mult, ALU.add, [("xt", b), "z"], ["z"])
        for c in range(2):
            S.op("dve", lambda e, c=c: e.bn_stats(out=st[:, c, :], in_=z[:, c * 512:(c + 1) * 512]), ["z"], ["st"])
        S.op("dve", lambda e: e.bn_aggr(out=mv[:, 0:2], in_=st.rearrange("p a b -> p (a b)")), ["st"], ["mv"])
        K.ts("dve", mv[:, 2:3], mv[:, 1:2], EPS, None, ALU.add, None, ["mv"], ["mv"])
        K.tt("pool", mv[:, 3:4], mv[:, 2:3], nhalf[:, 0:1], ALU.pow, ["mv", "nhalf"], ["mv"])
        K.stt("dve", mv[:, 4:5], mv[:, 0:1], -1.0, mv[:, 3:4], ALU.mult, ALU.mult, ["mv"], ["mv"])
        K.act(z, z, AF.Identity, ["z", "mv"], ["z"], bias=mv[:, 4:5], scale=mv[:, 3:4])
        K.tt("dve", z, z, cm_["lng"], ALU.mult, ["z", "bc_lng"], ["z"])
        K.tt("pool", cm_["xo"][b], z, cm_["lnb"], ALU.add, ["z", "bc_lnb"], [("xo", b)])
        K.dma("sp", xdst[t * 128:(t + 1) * 128, :], cm_["xo"][b], [("xo", b)], ["xdst"], "xo%d" % b)

    def transpose8(src_b, dstT, pT, srctok, dsttok, ident=None, eng="act"):
        ident = idb if ident is None else ident
        for half in range(2):
            for j in range(4):
                k = half * 4 + j
                K.tr(pT[:, j, :], src_b[:, k * 128:(k + 1) * 128], ident, [srctok, "cb", "c32"], ["pT"])
            K.cp(eng, dstT[:, half * 4:(half + 1) * 4, :], pT, ["pT"], [dsttok])

    def load_w_cast(dst, src, tok, tag):
        N = src.shape[-1]
        c0 = 0
        while c0 < N:
            c1 = min(N, c0 + 2048)
            K.dma("pool", dst[:, :, c0:c1], src[:, c0:c1].rearrange("(k p) n -> p k n", p=128), [], [tok], tag)
            c0 = c1

    def load_row_bf16(dst_b, tmp32, src_row, tok):
        K.dma("sp", tmp32, src_row, [], [tok + "_32"], "rows")
        K.cp("dve", dst_b, tmp32, [tok + "_32"], [tok])

    def phase_sgu(l, xsrc, xdst):
        i = l // 2
        K.phase()
        cm_ = alloc_common(0, l)
        win = K.sb([128, 8, 2 * D], BF16)
        wout = K.sb([128, 8, D], BF16)
        wsT = K.sb([128, 8, 128], BF16)
        ws32 = K.sb([128, 8, 128], F32)
        bsT = K.sb([128, 8], F32)
        r32 = K.sb([1, 2 * D], F32)
        r32b = K.sb([1, D], F32)
        binr = K.sb([1, 2 * D], BF16)
        boutr = K.sb([1, D], BF16)
        slg = K.sb([128, D], F32)
        slb = K.sb([128, D], F32)
        hb = K.sb([128, D], BF16)
        hT = K.sb([128, 8, 128], BF16)
        u = K.sb([128, D], F32)
        v = K.sb([128, D], F32)
        vb = K.sb([128, D], BF16)
        gb = K.sb([128, D], BF16)
        gT = K.sb([128, 8, 128], BF16)
        st2 = K.sb([128, 2, 6], F32)
        mv2 = K.sb([128, 8], F32)
        pT = K.ps(1).bitcast(BF16)[:, 0:512].rearrange("p (a b) -> p a b", a=4)
        pz = K.ps(2)
        psv = K.ps(2)
        py = K.ps(2)
        load_w_cast(win, sgu_w_in[i], "win", "wload")
        load_w_cast(wout, sgu_w_out[i], "wout", "wload")
        K.dma("sp", ws32, sgu_w_sT[i], [], ["ws32"], "rows")
        K.cp("dve", wsT, ws32, ["ws32"], ["wsT"])
        K.dma("sp", bsT, sgu_b_sT[i], [], ["bsT"], "rows")
        load_row_bf16(binr, r32, sgu_b_in[i:i + 1, :], "binr")
        load_row_bf16(boutr, r32b, sgu_b_out[i:i + 1, :], "boutr")
        K.dma("sp", slg, sgu_ln_g[i:i + 1, :].partition_broadcast(128), [], ["slg"], "bc")
        K.dma("sp", slb, sgu_ln_b[i:i + 1, :].partition_broadcast(128), [], ["slb"], "bc")
        for t in range(NT):
            front(cm_, xsrc, t, hb)
            transpose8(hb, hT, pT, "hb", "hT")
            for part, dst in ((0, u), (1, v)):
                for c in range(2):
                    col = part * D + c * 512
                    for k in range(8):
                        K.mm(pz[:, c * 512:(c + 1) * 512], hT[:, k, :], win[:, k, col:col + 512], k == 0, False, ["hT", "win"], ["pz"])
                    K.mm(pz[:, c * 512:(c + 1) * 512], ones_rb[0:1, 0:128], binr[0:1, col:col + 512], False, True, ["ones_rb", "binr"], ["pz"])
                K.act(dst, pz, AF.Gelu, ["pz"], ["u" if part == 0 else "v"])
            for c in range(2):
                S.op("dve", lambda e, c=c: e.bn_stats(out=st2[:, c, :], in_=v[:, c * 512:(c + 1) * 512]), ["v"], ["st2"])
            S.op("dve", lambda e: e.bn_aggr(out=mv2[:, 0:2], in_=st2.rearrange("p a b -> p (a b)")), ["st2"], ["mv2"])
            K.ts("dve", mv2[:, 2:3], mv2[:, 1:2], EPS, None, ALU.add, None, ["mv2"], ["mv2"])
            K.tt("pool", mv2[:, 3:4], mv2[:, 2:3], nhalf[:, 0:1], ALU.pow, ["mv2", "nhalf"], ["mv2"])
            K.ts("dve", v, v, mv2[:, 0:1], mv2[:, 3:4], ALU.subtract, ALU.mult, ["v", "mv2"], ["v"])
            K.tt("pool", v, v, slg, ALU.mult, ["v", "slg"], ["v"])
            K.tt("pool", vb, v, slb, ALU.add, ["v", "slb"], ["vb"])
            if t == 0:
                dbg("u", u, "u"); dbg("vb", vb, "vb", BF16); dbg("hb", hb, "hb", BF16); dbg("wsT", wsT, "wsT", BF16); dbg("mv2", mv2, "mv2")
            for g in range(8):
                K.mm(psv[:, g * 128:(g + 1) * 128], wsT[:, g, :], vb[:, g * 128:(g + 1) * 128], True, True, ["wsT", "vb"], ["psv"])
            if t == 0:
                K.cp("dve", cm_["h32"], psv, ["psv"], ["h32"]); dbg("psv", cm_["h32"], "h32")
            for g in range(8):
                K.stt("dve", gb[:, g * 128:(g + 1) * 128], psv[:, g * 128:(g + 1) * 128], bsT[:, g:g + 1], u[:, g * 128:(g + 1) * 128],
                      ALU.add, ALU.mult, ["psv", "bsT", "u"], ["gb"])
            if t == 0:
                dbg("gb", gb, "gb", BF16)
            transpose8(gb, gT, pT, "gb", "gT")
            for c in range(2):
                for k in range(8):
                    K.mm(py[:, c * 512:(c + 1) * 512], gT[:, k, :], wout[:, k, c * 512:(c + 1) * 512], k == 0, False, ["gT", "wout"], ["py"])
                K.mm(py[:, c * 512:(c + 1) * 512], ones_rb[0:1, 0:128], boutr[0:1, c * 512:(c + 1) * 512], False, True, ["ones_rb", "boutr"], ["py"])
            back(cm_, xdst, t, py, "py")

    def phase_gla(l, xsrc, xdst):
        i = l // 2
        K.phase()
        cm_ = alloc_common(0, l)
        win = K.sb([128, 8, GIN], BF16)
        wout = K.sb([128, 8, D], BF16)
        wg2 = [K.sb([16, 512], F32) for _ in range(2)]
        bg = [K.sb([1, 512], F32) for _ in range(2)]
        ng4 = K.sb([128, D], F32)
        hb = K.sb([128, D], BF16)
        hT = K.sb([128, 8, 128], BF16)
        vb = K.sb([128, D], BF16)
        kt32 = K.sb([128, 512], F32)
        lr = K.sb([16, 256], F32)
        rs = K.sb([128, D], F32)
        ex = K.sb([128, 512], F32)
        spl = [K.sb([128, 512], F32) for _ in range(2)]
        Eq = [K.sb([128, 512], F32) for _ in range(2)]
        Ek = [K.sb([128, 512], F32) for _ in range(2)]
        Ed = K.sb([128, 512], F32)
        qT = [K.sb([128, 512], BF16) for _ in range(2)]
        kT = [K.sb([128, 512], BF16) for _ in range(2)]
        kh = K.sb([128, 512], BF16)
        dec = K.sb([128, 4], F32)
        S32 = K.sb([128, D], F32)
        Sb = K.sb([128, D], BF16)
        Sbn = K.sb([128, D], BF16)
        sm32 = K.sb([128, 512], F32)
        smb = K.sb([128, 512], BF16)
        Mf4 = K.sb([128, 512], F32)
        Mb4 = K.sb([128, 512], F32)
        ss = K.sb([128, 8], F32)
        junk = K.sb([128, 256], F32)
        gb = K.sb([128, D], BF16)
        gT = K.sb([128, 8, 128], BF16)
        pT = K.ps(1).bitcast(BF16)[:, 0:512].rearrange("p (a b) -> p a b", a=4)
        pA = K.ps(2)
        pB = K.ps(2)
        pQ = K.ps(1)
        pK = K.ps(1)
        pM = K.ps(1)

        load_w_cast(win, gla_w_in[i], "win", "wload")
        load_w_cast(wout, gla_w_out[i], "wout", "wload")
        K.dma("sp", wg2[0], gla_wg2_f[i], [], ["wg2"], "rows")
        K.dma("sp", wg2[1], gla_wg2_b[i], [], ["wg2"], "rows")
        K.dma("sp", bg[0], gla_bg_f[i:i + 1, :], [], ["wg2"], "rows")
        K.dma("sp", bg[1], gla_bg_b[i:i + 1, :], [], ["wg2"], "rows")
        for hh in range(4):
            K.dma("sp", ng4[:, hh * 256:(hh + 1) * 256], gla_norm_g[i:i + 1, :].partition_broadcast(128), [], ["ng4"], "bc")
            K.cp("dve", Mf4[:, hh * 128:(hh + 1) * 128], cm(c32, 5), ["c32"], ["Mf4"])
            K.cp("dve", Mb4[:, hh * 128:(hh + 1) * 128], cm(c32, 6), ["c32"], ["Mb4"])

        def softplus_neg(dirn):
            K.mm(pM, lr[0:16, dirn * 128:(dirn + 1) * 128], wg2[dirn], True, False, ["lr", "wg2"], ["pM"])
            K.mm(pM, ones_r32[0:1, 0:128], bg[dirn], False, True, ["ones_r32", "wg2"], ["pM"])
            K.act(ex, pM, AF.Exp, ["pM"], ["ex"], scale=-1.0)
            K.ts("dve", ex, ex, 1.0, None, ALU.add, None, ["ex"], ["ex"])
            K.act(spl[dirn], ex, AF.Ln, ["ex"], [("spl", dirn)])

        def proj_common(t, need_q):
            front(cm_, xsrc, t, hb)
            transpose8(hb, hT, pT, "hb", "hT")
            for c in range(2):
                for k in range(8):
                    K.mm(pA[:, c * 512:(c + 1) * 512], hT[:, k, :], win[:, k, 1024 + c * 512:1024 + (c + 1) * 512], k == 0, k == 7, ["hT", "win"], ["pA"])
            K.cp("act", vb, pA, ["pA"], ["vb"])
            for k in range(8):
                K.mm(pM, hT[:, k, :], win[:, k, 512:1024], k == 0, k == 7, ["hT", "win"], ["pM"])
            K.cp("dve", kt32, pM, ["pM"], ["kt32"])
            for dirn in range(2):
                for k in range(8):
                    K.mm(pM[0:16, dirn * 128:(dirn + 1) * 128], win[:, k, 3072 + dirn * 16:3072 + (dirn + 1) * 16], hT[:, k, :], k == 0, k == 7, ["hT", "win"], ["pM"])
            K.cp("dve", lr, pM[0:16, 0:256], ["pM"], ["lr"])

        def state_update(dirn, Lmat, col):
            K.mm(pM, Lmat, spl[dirn], True, True, ["c32", ("spl", dirn)], ["pM"])
            K.act(Ed, pM, AF.Exp, ["pM"], ["Ed"])
            K.tt("dve", kh, kt32, Ed, ALU.mult, ["kt32", "Ed"], ["kh"])
            for hh in range(4):
                K.mm(pB[:, hh * 256:(hh + 1) * 256], kh[:, hh * 128:(hh + 1) * 128], vb[:, hh * 256:(hh + 1) * 256], True, True, ["kh", "vb"], ["pB"])
            for hh in range(4):
                K.stt("dve", S32[:, hh * 256:(hh + 1) * 256], S32[:, hh * 256:(hh + 1) * 256], dec[:, hh:hh + 1], pB[:, hh * 256:(hh + 1) * 256],
                      ALU.mult, ALU.add, ["S32", "dec", "pB"], ["S32"])
            K.cp("act", Sb, S32, ["S32"], ["Sb"])

        K.memset("pool", S32, 0.0, ["S32"])
        K.memset("pool", Sb, 0.0, ["Sb"])
        for t in range(NT - 1, -1, -1):
            proj_common(t, False)
            softplus_neg(1)
            K.dma("sp", sbn[t], Sb, ["Sb"], [("sbn", t)], "sbn")
            for hh in range(4):
                K.mm(pQ[:, hh:hh + 1], spl[1][:, hh * 128:(hh + 1) * 128], cm(c32, 1)[:, 127:128], True, True, [("spl", 1), "c32"], ["pQ"])
            K.act(dec, pQ[:, 0:4], AF.Exp, ["pQ"], ["dec"])
            state_update(1, cm(c32, 4), None)

        K.memset("pool", S32, 0.0, ["S32"])
        K.memset("pool", Sb, 0.0, ["Sb"])
        for t in range(NT):
            proj_common(t, True)
            K.dma("sp", Sbn, sbn[t], [("sbn", t)], ["Sbn"], "sbnl")
            for c in range(2):
                for k in range(8):
                    K.mm(pB[:, c * 512:(c + 1) * 512], hT[:, k, :], win[:, k, 2048 + c * 512:2048 + (c + 1) * 512], k == 0, k == 7, ["hT", "win"], ["pB"])
            K.act(rs, pB, AF.Silu, ["pB"], ["rs"])
            K.tt("pool", rs, rs, ng4, ALU.mult, ["rs", "ng4"], ["rs"])
            for hh in range(4):
                for k in range(8):
                    K.mm(pQ[:, hh * 128:(hh + 1) * 128], win[:, k, hh * 128:(hh + 1) * 128], hT[:, k, :], k == 0, k == 7, ["hT", "win"], ["pQ"])
            for hh in range(4):
                for k in range(8):
                    K.mm(pK[:, hh * 128:(hh + 1) * 128], win[:, k, 512 + hh * 128:512 + (hh + 1) * 128], hT[:, k, :], k == 0, k == 7, ["hT", "win"], ["pK"])
            softplus_neg(0)
            softplus_neg(1)
            for dirn in range(2):
                U = cm(c32, 1 + dirn)
                for hh in range(4):
                    K.mm(pM[:, hh * 128:(hh + 1) * 128], spl[dirn][:, hh * 128:(hh + 1) * 128], U, True, True, [("spl", dirn), "c32"], ["pM"])
                K.act(Eq[dirn], pM, AF.Exp, ["pM"], [("Eq", dirn)])
                K.act(Ek[dirn], pM, AF.Exp, ["pM"], [("Ek", dirn)], scale=-1.0)
                K.stt("dve", qT[dirn], pQ, 128.0 ** -0.5, Eq[dirn], ALU.mult, ALU.mult, ["pQ", ("Eq", dirn)], [("qT", dirn)])
                K.tt("dve", kT[dirn], pK, Ek[dirn], ALU.mult, ["pK", ("Ek", dirn)], [("kT", dirn)])
            for hh in range(4):
                K.mm(pM[:, hh * 128:(hh + 1) * 128], kT[0][:, hh * 128:(hh + 1) * 128], qT[0][:, hh * 128:(hh + 1) * 128], True, True, [("kT", 0), ("qT", 0)], ["pM"])
            K.tt("dve", sm32, pM, Mf4, ALU.mult, ["pM", "Mf4"], ["sm32"])
            for hh in range(4):
                K.mm(pM[:, hh * 128:(hh + 1) * 128], kT[1][:, hh * 128:(hh + 1) * 128], qT[1][:, hh * 128:(hh + 1) * 128], True, True, [("kT", 1), ("qT", 1)], ["pM"])
            K.tt("dve", ex, pM, Mb4, ALU.mult, ["pM", "Mb4"], ["ex"])
            K.tt("dve", smb, sm32, ex, ALU.add, ["sm32", "ex"], ["smb"])
            for hh in range(4):
                o_ = pA[:, hh * 256:(hh + 1) * 256]
                K.mm(o_, smb[:, hh * 128:(hh + 1) * 128], vb[:, hh * 256:(hh + 1) * 256], True, False, ["smb", "vb"], ["pA"])
                K.mm(o_, qT[0][:, hh * 128:(hh + 1) * 128], Sb[:, hh * 256:(hh + 1) * 256], False, False, [("qT", 0), "Sb"], ["pA"])
                K.mm(o_, qT[1][:, hh * 128:(hh + 1) * 128], Sbn[:, hh * 256:(hh + 1) * 256], False, True, [("qT", 1), "Sbn"], ["pA"])
            for hh in range(4):
                K.cp("dve", dec[:, hh:hh + 1], Eq[0][:, hh * 128 + 127:hh * 128 + 128], [("Eq", 0)], ["dec"])
            for hh in range(4):
                K.act(junk, pA[:, hh * 256:(hh + 1) * 256], AF.Square, ["pA"], ["junk", "ss"], accum=ss[:, hh:hh + 1])
            K.ts("dve", ss[:, 4:8], ss[:, 0:4], 1.0 / 256.0, EPS, ALU.mult, ALU.add, ["ss"], ["ss"])
            K.tt("pool", ss[:, 0:4], ss[:, 4:8], nhalf, ALU.pow, ["ss", "nhalf"], ["ss"])
            for hh in range(4):
                K.stt("dve", gb[:, hh * 256:(hh + 1) * 256], pA[:, hh * 256:(hh + 1) * 256], ss[:, hh:hh + 1], rs[:, hh * 256:(hh + 1) * 256],
                      ALU.mult, ALU.mult, ["pA", "ss", "rs"], ["gb"])
            state_update(0, cm(c32, 3), None)
            transpose8(gb, gT, pT, "gb", "gT")
            for c in range(2):
                for k in range(8):
                    K.mm(pB[:, c * 512:(c + 1) * 512], gT[:, k, :], wout[:, k, c * 512:(c + 1) * 512], k == 0, k == 7, ["gT", "wout"], ["pB"])
            back(cm_, xdst, t, pB, "pB")

    def phase_moe(l, xsrc, xdst):
        K.phase()
        cm_ = {}
        wr = K.sb([128, 8, NE], F32)
        br = K.sb([1, NE], F32)
        desti = K.sb([128, NT * 4], I32)
        dumpf = K.sb([128, 4], F32)
        ecb = K.sb([128, NE], F32)
        cnt = K.sb([128, NE], F32)
        K.dma("sp", wr, moe_w_router[l].rearrange("(k p) n -> p k n", p=128), [], ["wr"], "rows")
        K.dma("sp", br, moe_b_router[l:l + 1, :], [], ["wr"], "rows")
        K.dma("sp", dumpf, dumpidx_in, [], ["dumpf"], "rows")
        K.dma("sp", ecb, ecrow_in.partition_broadcast(128), [], ["ecb"], "bc")
        K.memset("pool", cnt, 0.0, ["cnt"])
        mark = K.off
        alloc_bc(1, l, ("scale", "shift"), cm_)
        alloc_work(cm_)
        hb = K.sb([128, D], BF16)
        hT32 = K.sb([128, 8, 128], F32)
        lg = K.sb([128, NE], F32)
        top8 = K.sb([128, 8], F32)
        sm = K.sb([128, 8], F32)
        w4 = K.sb([128, 4], F32)
        w4x = K.sb([128, 4, 8], F32)
        oh = [K.sb([128, NE], F32) for _ in range(4)]
        maskb = K.sb([128, NE], BF16)
        rank = K.sb([128, NE], F32)
        over = K.sb([128, NE], F32)
        dfull = K.sb([128, NE], F32)
        tmp = K.sb([128, NE], F32)
        destf = K.sb([128, 8], F32)
        pT32 = K.ps(1).rearrange("p (a b) -> p a b", a=4)
        pL = K.ps(1)
        pR = K.ps(1)
        K.memset("pool", w4x, 0.0, ["w4x"])
        for t in range(NT):
            front(cm_, xsrc, t, hb)
            K.tt("dve", cm_["h32"], cm_["h32"], cm_["shift"], ALU.add, ["h32", "bc_shift"], ["h32"])
            transpose8(cm_["h32"], hT32, pT32, "h32", "hT32", ident=id32)
            for k in range(8):
                K.mm(pL[:, 0:NE], hT32[:, k, :], wr[:, k, :], k == 0, False, ["hT32", "wr"], ["pL"])
            K.mm(pL[:, 0:NE], ones_r32[0:1, 0:128], br, False, True, ["ones_r32", "wr"], ["pL"])
            K.cp("dve", lg, pL[:, 0:NE], ["pL"], ["lg"])
            S.op("dve", lambda e: e.max(out=top8, in_=lg), ["lg"], ["top8"])
            K.ts("dve", sm[:, 0:1], top8[:, 0:1], -1.0, None, ALU.mult, None, ["top8"], ["sm"])
            K.act(w4, top8[:, 0:4], AF.Exp, ["top8", "sm"], ["w4", "sm"], bias=sm[:, 0:1], accum=sm[:, 1:2])
            S.op("dve", lambda e: e.reciprocal(out=sm[:, 2:3], in_=sm[:, 1:2]), ["sm"], ["sm"])
            K.ts("dve", w4, w4, sm[:, 2:3], None, ALU.mult, None, ["w4", "sm"], ["w4"])
            for k in range(4):
                K.cp("dve", w4x[:, k, 0:1], w4[:, k:k + 1], ["w4"], ["w4x"])
                K.ts("dve", oh[k], lg, top8[:, k:k + 1], None, ALU.is_equal, None, ["lg", "top8"], [("oh", k)])
            K.ts("dve", maskb, lg, top8[:, 3:4], None, ALU.is_ge, None, ["lg", "top8"], ["maskb"])
            K.mm(pR[:, 0:NE], cm(cb, 8), maskb, True, True, ["cb", "maskb"], ["pR"])
            K.tt("dve", rank, pR[:, 0:NE], cnt, ALU.add, ["pR", "cnt"], ["rank"])
            K.mm(pR[:, 0:NE], cm(cb, 7), maskb, True, True, ["cb", "maskb"], ["pR"])
            K.tt("dve", cnt, pR[:, 0:NE], cnt, ALU.add, ["pR", "cnt"], ["cnt"])
            K.ts("dve", over, rank, float(C), None, ALU.is_ge, None, ["rank"], ["over"])
            K.tt("dve", dfull, rank, ecb, ALU.add, ["rank", "ecb"], ["dfull"])
            K.ts("dve", tmp, over, -1.0, 1.0, ALU.mult, ALU.add, ["over"], ["tmp"])
            K.tt("dve", dfull, dfull, tmp, ALU.mult, ["dfull", "tmp"], ["dfull"])
            for k in range(4):
                K.tt("dve", tmp, oh[k], dfull, ALU.mult, [("oh", k), "dfull"], ["tmp"])
                S.op("dve", lambda e, k=k: e.reduce_sum(out=destf[:, k:k + 1], in_=tmp, axis=AX.X), ["tmp"], ["destf"])
                K.tt("dve", tmp, oh[k], over, ALU.mult, [("oh", k), "over"], ["tmp"])
                S.op("dve", lambda e, k=k: e.reduce_sum(out=destf[:, 4 + k:5 + k], in_=tmp, axis=AX.X), ["tmp"], ["destf"])
            K.tt("dve", destf[:, 4:8], destf[:, 4:8], dumpf, ALU.mult, ["destf", "dumpf"], ["destf"])
            K.tt("dve", destf[:, 0:4], destf[:, 0:4], destf[:, 4:8], ALU.add, ["destf"], ["destf"])
            K.cp("dve", desti[:, t * 4:(t + 1) * 4], destf[:, 0:4], ["destf"], [("desti", t)])
            for k in range(4):
                idx = desti[:, t * 4 + k:t * 4 + k + 1]
                S.op("pool", lambda e, idx=idx: e.indirect_dma_start(out=Xs, out_offset=bass.IndirectOffsetOnAxis(ap=idx, axis=0), in_=hb, in_offset=None),
                     ["hb", ("desti", t)], ["Xs"], dma_tag="scatx%d" % k)
                S.op("pool", lambda e, idx=idx, k=k: e.indirect_dma_start(out=Ws, out_offset=bass.IndirectOffsetOnAxis(ap=idx, axis=0), in_=w4x[:, k, :], in_offset=None),
                     ["w4x", ("desti", t)], ["Ws"], dma_tag="scatw%d" % k)
        S.fence()
        K.off = mark
        K.pso = 0
        GS = 512 if C % 512 == 0 else 384
        NG = C // GS
        NB = GS // 128
        assert NG * GS == C
        bup = K.sb([128, NE, 16], F32)
        K.dma("sp", bup, moe_b_upT[l], [], ["bup"], "rows")
        K.ts("dve", bup[:, :, 8:16], bup[:, :, 8:16], 1.0, None, ALU.add, None, ["bup"], ["bup"])
        wup = [K.sb([128, 8, 2 * D], BF16) for _ in range(2)]
        wdn = [K.sb([128, 8, D], BF16) for _ in range(2)]
        NST = 3
        stage = [K.sb([128, 2 * D], F32) for _ in range(NST)]
        bd32 = [K.sb([1, D], F32) for _ in range(2)]
        bdb = [K.sb([1, D], BF16) for _ in range(2)]
        xs = [K.sb([128, D], BF16) for _ in range(2)]
        wsl = [K.sb([128, 8], F32) for _ in range(16)]
        xsT = [K.sb([128, 8, GS], BF16) for _ in range(2)]
        actT = [K.sb([128, 8, GS], BF16) for _ in range(2)]
        gt = [K.sb([128, GS], F32) for _ in range(2)]
        sg = [K.sb([128, GS], F32) for _ in range(2)]
        lA = [K.sb([128, GS], F32) for _ in range(2)]
        ys = [K.sb([128, D], F32) for _ in range(2)]
        pT = K.ps(1).bitcast(BF16)[:, 0:512].rearrange("p (a b) -> p a b", a=4)
        pG = [K.ps(1) for _ in range(2)]
        pLn = [K.ps(1) for _ in range(2)]
        pY = K.ps(2)
        cnts = {"ns": 0, "nx": 0, "nw": 0, "ny": 0, "nj": 0}

        def weight_steps(e_):
            be = e_ % 2
            steps = []
            for k in range(8):
                def f(k=k):
                    b = cnts["ns"] % NST
                    cnts["ns"] += 1
                    K.dma("sp", stage[b], moe_w_up[l, e_, k * 128:(k + 1) * 128, :], [], [("stage", b)], "stage%d" % b)
                    st_v = stage[b].rearrange("p (f two) -> p two f", two=2)
                    K.cp("act", wup[be][:, k, :].rearrange("p (two f) -> p two f", two=2), st_v, [("stage", b)], [("wup", be)])
                steps.append(f)
            for k2 in range(4):
                def f(k2=k2):
                    b = cnts["ns"] % NST
                    cnts["ns"] += 1
                    K.dma("sp", stage[b].rearrange("p (a n) -> p a n", a=2),
                          moe_w_down[l, e_, k2 * 256:(k2 + 1) * 256, :].rearrange("(a p) n -> p a n", p=128), [], [("stage", b)], "stage%d" % b)
                    K.cp("dve", wdn[be][:, 2 * k2:2 * k2 + 2, :], stage[b].rearrange("p (a n) -> p a n", a=2), [("stage", b)], [("wdn", be)])
                steps.append(f)

            def f():
                K.dma("sp", bd32[be], moe_b_down[l, e_:e_ + 1, :], [], [("bd32", be)], "bd32_%d" % be)
                K.cp("dve", bdb[be], bd32[be], [("bd32", be)], [("bdb", be)])
            steps.append(f)
            return steps

        units = [(e_, g) for e_ in range(NE) for g in range(NG)]
        wslots = {}

        def unit_A(u):
            e_, g = units[u]
            gb_ = u % 2
            base = e_ * C + g * GS
            wl = []
            for blk in range(NB):
                b = cnts["nx"] % 2
                cnts["nx"] += 1
                wi = cnts["nw"] % 16
                cnts["nw"] += 1
                wl.append(wi)
                r0 = base + blk * 128
                K.dma("sp", xs[b], Xs[r0:r0 + 128, :], ["Xs"], [("xs", b)], "xs%d" % b)
                K.dma("sp", wsl[wi], Ws[r0:r0 + 128, :], ["Ws"], [("wsl", wi)], "wsl%d" % wi)
                for half in range(2):
                    for j in range(4):
                        k = half * 4 + j
                        K.tr(pT[:, j, :], xs[b][:, k * 128:(k + 1) * 128], idb, [("xs", b), "cb"], ["pT"])
                    K.cp("act" if half == 0 else "dve", xsT[gb_][:, half * 4:(half + 1) * 4, blk * 128:(blk + 1) * 128], pT, ["pT"], [("xsT", gb_)])
            wslots[u] = wl

        def unit_U(u, pref):
            e_, g = units[u]
            be = e_ % 2
            gb_ = u % 2
            for j in range(8):
                jb = cnts["nj"] % 2
                cnts["nj"] += 1
                for k in range(8):
                    K.mm(pG[jb][:, 0:GS], wup[be][:, k, j * 128:(j + 1) * 128], xsT[gb_][:, k, :], k == 0, k == 7, [("wup", be), ("xsT", gb_)], [("pG", jb)])
                for k in range(8):
                    K.mm(pLn[jb][:, 0:GS], wup[be][:, k, D + j * 128:D + (j + 1) * 128], xsT[gb_][:, k, :], k == 0, k == 7, [("wup", be), ("xsT", gb_)], [("pLn", jb)])
                K.ts("dve", gt[jb], pG[jb][:, 0:GS], bup[:, e_, j:j + 1], 7.0, ALU.add, ALU.min, [("pG", jb), "bup"], [("gt", jb)])
                K.act(sg[jb], gt[jb], AF.Sigmoid, [("gt", jb)], [("sg", jb)], scale=1.702)
                K.ts("dve", lA[jb], pLn[jb][:, 0:GS], bup[:, e_, 8 + j:9 + j], -6.0, ALU.add, ALU.max, [("pLn", jb), "bup"], [("lA", jb)])
                K.tt("dve", gt[jb], gt[jb], sg[jb], ALU.mult, [("gt", jb), ("sg", jb)], [("gt", jb)])
                K.stt("dve", actT[gb_][:, j, :], lA[jb], 8.0, gt[jb], ALU.min, ALU.mult, [("lA", jb), ("gt", jb)], [("actT", gb_)])
                if pref:
                    pref.pop(0)()
                    if len(pref) > 8 - j and j % 2 == 1:
                        pref.pop(0)()

        def unit_D(u):
            e_, g = units[u]
            be = e_ % 2
            gb_ = u % 2
            base = e_ * C + g * GS
            for blk in range(NB):
                wi = wslots[u][blk]
                for c in range(2):
                    for k in range(8):
                        K.mm(pY[:, c * 512:(c + 1) * 512], actT[gb_][:, k, blk * 128:(blk + 1) * 128], wdn[be][:, k, c * 512:(c + 1) * 512], k == 0, False, [("actT", gb_), ("wdn", be)], ["pY"])
                    K.mm(pY[:, c * 512:(c + 1) * 512], ones_rb[0:1, 0:128], bdb[be][0:1, c * 512:(c + 1) * 512], False, True, ["ones_rb", ("bdb", be)], ["pY"])
                b = cnts["ny"] % 2
                cnts["ny"] += 1
                K.act(ys[b], pY, AF.Copy, ["pY", ("wsl", wi)], [("ys", b)], scale=wsl[wi][:, 0:1])
                r0 = base + blk * 128
                K.dma("act", Ys[r0:r0 + 128, :], ys[b], [("ys", b)], ["Ys"], "ys%d" % b)

        for f in weight_steps(0):
            f()
        unit_A(0)
        pref = []
        for u in range(len(units)):
            e_, g = units[u]
            if g == 0:
                for f in pref:
                    f()
                pref = weight_steps(e_ + 1) if e_ + 1 < NE else []
            if u + 1 < len(units):
                unit_A(u + 1)
            unit_U(u, pref)
            unit_D(u)
        for f in pref:
            f()
        S.fence()
        K.off = mark
        K.pso = 0
        alloc_bc(1, l, ("gate", "lng", "lnb"), cm_)
        alloc_work(cm_)
        yk = [K.sb([128, D], F32) for _ in range(4)]
        for t in range(NT):
            b = t % 2
            K.dma("sp", cm_["xt"][b], xsrc[t * 128:(t + 1) * 128, :], ["xsrc"], [("xt", b)], "xt%d" % b)
            for k in range(4):
                idx = desti[:, t * 4 + k:t * 4 + k + 1]
                S.op("pool", lambda e, idx=idx, k=k: e.indirect_dma_start(out=yk[k], out_offset=None, in_=Ys, in_offset=bass.IndirectOffsetOnAxis(ap=idx, axis=0)),
                     ["Ys", ("desti", t)], [("yk", k)], dma_tag="gath%d" % k)
            K.tt("dve", yk[0], yk[0], yk[1], ALU.add, [("yk", 0), ("yk", 1)], [("yk", 0)])
            K.tt("pool", yk[2], yk[2], yk[3], ALU.add, [("yk", 2), ("yk", 3)], [("yk", 2)])
            K.tt("dve", yk[0], yk[0], yk[2], ALU.add, [("yk", 0), ("yk", 2)], [("yk", 0)])
            back(cm_, xdst, t, yk[0], ("yk", 0))

    def phase_init():
        K.phase()
        zt = K.sb([128, 4096], F32)
        K.memset("pool", zt, 0.0, ["zt"])
        ztb = zt.bitcast(BF16)
        nrows = NSLOT + NDUMP
        assert nrows % 512 == 0 and (nrows // 128) * 8 <= 4096
        for r in range(0, nrows, 512):
            K.dma("sp", Xs[r:r + 512, :].rearrange("(p a) d -> p (a d)", p=128), ztb[:, 0:4 * D], ["zt"], ["Xs"], "zinit%d" % ((r // 512) % 4))
        K.dma("sp", Ws.rearrange("(p a) d -> p (a d)", p=128), zt[:, 0:(nrows // 128) * 8], ["zt"], ["Ws"], "zinit")
        K.dma("sp", Ys[NSLOT:NSLOT + NDUMP, :].rearrange("(p a) d -> p (a d)", p=128), zt[:, 0:4 * D], ["zt"], ["Ys"], "zinit")

    phase_mod()
    if do_moe:
        phase_init()
    cur = x_in
    bufs = [xa, xb_]
    nb = 0
    stages = []
    for l in layer_list:
        if do_mixer:
            stages.append(("mix", l))
        if do_moe:
            stages.append(("moe", l))
    for si, (kind, l) in enumerate(stages):
        dst = out if si == len(stages) - 1 else bufs[nb % 2]
        nb += 1
        if kind == "mix":
            if l % 2 == 0:
                phase_gla(l, cur, dst)
            else:
                phase_sgu(l, cur, dst)
        else:
            phase_moe(l, cur, dst)
        cur = dst
    cnt = S.emit()
    return nc, cnt


def prep_shared(inputs, C):
    f = lambda a: np.ascontiguousarray(np.asarray(a, dtype=np.float32))
    sh = {}
    for k_ in ("ada_w", "ada_b", "ln_g", "ln_b", "gla_w_in", "gla_wg2_f", "gla_bg_f", "gla_wg2_b", "gla_bg_b", "gla_norm_g", "gla_w_out",
               "sgu_w_in", "sgu_b_in", "sgu_ln_g", "sgu_ln_b", "sgu_w_out", "sgu_b_out", "moe_w_router", "moe_b_router", "moe_w_up",
               "moe_w_down", "moe_b_down"):
        sh[k_] = f(inputs[k_])
    sh["sgu_w_sT"] = f(np.transpose(np.asarray(inputs["sgu_w_s"]), (0, 3, 1, 2)))
    sh["sgu_b_sT"] = f(np.transpose(np.asarray(inputs["sgu_b_s"]), (0, 2, 1)))
    bu = np.asarray(inputs["moe_b_up"], dtype=np.float32)
    bg = bu[:, :, 0::2].reshape(4, NE, 8, 128)
    bl = bu[:, :, 1::2].reshape(4, NE, 8, 128)
    sh["moe_b_upT"] = f(np.transpose(np.concatenate([bg, bl], axis=2), (0, 3, 1, 2)))
    sh["consts"] = make_consts()
    sh["dumpidx"] = (NE * C + np.arange(4)[None, :] * 128 + np.arange(128)[:, None]).astype(np.float32)
    sh["ecrow"] = (np.arange(NE, dtype=np.float32) * C)[None, :]
    return sh


CAP = 1024
_cache = {}


def kernel(**inputs):
    x = np.asarray(inputs["x"], dtype=np.float32)
    c = np.asarray(inputs["c"], dtype=np.float32)
    B, T, _ = x.shape
    key = (T, CAP)
    if key not in _cache:
        _cache[key] = build_program(T, CAP)[0]
    nc = _cache[key]
    sh = prep_shared(inputs, CAP)
    in_maps = []
    for b in range(B):
        m = dict(sh)
        m["x"] = np.ascontiguousarray(x[b])
        m["cT"] = np.ascontiguousarray(c[b].reshape(8, 128).T)
        in_maps.append(m)
    res = run_bass_kernel_spmd(nc, in_maps, core_ids=list(range(B)))
    return np.stack([np.asarray(r["out"], dtype=np.float32) for r in res.results], axis=0)
```

```python
import numpy as np
from contextlib import ExitStack
import concourse.bass as bass
import concourse.mybir as mybir
from concourse.bass_utils import run_bass_kernel_spmd

F32 = mybir.dt.float32
BF16 = mybir.dt.bfloat16
I32 = mybir.dt.int32
AF = mybir.ActivationFunctionType
ALU = mybir.AluOpType
AX = mybir.AxisListType

D = 1024
DEPTH = 4
ALPHA = (2.0 * DEPTH) ** 0.25
EPS = 1e-5
NE = 32
GIN = 3104
NDUMP = 512
DEBUG = False
PIPE_B = False
PIPE_F = True


class _Op:
    __slots__ = ("eng", "fn", "deps", "is_dma", "tag", "val", "signal", "idx")


class Sched:
    ENGS = ("pe", "dve", "act", "pool", "sp")

    def __init__(self, nc, same_engine_sync=True):
        self.nc = nc
        self.ops = []
        self.last_w = {}
        self.readers = {}
        self.same_engine_sync = same_engine_sync
        self.dma_count = {}
        self.last_dma = {}

    def op(self, eng, fn, reads=(), writes=(), dma_tag=None):
        o = _Op()
        o.eng = eng
        o.fn = fn
        o.is_dma = dma_tag is not None
        o.tag = dma_tag
        o.signal = False
        o.idx = len(self.ops)
        deps = {}
        for t in reads:
            w = self.last_w.get(t)
            if w is not None:
                deps[w.idx] = w
        for t in writes:
            w = self.last_w.get(t)
            if w is not None:
                deps[w.idx] = w
            for r in self.readers.get(t, ()):
                deps[r.idx] = r
        if o.is_dma:
            p = self.last_dma.get(dma_tag)
            if p is not None:
                deps[p.idx] = p
            self.last_dma[dma_tag] = o
        o.deps = list(deps.values())
        for t in writes:
            self.last_w[t] = o
            self.readers[t] = []
        for t in reads:
            lst = self.readers.setdefault(t, [])
            key = (o.eng, o.tag)
            lst[:] = [r for r in lst if (r.eng, r.tag) != key]
            lst.append(o)
        if o.is_dma:
            c = self.dma_count.get(dma_tag, 0) + 1
            self.dma_count[dma_tag] = c
            o.val = 16 * c
        self.ops.append(o)
        return o

    def fence(self):
        last = {}
        for o in self.ops:
            if o.fn is None:
                continue
            last[(o.eng, o.tag)] = o
        deps = list(last.values())
        for e in self.ENGS:
            o = _Op()
            o.eng = e
            o.fn = None
            o.is_dma = False
            o.tag = None
            o.signal = False
            o.idx = len(self.ops)
            o.deps = list(deps)
            self.ops.append(o)
        self.last_w = {}
        self.readers = {}

    def dma(self, eng, out, in_, reads, writes, tag, **kw):
        return self.op(eng, lambda e: e.dma_start(out=out, in_=in_, **kw), reads, writes, dma_tag=tag)

    def _needs_wait(self, o, d):
        if d.is_dma:
            return True
        if d.eng == o.eng and not o.is_dma:
            if o.eng == "pe":
                return False
            return self.same_engine_sync
        return True

    def emit(self):
        nc = self.nc
        for o in self.ops:
            for d in o.deps:
                if self._needs_wait(o, d) and not d.is_dma:
                    d.signal = True
        cnt = {e: 0 for e in self.ENGS}
        for o in self.ops:
            if not o.is_dma and o.signal:
                cnt[o.eng] += 1
                o.val = cnt[o.eng]
        streams = {e: [] for e in self.ENGS}
        for o in self.ops:
            streams[o.eng].append(o)
        with ExitStack() as es:
            esem = {e: es.enter_context(nc.semaphore("s_" + e)) for e in self.ENGS}
            dsem = {t: es.enter_context(nc.semaphore("d_" + str(t))) for t in self.dma_count}
            block = es.enter_context(nc.Block())

            def run(ename, eng):
                known = {}
                for o in streams[ename]:
                    for d in o.deps:
                        if not self._needs_wait(o, d):
                            continue
                        if d.is_dma:
                            s, v, k = dsem[d.tag], d.val, ("d", d.tag)
                        else:
                            s, v, k = esem[d.eng], d.val, ("e", d.eng)
                        if known.get(k, 0) >= v:
                            continue
                        known[k] = v
                        eng.wait_ge(s, v)
                    if o.fn is None:
                        continue
                    ins = o.fn(eng)
                    if o.is_dma:
                        ins.then_inc(dsem[o.tag], 16)
                    elif o.signal:
                        ins.then_inc(esem[ename], 1)
                if ename == "sp":
                    for t, c in self.dma_count.items():
                        eng.wait_ge(dsem[t], 16 * c)

            @block.tensor
            def _(e):
                run("pe", e)

            @block.vector
            def _(e):
                run("dve", e)

            @block.scalar
            def _(e):
                run("act", e)

            @block.gpsimd
            def _(e):
                run("pool", e)

            @block.sync
            def _(e):
                run("sp", e)
        return cnt


SB_BYTES = 211968


class KB:
    def __init__(self, nc):
        self.nc = nc
        self.S = Sched(nc)
        self.SB = nc.alloc_sbuf_tensor("SB", [128, SB_BYTES // 4], F32).ap()
        self.PS = nc.alloc_psum_tensor("PS", [128, 4096], F32).ap()
        self.off = 0
        self.base = 0
        self.pso = 0
        self.uid = 0

    def persist(self):
        self.base = self.off

    def phase(self):
        self.S.fence()
        self.off = self.base
        self.pso = 0

    def sb(self, shape, dt, np_=128):
        n = int(np.prod(shape[1:]))
        bpe = 2 if dt == BF16 else 4
        nb = (n * bpe + 63) // 64 * 64
        assert self.off + nb <= SB_BYTES, ("SBUF overflow", self.off, nb)
        a = self.SB[0:shape[0], self.off // 4:(self.off + nb) // 4]
        self.off += nb
        if dt != F32:
            a = a.bitcast(dt)
        a = a[:, 0:n]
        if len(shape) == 3:
            a = a.rearrange("p (a b) -> p a b", a=shape[1])
        return a

    def ps(self, nbanks=1):
        assert self.pso + nbanks <= 8
        a = self.PS[:, self.pso * 512:(self.pso + nbanks) * 512]
        self.pso += nbanks
        return a

    def mm(self, out, lhsT, rhs, start, stop, r, w):
        self.S.op("pe", lambda e: e.matmul(out, lhsT=lhsT, rhs=rhs, start=start, stop=stop), r, w)

    def tr(self, out, in_, ident, r, w):
        self.S.op("pe", lambda e: e.transpose(out=out, in_=in_, identity=ident), r, w)

    def tt(self, eng, out, in0, in1, op, r, w):
        self.S.op(eng, lambda e: e.tensor_tensor(out=out, in0=in0, in1=in1, op=op), r, w)

    def ts(self, eng, out, in0, s1, s2, op0, op1, r, w):
        if op1 is None:
            self.S.op(eng, lambda e: e.tensor_scalar(out=out, in0=in0, scalar1=s1, scalar2=None, op0=op0), r, w)
        else:
            self.S.op(eng, lambda e: e.tensor_scalar(out=out, in0=in0, scalar1=s1, scalar2=s2, op0=op0, op1=op1), r, w)

    def stt(self, eng, out, in0, sc, in1, op0, op1, r, w):
        self.S.op(eng, lambda e: e.scalar_tensor_tensor(out=out, in0=in0, scalar=sc, in1=in1, op0=op0, op1=op1), r, w)

    def cp(self, eng, out, in_, r, w):
        if eng == "act":
            self.S.op(eng, lambda e: e.copy(out=out, in_=in_), r, w)
        else:
            self.S.op(eng, lambda e: e.tensor_copy(out=out, in_=in_), r, w)

    def act(self, out, in_, func, r, w, bias=None, scale=None, accum=None):
        kw = {}
        if bias is not None:
            kw["bias"] = bias
        if scale is not None:
            kw["scale"] = scale
        if accum is not None:
            kw["accum_out"] = accum
        self.S.op("act", lambda e: e.activation(out=out, in_=in_, func=func, **kw), r, w)

    def memset(self, eng, ap, v, w):
        self.S.op(eng, lambda e: e.memset(ap, v), [], w)

    def dma(self, eng, out, in_, r, w, tag, **kw):
        self.S.dma(eng, out, in_, r, w, tag, **kw)


def make_consts():
    j = np.arange(128)[:, None]
    i = np.arange(128)[None, :]
    c = -1.0 / 16.0
    mats = [
        np.eye(128),
        (j <= i) * c,
        (j >= i) * c,
        (j > i) * c,
        (j < i) * c,
        (j <= i) * 1.0,
        (j >= i) * 1.0,
        np.ones((128, 128)),
        (j < i) * 1.0,
    ]
    return np.concatenate(mats, axis=1).astype(np.float32)


NCONST = 9


def build_program(T, C, layer_list=(0, 1, 2, 3), do_mixer=True, do_moe=True):
    NT = T // 128
    NSLOT = NE * C
    nc = bass.Bass("TRN2", target_bir_lowering=False)
    dt_in = lambda name, shape, dt=F32: nc.dram_tensor(name, list(shape), dt, kind="ExternalInput").ap()
    dt_int = lambda name, shape, dt=F32: nc.dram_tensor(name, list(shape), dt, kind="Internal").ap()
    x_in = dt_in("x", [T, D])
    cT_in = dt_in("cT", [128, 8])
    ada_w = dt_in("ada_w", [4, D, 6 * D])
    ada_b = dt_in("ada_b", [4, 6 * D])
    ln_g = dt_in("ln_g", [4, 2, D])
    ln_b = dt_in("ln_b", [4, 2, D])
    gla_w_in = dt_in("gla_w_in", [2, D, GIN])
    gla_wg2_f = dt_in("gla_wg2_f", [2, 16, 512])
    gla_bg_f = dt_in("gla_bg_f", [2, 512])
    gla_wg2_b = dt_in("gla_wg2_b", [2, 16, 512])
    gla_bg_b = dt_in("gla_bg_b", [2, 512])
    gla_norm_g = dt_in("gla_norm_g", [2, 256])
    gla_w_out = dt_in("gla_w_out", [2, D, D])
    sgu_w_in = dt_in("sgu_w_in", [2, D, 2 * D])
    sgu_b_in = dt_in("sgu_b_in", [2, 2 * D])
    sgu_ln_g = dt_in("sgu_ln_g", [2, D])
    sgu_ln_b = dt_in("sgu_ln_b", [2, D])
    sgu_w_sT = dt_in("sgu_w_sT", [2, 128, 8, 128])
    sgu_b_sT = dt_in("sgu_b_sT", [2, 128, 8])
    sgu_w_out = dt_in("sgu_w_out", [2, D, D])
    sgu_b_out = dt_in("sgu_b_out", [2, D])
    moe_w_router = dt_in("moe_w_router", [4, D, NE])
    moe_b_router = dt_in("moe_b_router", [4, NE])
    moe_w_up = dt_in("moe_w_up", [4, NE, D, 2 * D])
    moe_b_upT = dt_in("moe_b_upT", [4, 128, NE, 16])
    moe_w_down = dt_in("moe_w_down", [4, NE, D, D])
    moe_b_down = dt_in("moe_b_down", [4, NE, D])
    consts_in = dt_in("consts", [128, NCONST * 128])
    dumpidx_in = dt_in("dumpidx", [128, 4])
    ecrow_in = dt_in("ecrow", [1, NE])
    out = nc.dram_tensor("out", [T, D], F32, kind="ExternalOutput").ap()

    xa = dt_int("xa", [T, D])
    xb_ = dt_int("xb", [T, D])
    modd = dt_int("modd", [4, 6 * D])
    sbn = dt_int("sbn", [NT, 128, 1024], BF16)
    Xs = dt_int("Xs", [NSLOT + NDUMP, D], BF16)
    Ws = dt_int("Ws", [NSLOT + NDUMP, 8])
    Ys = dt_int("Ys", [NSLOT + NDUMP, D])

    K = KB(nc)
    S = K.S
    dbg_outs = {}

    def dbg(name, ap, tok, dt=F32):
        if not DEBUG:
            return
        shape = [ap.shape[0], int(np.prod(ap.shape[1:]))]
        d_ = nc.dram_tensor("dbg_" + name, shape, dt, kind="ExternalOutput").ap()
        src = ap if len(ap.shape) == 2 else ap.rearrange("p a b -> p (a b)")
        K.dma("sp", d_, src, [tok], [], "dbg_" + name)
        dbg_outs[name] = d_

    c32 = K.sb([128, NCONST * 128], F32)
    cb = K.sb([128, NCONST * 128], BF16)
    ones_r32 = K.sb([1, 512], F32)
    ones_rb = K.sb([1, 512], BF16)
    nhalf = K.sb([128, 4], F32)
    K.dma("sp", c32, consts_in, [], ["c32"], "c32")
    K.cp("dve", cb, c32, ["c32"], ["cb"])
    K.memset("pool", ones_r32, 1.0, ["ones_r32"])
    K.cp("dve", ones_rb, ones_r32, ["ones_r32"], ["ones_rb"])
    K.memset("pool", nhalf, -0.5, ["nhalf"])
    cm = lambda t, i: t[:, i * 128:(i + 1) * 128]
    id32, idb = cm(c32, 0), cm(cb, 0)
    CT = ["c32", "cb", "ones_r32", "ones_rb", "nhalf"]
    K.persist()

    def phase_mod():
        K.phase()
        cT = K.sb([128, 8], F32)
        sc = K.sb([128, 8], F32)
        wt = [K.sb([128, 8, 512], F32) for _ in range(2)]
        ab = K.sb([1, 6 * D], F32)
        mrow = K.sb([1, 6 * D], F32)
        pm = K.ps(1)
        K.dma("sp", cT, cT_in, [], ["cT"], "cT")
        K.act(sc, cT, AF.Silu, ["cT"], ["sc"])
        n = 0
        for l in layer_list:
            K.dma("sp", ab, ada_b[l:l + 1, :], [], ["ab"], "ab")
            for j in range(12):
                b = n % 2
                n += 1
                K.dma("sp", wt[b], ada_w[l][:, j * 512:(j + 1) * 512].rearrange("(k p) n -> p k n", p=128), [], [("wt", b)], "wt%d" % b)
                for k in range(8):
                    K.mm(pm[0:1, :], sc[:, k:k + 1], wt[b][:, k, :], k == 0, k == 7, ["sc", ("wt", b)], ["pm"])
                plus = 1.0 if (j // 2) in (1, 2, 4, 5) else 0.0
                K.stt("dve", mrow[:, j * 512:(j + 1) * 512], pm[0:1, :], plus, ab[:, j * 512:(j + 1) * 512], ALU.add, ALU.add, ["pm", "ab"], ["mrow"])
            K.dma("sp", modd[l:l + 1, :], mrow, ["mrow"], ["modd"], "mrow")

    def load_bc(dst, row, tok):
        K.dma("sp", dst, row.partition_broadcast(128), ["modd"], [tok], "bc")

    def alloc_bc(sub, l, names=("scale", "shift", "gate", "lng", "lnb"), t=None):
        t = {} if t is None else t
        o = sub * 3 * D
        srcs = {"shift": modd[l:l + 1, o:o + D], "scale": modd[l:l + 1, o + D:o + 2 * D], "gate": modd[l:l + 1, o + 2 * D:o + 3 * D],
                "lng": ln_g[l, sub:sub + 1, :], "lnb": ln_b[l, sub:sub + 1, :]}
        for nm in names:
            t[nm] = K.sb([128, D], F32)
            load_bc(t[nm], srcs[nm], "bc_" + nm)
        return t

    def alloc_work(t):
        t["xt"] = [K.sb([128, D], F32) for _ in range(2)]
        t["h32"] = K.sb([128, D], F32)
        t["z"] = K.sb([128, D], F32)
        t["xo"] = [K.sb([128, D], F32) for _ in range(2)]
        t["st"] = K.sb([128, 2, 6], F32)
        t["mv"] = K.sb([128, 8], F32)
        return t

    def alloc_common(sub, l):
        return alloc_work(alloc_bc(sub, l))

    BCT = ["bc_shift", "bc_scale", "bc_gate", "bc_lng", "bc_lnb"]

    def front(cm_, xsrc, t, hb):
        b = t % 2
        K.dma("sp", cm_["xt"][b], xsrc[t * 128:(t + 1) * 128, :], ["xsrc"], [("xt", b)], "xt%d" % b)
        K.tt("dve", cm_["h32"], cm_["xt"][b], cm_["scale"], ALU.mult, [("xt", b), "bc_scale"], ["h32"])
        if hb is not None:
            K.tt("dve", hb, cm_["h32"], cm_["shift"], ALU.add, ["h32", "bc_shift"], ["hb"])
        else:
            K.tt("dve", cm_["h32"], cm_["h32"], cm_["shift"], ALU.add, ["h32", "bc_shift"], ["h32"])

    def back(cm_, xdst, t, y_ap, ytok):
        b = t % 2
        z, mv, st = cm_["z"], cm_["mv"], cm_["st"]
        K.tt("dve", z, y_ap, cm_["gate"], ALU.mult, [ytok, "bc_gate"], ["z"])
        K.stt("dve", z, cm_["xt"][b], ALPHA, z, ALU.mult, ALU.add, [("xt", b), "z"], ["z"])
        for c in range(2):
            S.op("dve", lambda e, c=c: e.bn_stats(out=st[:, c, :], in_=z[:, c * 512:(c + 1) * 512]), ["z"], ["st"])
        S.op("dve", lambda e: e.bn_aggr(out=mv[:, 0:2], in_=st.rearrange("p a b -> p (a b)")), ["st"], ["mv"])
        K.ts("dve", mv[:, 2:3], mv[:, 1:2], EPS, None, ALU.add, None, ["mv"], ["mv"])
        K.tt("pool", mv[:, 3:4], mv[:, 2:3], nhalf[:, 0:1], ALU.pow, ["mv", "nhalf"], ["mv"])
        K.stt("dve", mv[:, 4:5], mv[:, 0:1], -1.0, mv[:, 3:4], ALU.mult, ALU.mult, ["mv"], ["mv"])
        K.act(z, z, AF.Identity, ["z", "mv"], ["z"], bias=mv[:, 4:5], scale=mv[:, 3:4])
        K.tt("dve", z, z, cm_["lng"], ALU.mult, ["z", "bc_lng"], ["z"])
        K.tt("pool", cm_["xo"][b], z, cm_["lnb"], ALU.add, ["z", "bc_lnb"], [("xo", b)])
        K.dma("sp", xdst[t * 128:(t + 1) * 128, :], cm_["xo"][b], [("xo", b)], ["xdst"], "xo%d" % b)

    def transpose8(src_b, dstT, pT, srctok, dsttok, ident=None, eng="act", ptok="pT"):
        ident = idb if ident is None else ident
        for half in range(2):
            for j in range(4):
                k = half * 4 + j
                K.tr(pT[:, j, :], src_b[:, k * 128:(k + 1) * 128], ident, [srctok, "cb", "c32"], [ptok])
            K.cp(eng, dstT[:, half * 4:(half + 1) * 4, :], pT, [ptok], [dsttok])

    def load_w_cast(dst, src, tok, tag):
        N = src.shape[-1]
        c0 = 0
        while c0 < N:
            c1 = min(N, c0 + 2048)
            K.dma("pool", dst[:, :, c0:c1], src[:, c0:c1].rearrange("(k p) n -> p k n", p=128), [], [tok], tag)
            c0 = c1

    def load_row_bf16(dst_b, tmp32, src_row, tok):
        K.dma("sp", tmp32, src_row, [], [tok + "_32"], "rows")
        K.cp("dve", dst_b, tmp32, [tok + "_32"], [tok])

    def phase_sgu(l, xsrc, xdst):
        i = l // 2
        K.phase()
        cm_ = alloc_common(0, l)
        win = K.sb([128, 8, 2 * D], BF16)
        wout = K.sb([128, 8, D], BF16)
        wsT = K.sb([128, 8, 128], BF16)
        ws32 = K.sb([128, 8, 128], F32)
        bsT = K.sb([128, 8], F32)
        r32 = K.sb([1, 2 * D], F32)
        r32b = K.sb([1, D], F32)
        binr = K.sb([1, 2 * D], BF16)
        boutr = K.sb([1, D], BF16)
        slg = K.sb([128, D], F32)
        slb = K.sb([128, D], F32)
        hb = K.sb([128, D], BF16)
        hT = K.sb([128, 8, 128], BF16)
        u = [K.sb([128, D], F32) for _ in range(2)]
        v = K.sb([128, D], F32)
        vb = [K.sb([128, D], BF16) for _ in range(2)]
        gb = K.sb([128, D], BF16)
        gT = K.sb([128, 8, 128], BF16)
        st2 = K.sb([128, 2, 6], F32)
        mv2 = K.sb([128, 8], F32)
        pT = K.ps(1).bitcast(BF16)[:, 0:512].rearrange("p (a b) -> p a b", a=4)
        pT2 = K.ps(1).bitcast(BF16)[:, 0:512].rearrange("p (a b) -> p a b", a=4)
        pz = K.ps(2)
        psv = K.ps(2)
        py = K.ps(2)
        load_w_cast(win, sgu_w_in[i], "win", "wload")
        load_w_cast(wout, sgu_w_out[i], "wout", "wload")
        K.dma("sp", ws32, sgu_w_sT[i], [], ["ws32"], "rows")
        K.cp("dve", wsT, ws32, ["ws32"], ["wsT"])
        K.dma("sp", bsT, sgu_b_sT[i], [], ["bsT"], "rows")
        load_row_bf16(binr, r32, sgu_b_in[i:i + 1, :], "binr")
        load_row_bf16(boutr, r32b, sgu_b_out[i:i + 1, :], "boutr")
        K.dma("sp", slg, sgu_ln_g[i:i + 1, :].partition_broadcast(128), [], ["slg"], "bc")
        K.dma("sp", slb, sgu_ln_b[i:i + 1, :].partition_broadcast(128), [], ["slb"], "bc")

        def s1(t):
            p = t % 2
            front(cm_, xsrc, t, hb)
            transpose8(hb, hT, pT, "hb", "hT", ptok="pT")
            for part in range(2):
                dst = u[p] if part == 0 else v
                for c in range(2):
                    col = part * D + c * 512
                    for k in range(8):
                        K.mm(pz[:, c * 512:(c + 1) * 512], hT[:, k, :], win[:, k, col:col + 512], k == 0, False, ["hT", "win"], ["pz"])
                    K.mm(pz[:, c * 512:(c + 1) * 512], ones_rb[0:1, 0:128], binr[0:1, col:col + 512], False, True, ["ones_rb", "binr"], ["pz"])
                K.act(dst, pz, AF.Gelu, ["pz"], [("u", p) if part == 0 else "v"])
            for c in range(2):
                S.op("dve", lambda e, c=c: e.bn_stats(out=st2[:, c, :], in_=v[:, c * 512:(c + 1) * 512]), ["v"], ["st2"])
            S.op("dve", lambda e: e.bn_aggr(out=mv2[:, 0:2], in_=st2.rearrange("p a b -> p (a b)")), ["st2"], ["mv2"])
            K.ts("dve", mv2[:, 2:3], mv2[:, 1:2], EPS, None, ALU.add, None, ["mv2"], ["mv2"])
            K.tt("pool", mv2[:, 3:4], mv2[:, 2:3], nhalf[:, 0:1], ALU.pow, ["mv2", "nhalf"], ["mv2"])
            K.stt("dve", mv2[:, 4:5], mv2[:, 0:1], -1.0, mv2[:, 3:4], ALU.mult, ALU.mult, ["mv2"], ["mv2"])
            K.act(v, v, AF.Identity, ["v", "mv2"], ["v"], bias=mv2[:, 4:5], scale=mv2[:, 3:4])
            K.tt("dve", v, v, slg, ALU.mult, ["v", "slg"], ["v"])
            K.tt("pool", vb[p], v, slb, ALU.add, ["v", "slb"], [("vb", p)])

        def s2(t):
            p = t % 2
            for g in range(8):
                K.mm(psv[:, g * 128:(g + 1) * 128], wsT[:, g, :], vb[p][:, g * 128:(g + 1) * 128], True, True, ["wsT", ("vb", p)], ["psv"])
            for g in range(8):
                K.stt("dve", gb[:, g * 128:(g + 1) * 128], psv[:, g * 128:(g + 1) * 128], bsT[:, g:g + 1], u[p][:, g * 128:(g + 1) * 128],
                      ALU.add, ALU.mult, ["psv", "bsT", ("u", p)], ["gb"])
            transpose8(gb, gT, pT2, "gb", "gT", ptok="pT2")
            for c in range(2):
                for k in range(8):
                    K.mm(py[:, c * 512:(c + 1) * 512], gT[:, k, :], wout[:, k, c * 512:(c + 1) * 512], k == 0, False, ["gT", "wout"], ["py"])
                K.mm(py[:, c * 512:(c + 1) * 512], ones_rb[0:1, 0:128], boutr[0:1, c * 512:(c + 1) * 512], False, True, ["ones_rb", "boutr"], ["py"])
            back(cm_, xdst, t, py, "py")

        s1(0)
        for t in range(NT):
            if t + 1 < NT:
                s1(t + 1)
            s2(t)

    def phase_gla(l, xsrc, xdst):
        i = l // 2
        K.phase()
        cm_ = alloc_common(0, l)
        win = K.sb([128, 8, GIN], BF16)
        wout = K.sb([128, 8, D], BF16)
        wg2 = [K.sb([16, 512], F32) for _ in range(2)]
        bg = [K.sb([1, 512], F32) for _ in range(2)]
        ng4 = K.sb([128, D], F32)
        hb = K.sb([128, D], BF16)
        hT = K.sb([128, 8, 128], BF16)
        vb = [K.sb([128, D], BF16) for _ in range(2)]
        kt32 = K.sb([128, 512], F32)
        lr = K.sb([16, 256], F32)
        rs = [K.sb([128, D], F32) for _ in range(2)]
        ex = K.sb([128, 512], F32)
        spl = [K.sb([128, 512], F32) for _ in range(2)]
        Eq = [K.sb([128, 512], F32) for _ in range(2)]
        Ek = [K.sb([128, 512], F32) for _ in range(2)]
        Ed = K.sb([128, 512], F32)
        qT = [[K.sb([128, 512], BF16) for _ in range(2)] for _ in range(2)]
        kT = [K.sb([128, 512], BF16) for _ in range(2)]
        kh = [K.sb([128, 512], BF16) for _ in range(2)]
        dec = [K.sb([128, 4], F32) for _ in range(2)]
        S32 = K.sb([128, D], F32)
        Sb = K.sb([128, D], BF16)
        Sbn = K.sb([128, D], BF16)
        sm32 = K.sb([128, 512], F32)
        smb = [K.sb([128, 512], BF16) for _ in range(2)]
        Mf4 = K.sb([128, 512], F32)
        Mb4 = K.sb([128, 512], F32)
        ss = K.sb([128, 8], F32)
        junk = K.sb([128, 256], F32)
        gb = K.sb([128, D], BF16)
        gT = K.sb([128, 8, 128], BF16)
        pT = K.ps(1).bitcast(BF16)[:, 0:512].rearrange("p (a b) -> p a b", a=4)
        pA = K.ps(2)
        pB = K.ps(2)
        pQ = K.ps(1)
        pK = K.ps(1)
        pM = K.ps(1)

        load_w_cast(win, gla_w_in[i], "win", "wload")
        load_w_cast(wout, gla_w_out[i], "wout", "wload")
        K.dma("sp", wg2[0], gla_wg2_f[i], [], ["wg2"], "rows")
        K.dma("sp", wg2[1], gla_wg2_b[i], [], ["wg2"], "rows")
        K.dma("sp", bg[0], gla_bg_f[i:i + 1, :], [], ["wg2"], "rows")
        K.dma("sp", bg[1], gla_bg_b[i:i + 1, :], [], ["wg2"], "rows")
        for hh in range(4):
            K.dma("sp", ng4[:, hh * 256:(hh + 1) * 256], gla_norm_g[i:i + 1, :].partition_broadcast(128), [], ["ng4"], "bc")
            K.cp("dve", Mf4[:, hh * 128:(hh + 1) * 128], cm(c32, 5), ["c32"], ["Mf4"])
            K.cp("dve", Mb4[:, hh * 128:(hh + 1) * 128], cm(c32, 6), ["c32"], ["Mb4"])

        def softplus_neg(dirn):
            K.mm(pM, lr[0:16, dirn * 128:(dirn + 1) * 128], wg2[dirn], True, False, ["lr", "wg2"], ["pM"])
            K.mm(pM, ones_r32[0:1, 0:128], bg[dirn], False, True, ["ones_r32", "wg2"], ["pM"])
            K.act(ex, pM, AF.Exp, ["pM"], ["ex"], scale=-1.0)
            K.ts("dve", ex, ex, 1.0, None, ALU.add, None, ["ex"], ["ex"])
            K.act(spl[dirn], ex, AF.Ln, ["ex"], [("spl", dirn)])

        def proj_common(t, p):
            front(cm_, xsrc, t, hb)
            transpose8(hb, hT, pT, "hb", "hT")
            for c in range(2):
                for k in range(8):
                    K.mm(pA[:, c * 512:(c + 1) * 512], hT[:, k, :], win[:, k, 1024 + c * 512:1024 + (c + 1) * 512], k == 0, k == 7, ["hT", "win"], ["pA"])
            K.cp("act", vb[p], pA, ["pA"], [("vb", p)])
            for k in range(8):
                K.mm(pM, hT[:, k, :], win[:, k, 512:1024], k == 0, k == 7, ["hT", "win"], ["pM"])
            K.cp("dve", kt32, pM, ["pM"], ["kt32"])
            for dirn in range(2):
                for k in range(8):
                    K.mm(pM[0:16, dirn * 128:(dirn + 1) * 128], win[:, k, 3072 + dirn * 16:3072 + (dirn + 1) * 16], hT[:, k, :], k == 0, k == 7, ["hT", "win"], ["pM"])
            K.cp("dve", lr, pM[0:16, 0:256], ["pM"], ["lr"])

        def khat(dirn, Lmat, p):
            K.mm(pM, Lmat, spl[dirn], True, True, ["c32", ("spl", dirn)], ["pM"])
            K.act(Ed, pM, AF.Exp, ["pM"], ["Ed"])
            K.tt("dve", kh[p], kt32, Ed, ALU.mult, ["kt32", "Ed"], [("kh", p)])

        def state_update(p):
            for hh in range(4):
                K.mm(pB[:, hh * 256:(hh + 1) * 256], kh[p][:, hh * 128:(hh + 1) * 128], vb[p][:, hh * 256:(hh + 1) * 256], True, True, [("kh", p), ("vb", p)], ["pB"])
            for hh in range(4):
                K.stt("dve", S32[:, hh * 256:(hh + 1) * 256], S32[:, hh * 256:(hh + 1) * 256], dec[p][:, hh:hh + 1], pB[:, hh * 256:(hh + 1) * 256],
                      ALU.mult, ALU.add, ["S32", ("dec", p), "pB"], ["S32"])
            K.cp("act", Sb, S32, ["S32"], ["Sb"])

        def s1b(t, p):
            proj_common(t, p)
            softplus_neg(1)
            for hh in range(4):
                K.mm(pQ[:, hh:hh + 1], spl[1][:, hh * 128:(hh + 1) * 128], cm(c32, 1)[:, 127:128], True, True, [("spl", 1), "c32"], ["pQ"])
            K.act(dec[p], pQ[:, 0:4], AF.Exp, ["pQ"], [("dec", p)])
            khat(1, cm(c32, 4), p)

        def s2b(t, p):
            K.dma("sp", sbn[t], Sb, ["Sb"], [("sbn", t)], "sbn")
            state_update(p)

        K.memset("pool", S32, 0.0, ["S32"])
        K.memset("pool", Sb, 0.0, ["Sb"])
        order = list(range(NT - 1, -1, -1))
        if PIPE_B:
            s1b(order[0], 0)
            for n, t in enumerate(order):
                if n + 1 < NT:
                    s1b(order[n + 1], (n + 1) % 2)
                s2b(t, n % 2)
        else:
            for n, t in enumerate(order):
                s1b(t, n % 2)
                s2b(t, n % 2)

        def s1f(t, p):
            proj_common(t, p)
            for c in range(2):
                for k in range(8):
                    K.mm(pB[:, c * 512:(c + 1) * 512], hT[:, k, :], win[:, k, 2048 + c * 512:2048 + (c + 1) * 512], k == 0, k == 7, ["hT", "win"], ["pB"])
            K.act(rs[p], pB, AF.Silu, ["pB"], [("rs", p)])
            K.tt("pool", rs[p], rs[p], ng4, ALU.mult, [("rs", p), "ng4"], [("rs", p)])
            for hh in range(4):
                for k in range(8):
                    K.mm(pQ[:, hh * 128:(hh + 1) * 128], win[:, k, hh * 128:(hh + 1) * 128], hT[:, k, :], k == 0, k == 7, ["hT", "win"], ["pQ"])
            for hh in range(4):
                for k in range(8):
                    K.mm(pK[:, hh * 128:(hh + 1) * 128], win[:, k, 512 + hh * 128:512 + (hh + 1) * 128], hT[:, k, :], k == 0, k == 7, ["hT", "win"], ["pK"])
            softplus_neg(0)
            softplus_neg(1)
            for dirn in range(2):
                U = cm(c32, 1 + dirn)
                for hh in range(4):
                    K.mm(pM[:, hh * 128:(hh + 1) * 128], spl[dirn][:, hh * 128:(hh + 1) * 128], U, True, True, [("spl", dirn), "c32"], ["pM"])
                K.act(Eq[dirn], pM, AF.Exp, ["pM"], [("Eq", dirn)])
                K.act(Ek[dirn], pM, AF.Exp, ["pM"], [("Ek", dirn)], scale=-1.0)
                K.stt("dve", qT[p][dirn], pQ, 128.0 ** -0.5, Eq[dirn], ALU.mult, ALU.mult, ["pQ", ("Eq", dirn)], [("qT", p, dirn)])
                K.tt("dve", kT[dirn], pK, Ek[dirn], ALU.mult, ["pK", ("Ek", dirn)], [("kT", dirn)])
            for hh in range(4):
                K.mm(pM[:, hh * 128:(hh + 1) * 128], kT[0][:, hh * 128:(hh + 1) * 128], qT[p][0][:, hh * 128:(hh + 1) * 128], True, True, [("kT", 0), ("qT", p, 0)], ["pM"])
            K.tt("dve", sm32, pM, Mf4, ALU.mult, ["pM", "Mf4"], ["sm32"])
            for hh in range(4):
                K.mm(pM[:, hh * 128:(hh + 1) * 128], kT[1][:, hh * 128:(hh + 1) * 128], qT[p][1][:, hh * 128:(hh + 1) * 128], True, True, [("kT", 1), ("qT", p, 1)], ["pM"])
            K.tt("dve", ex, pM, Mb4, ALU.mult, ["pM", "Mb4"], ["ex"])
            K.tt("dve", smb[p], sm32, ex, ALU.add, ["sm32", "ex"], [("smb", p)])
            for hh in range(4):
                K.cp("dve", dec[p][:, hh:hh + 1], Eq[0][:, hh * 128 + 127:hh * 128 + 128], [("Eq", 0)], [("dec", p)])
            khat(0, cm(c32, 3), p)

        def s2f(t, p):
            K.dma("sp", Sbn, sbn[t], [("sbn", t)], ["Sbn"], "sbnl")
            for hh in range(4):
                o_ = pA[:, hh * 256:(hh + 1) * 256]
                K.mm(o_, smb[p][:, hh * 128:(hh + 1) * 128], vb[p][:, hh * 256:(hh + 1) * 256], True, False, [("smb", p), ("vb", p)], ["pA"])
                K.mm(o_, qT[p][0][:, hh * 128:(hh + 1) * 128], Sb[:, hh * 256:(hh + 1) * 256], False, False, [("qT", p, 0), "Sb"], ["pA"])
                K.mm(o_, qT[p][1][:, hh * 128:(hh + 1) * 128], Sbn[:, hh * 256:(hh + 1) * 256], False, True, [("qT", p, 1), "Sbn"], ["pA"])
            for hh in range(4):
                K.act(junk, pA[:, hh * 256:(hh + 1) * 256], AF.Square, ["pA"], ["junk", "ss"], accum=ss[:, hh:hh + 1])
            K.ts("dve", ss[:, 4:8], ss[:, 0:4], 1.0 / 256.0, EPS, ALU.mult, ALU.add, ["ss"], ["ss"])
            K.tt("pool", ss[:, 0:4], ss[:, 4:8], nhalf, ALU.pow, ["ss", "nhalf"], ["ss"])
            for hh in range(4):
                K.stt("dve", gb[:, hh * 256:(hh + 1) * 256], pA[:, hh * 256:(hh + 1) * 256], ss[:, hh:hh + 1], rs[p][:, hh * 256:(hh + 1) * 256],
                      ALU.mult, ALU.mult, ["pA", "ss", ("rs", p)], ["gb"])
            state_update(p)
            transpose8(gb, gT, pT, "gb", "gT")
            for c in range(2):
                for k in range(8):
                    K.mm(pB[:, c * 512:(c + 1) * 512], gT[:, k, :], wout[:, k, c * 512:(c + 1) * 512], k == 0, k == 7, ["gT", "wout"], ["pB"])
            back(cm_, xdst, t, pB, "pB")

        K.memset("pool", S32, 0.0, ["S32"])
        K.memset("pool", Sb, 0.0, ["Sb"])
        if PIPE_F:
            s1f(0, 0)
            for t in range(NT):
                if t + 1 < NT:
                    s1f(t + 1, (t + 1) % 2)
                s2f(t, t % 2)
        else:
            for t in range(NT):
                s1f(t, t % 2)
                s2f(t, t % 2)

    def phase_moe(l, xsrc, xdst):
        K.phase()
        cm_ = {}
        wr = K.sb([128, 8, NE], F32)
        br = K.sb([1, NE], F32)
        desti = K.sb([128, NT * 4], I32)
        dumpf = K.sb([128, 4], F32)
        ecb = K.sb([128, NE], F32)
        cnt = K.sb([128, NE], F32)
        K.dma("sp", wr, moe_w_router[l].rearrange("(k p) n -> p k n", p=128), [], ["wr"], "rows")
        K.dma("sp", br, moe_b_router[l:l + 1, :], [], ["wr"], "rows")
        K.dma("sp", dumpf, dumpidx_in, [], ["dumpf"], "rows")
        K.ts("dve", dumpf, dumpf, 1.0, None, ALU.add, None, ["dumpf"], ["dumpf"])
        K.dma("sp", ecb, ecrow_in.partition_broadcast(128), [], ["ecb"], "bc")
        K.memset("pool", cnt, 0.0, ["cnt"])
        mark = K.off
        alloc_bc(1, l, ("scale", "shift"), cm_)
        alloc_work(cm_)
        RT = []
        for p in range(2):
            r_ = {"p": p}
            r_["h32"] = K.sb([128, D], F32)
            r_["hb"] = K.sb([128, D], BF16)
            r_["hT32"] = K.sb([128, 8, 128], F32)
            for nm in ("lg", "rank", "over", "dfull", "tmp", "tmp2"):
                r_[nm] = K.sb([128, NE], F32)
            r_["oh"] = [K.sb([128, NE], F32) for _ in range(4)]
            r_["maskb"] = K.sb([128, NE], BF16)
            for nm in ("top8", "sm", "destf"):
                r_[nm] = K.sb([128, 8], F32)
            r_["w4"] = K.sb([128, 4], F32)
            r_["w4x"] = K.sb([128, 4, 8], F32)
            r_["pT32"] = K.ps(1).rearrange("p (a b) -> p a b", a=4)
            r_["pL"] = K.ps(1)
            r_["pR"] = K.ps(1)
            K.memset("pool", r_["w4x"], 0.0, [("w4x", p)])
            RT.append(r_)

        def route_tile(r_, t):
            p = r_["p"]
            T_ = lambda nm: (nm, p)
            lg, top8, sm, w4, w4x, oh, maskb = r_["lg"], r_["top8"], r_["sm"], r_["w4"], r_["w4x"], r_["oh"], r_["maskb"]
            rank, over, dfull, tmp, tmp2, destf, pL, pR = r_["rank"], r_["over"], r_["dfull"], r_["tmp"], r_["tmp2"], r_["destf"], r_["pL"], r_["pR"]
            hT32 = r_["hT32"]
            for half in range(2):
                for j in range(4):
                    k = half * 4 + j
                    K.tr(r_["pT32"][:, j, :], r_["h32"][:, k * 128:(k + 1) * 128], id32, [T_("h32"), "c32"], [T_("pT32")])
                K.cp("act", hT32[:, half * 4:(half + 1) * 4, :], r_["pT32"], [T_("pT32")], [T_("hT32")])
            for k in range(8):
                K.mm(pL[:, 0:NE], hT32[:, k, :], wr[:, k, :], k == 0, False, [T_("hT32"), "wr"], [T_("pL")])
            K.mm(pL[:, 0:NE], ones_r32[0:1, 0:128], br, False, True, ["ones_r32", "wr"], [T_("pL")])
            K.cp("dve", lg, pL[:, 0:NE], [T_("pL")], [T_("lg")])
            S.op("dve", lambda e: e.max(out=top8, in_=lg), [T_("lg")], [T_("top8")])
            K.ts("dve", sm[:, 0:1], top8[:, 0:1], -1.0, None, ALU.mult, None, [T_("top8")], [T_("sm")])
            K.act(w4, top8[:, 0:4], AF.Exp, [T_("top8"), T_("sm")], [T_("w4"), T_("sm")], bias=sm[:, 0:1], accum=sm[:, 1:2])
            S.op("dve", lambda e: e.reciprocal(out=sm[:, 2:3], in_=sm[:, 1:2]), [T_("sm")], [T_("sm")])
            K.ts("dve", w4x[:, :, 0:1], w4.rearrange("p (a b) -> p a b", b=1), sm[:, 2:3], None, ALU.mult, None, [T_("w4"), T_("sm")], [T_("w4x")])
            for k in range(4):
                K.ts("dve", oh[k], lg, top8[:, k:k + 1], None, ALU.is_equal, None, [T_("lg"), T_("top8")], [T_("oh%d" % k)])
            K.ts("dve", maskb, lg, top8[:, 3:4], None, ALU.is_ge, None, [T_("lg"), T_("top8")], [T_("maskb")])
            K.mm(pR[:, 0:NE], cm(cb, 8), maskb, True, True, ["cb", T_("maskb")], [T_("pR")])
            K.mm(pR[:, NE:2 * NE], cm(cb, 7), maskb, True, True, ["cb", T_("maskb")], [T_("pR")])
            K.tt("dve", rank, pR[:, 0:NE], cnt, ALU.add, [T_("pR"), "cnt"], [T_("rank")])
            K.tt("dve", cnt, pR[:, NE:2 * NE], cnt, ALU.add, [T_("pR"), "cnt"], ["cnt"])
            K.ts("dve", over, rank, float(C), -1.0, ALU.is_ge, ALU.add, [T_("rank")], [T_("over")])
            K.stt("dve", dfull, rank, 1.0, ecb, ALU.add, ALU.add, [T_("rank"), "ecb"], [T_("dfull")])
            K.stt("dve", dfull, dfull, -1.0, over, ALU.mult, ALU.mult, [T_("dfull"), T_("over")], [T_("dfull")])
            for k in range(4):
                K.tt("dve", tmp, oh[k], dfull, ALU.mult, [T_("oh%d" % k), T_("dfull")], [T_("tmp")])
                S.op("dve", lambda e, k=k: e.reduce_sum(out=destf[:, k:k + 1], in_=tmp, axis=AX.X), [T_("tmp")], [T_("destf")])
            K.ts("dve", destf[:, 4:8], destf[:, 0:4], 0.0, None, ALU.is_equal, None, [T_("destf")], [T_("destf")])
            K.tt("dve", destf[:, 4:8], destf[:, 4:8], dumpf, ALU.mult, [T_("destf"), "dumpf"], [T_("destf")])
            K.stt("dve", destf[:, 0:4], destf[:, 0:4], -1.0, destf[:, 4:8], ALU.add, ALU.add, [T_("destf")], [T_("destf")])
            K.cp("dve", desti[:, t * 4:(t + 1) * 4], destf[:, 0:4], [T_("destf")], [("desti", t)])
            for k in range(4):
                idx = desti[:, t * 4 + k:t * 4 + k + 1]
                S.op("pool", lambda e, idx=idx: e.indirect_dma_start(out=Xs, out_offset=bass.IndirectOffsetOnAxis(ap=idx, axis=0), in_=r_["hb"], in_offset=None),
                     [T_("hb"), ("desti", t)], ["Xs"], dma_tag="scatx%d_%d" % (k, p))
                S.op("pool", lambda e, idx=idx, k=k: e.indirect_dma_start(out=Ws, out_offset=bass.IndirectOffsetOnAxis(ap=idx, axis=0), in_=w4x[:, k, :], in_offset=None),
                     [T_("w4x"), ("desti", t)], ["Ws"], dma_tag="scatw%d_%d" % (k, p))

        for t in range(NT):
            r_ = RT[t % 2]
            p = t % 2
            b = t % 2
            K.dma("sp", cm_["xt"][b], xsrc[t * 128:(t + 1) * 128, :], ["xsrc"], [("xt", b)], "xt%d" % b)
            K.tt("dve", r_["h32"], cm_["xt"][b], cm_["scale"], ALU.mult, [("xt", b), "bc_scale"], [("h32", p)])
            K.tt("pool", r_["hb"], r_["h32"], cm_["shift"], ALU.add, [("h32", p), "bc_shift"], [("hb", p)])
            K.tt("dve", r_["h32"], r_["h32"], cm_["shift"], ALU.add, [("h32", p), "bc_shift"], [("h32", p)])
            route_tile(r_, t)
        dbg("cnt%d" % l, cnt, "cnt")
        S.fence()
        K.off = mark
        K.pso = 0
        GS = 512 if C % 512 == 0 else 384
        NG = C // GS
        NB = GS // 128
        assert NG * GS == C
        bup = K.sb([128, NE, 16], F32)
        K.dma("sp", bup, moe_b_upT[l], [], ["bup"], "rows")
        K.ts("dve", bup[:, :, 8:16], bup[:, :, 8:16], 1.0, None, ALU.add, None, ["bup"], ["bup"])
        wup = [K.sb([128, 8, 2 * D], BF16) for _ in range(2)]
        wdn = [K.sb([128, 8, D], BF16) for _ in range(2)]
        NST = 3
        stage = [K.sb([128, 2 * D], F32) for _ in range(NST)]
        bd32 = [K.sb([1, D], F32) for _ in range(2)]
        bdb = [K.sb([1, D], BF16) for _ in range(2)]
        xs = [K.sb([128, D], BF16) for _ in range(2)]
        wsl = [K.sb([128, 8], F32) for _ in range(16)]
        xsT = [K.sb([128, 8, GS], BF16) for _ in range(2)]
        actT = [K.sb([128, 8, GS], BF16) for _ in range(2)]
        gt = [K.sb([128, GS], F32) for _ in range(2)]
        sg = [K.sb([128, GS], F32) for _ in range(2)]
        lA = [K.sb([128, GS], F32) for _ in range(2)]
        ys = [K.sb([128, D], F32) for _ in range(2)]
        pT = K.ps(1).bitcast(BF16)[:, 0:512].rearrange("p (a b) -> p a b", a=4)
        pG = [K.ps(1) for _ in range(2)]
        pLn = [K.ps(1) for _ in range(2)]
        pY = K.ps(2)
        cnts = {"ns": 0, "nx": 0, "nw": 0, "ny": 0, "nj": 0}

        def weight_steps(e_):
            be = e_ % 2
            steps = []
            for k in range(8):
                def f(k=k):
                    b = cnts["ns"] % NST
                    cnts["ns"] += 1
                    K.dma("sp", stage[b], moe_w_up[l, e_, k * 128:(k + 1) * 128, :], [], [("stage", b)], "stage%d" % b)
                    st_v = stage[b].rearrange("p (f two) -> p two f", two=2)
                    K.cp("act", wup[be][:, k, :].rearrange("p (two f) -> p two f", two=2), st_v, [("stage", b)], [("wup", be)])
                steps.append(f)
            for k2 in range(4):
                def f(k2=k2):
                    b = cnts["ns"] % NST
                    cnts["ns"] += 1
                    K.dma("sp", stage[b].rearrange("p (a n) -> p a n", a=2),
                          moe_w_down[l, e_, k2 * 256:(k2 + 1) * 256, :].rearrange("(a p) n -> p a n", p=128), [], [("stage", b)], "stage%d" % b)
                    K.cp("dve", wdn[be][:, 2 * k2:2 * k2 + 2, :], stage[b].rearrange("p (a n) -> p a n", a=2), [("stage", b)], [("wdn", be)])
                steps.append(f)

            def f():
                K.dma("sp", bd32[be], moe_b_down[l, e_:e_ + 1, :], [], [("bd32", be)], "bd32_%d" % be)
                K.cp("dve", bdb[be], bd32[be], [("bd32", be)], [("bdb", be)])
            steps.append(f)
            return steps

        units = [(e_, g) for e_ in range(NE) for g in range(NG)]
        wslots = {}

        def unit_A(u):
            e_, g = units[u]
            gb_ = u % 2
            base = e_ * C + g * GS
            wl = []
            for blk in range(NB):
                b = cnts["nx"] % 2
                cnts["nx"] += 1
                wi = cnts["nw"] % 16
                cnts["nw"] += 1
                wl.append(wi)
                r0 = base + blk * 128
                K.dma("sp", xs[b], Xs[r0:r0 + 128, :], ["Xs"], [("xs", b)], "xs%d" % b)
                K.dma("sp", wsl[wi], Ws[r0:r0 + 128, :], ["Ws"], [("wsl", wi)], "wsl%d" % wi)
                for half in range(2):
                    for j in range(4):
                        k = half * 4 + j
                        K.tr(pT[:, j, :], xs[b][:, k * 128:(k + 1) * 128], idb, [("xs", b), "cb"], ["pT"])
                    K.cp("act" if half == 0 else "dve", xsT[gb_][:, half * 4:(half + 1) * 4, blk * 128:(blk + 1) * 128], pT, ["pT"], [("xsT", gb_)])
            wslots[u] = wl

        def unit_U(u, pref):
            e_, g = units[u]
            be = e_ % 2
            gb_ = u % 2
            for j in range(8):
                jb = cnts["nj"] % 2
                cnts["nj"] += 1
                for k in range(8):
                    K.mm(pG[jb][:, 0:GS], wup[be][:, k, j * 128:(j + 1) * 128], xsT[gb_][:, k, :], k == 0, k == 7, [("wup", be), ("xsT", gb_)], [("pG", jb)])
                for k in range(8):
                    K.mm(pLn[jb][:, 0:GS], wup[be][:, k, D + j * 128:D + (j + 1) * 128], xsT[gb_][:, k, :], k == 0, k == 7, [("wup", be), ("xsT", gb_)], [("pLn", jb)])
                K.ts("dve", gt[jb], pG[jb][:, 0:GS], bup[:, e_, j:j + 1], 7.0, ALU.add, ALU.min, [("pG", jb), "bup"], [("gt", jb)])
                K.act(sg[jb], gt[jb], AF.Sigmoid, [("gt", jb)], [("sg", jb)], scale=1.702)
                K.ts("dve", lA[jb], pLn[jb][:, 0:GS], bup[:, e_, 8 + j:9 + j], -6.0, ALU.add, ALU.max, [("pLn", jb), "bup"], [("lA", jb)])
                K.tt("dve", gt[jb], gt[jb], sg[jb], ALU.mult, [("gt", jb), ("sg", jb)], [("gt", jb)])
                K.stt("dve", actT[gb_][:, j, :], lA[jb], 8.0, gt[jb], ALU.min, ALU.mult, [("lA", jb), ("gt", jb)], [("actT", gb_)])
                if pref:
                    pref.pop(0)()
                    if len(pref) > 8 - j and j % 2 == 1:
                        pref.pop(0)()

        def unit_D(u):
            e_, g = units[u]
            be = e_ % 2
            gb_ = u % 2
            base = e_ * C + g * GS
            for blk in range(NB):
                wi = wslots[u][blk]
                for c in range(2):
                    for k in range(8):
                        K.mm(pY[:, c * 512:(c + 1) * 512], actT[gb_][:, k, blk * 128:(blk + 1) * 128], wdn[be][:, k, c * 512:(c + 1) * 512], k == 0, False, [("actT", gb_), ("wdn", be)], ["pY"])
                    K.mm(pY[:, c * 512:(c + 1) * 512], ones_rb[0:1, 0:128], bdb[be][0:1, c * 512:(c + 1) * 512], False, True, ["ones_rb", ("bdb", be)], ["pY"])
                b = cnts["ny"] % 2
                cnts["ny"] += 1
                K.act(ys[b], pY, AF.Copy, ["pY", ("wsl", wi)], [("ys", b)], scale=wsl[wi][:, 0:1])
                r0 = base + blk * 128
                K.dma("act", Ys[r0:r0 + 128, :], ys[b], [("ys", b)], ["Ys"], "ys%d" % b)

        for f in weight_steps(0):
            f()
        unit_A(0)
        pref = []
        for u in range(len(units)):
            e_, g = units[u]
            if g == 0:
                for f in pref:
                    f()
                pref = weight_steps(e_ + 1) if e_ + 1 < NE else []
            if u + 1 < len(units):
                unit_A(u + 1)
            unit_U(u, pref)
            unit_D(u)
        for f in pref:
            f()
        S.fence()
        K.off = mark
        K.pso = 0
        alloc_bc(1, l, ("gate", "lng", "lnb"), cm_)
        alloc_work(cm_)
        yk = [[K.sb([128, D], F32) for _ in range(4)] for _ in range(2)]
        for t in range(NT):
            b = t % 2
            y_ = yk[b]
            K.dma("sp", cm_["xt"][b], xsrc[t * 128:(t + 1) * 128, :], ["xsrc"], [("xt", b)], "xt%d" % b)
            for k in range(4):
                idx = desti[:, t * 4 + k:t * 4 + k + 1]
                S.op("pool", lambda e, idx=idx, k=k, y_=y_: e.indirect_dma_start(out=y_[k], out_offset=None, in_=Ys, in_offset=bass.IndirectOffsetOnAxis(ap=idx, axis=0)),
                     ["Ys", ("desti", t)], [("yk", b, k)], dma_tag="gath%d_%d" % (k, b))
            K.tt("dve", y_[0], y_[0], y_[1], ALU.add, [("yk", b, 0), ("yk", b, 1)], [("yk", b, 0)])
            K.tt("pool", y_[2], y_[2], y_[3], ALU.add, [("yk", b, 2), ("yk", b, 3)], [("yk", b, 2)])
            K.tt("dve", y_[0], y_[0], y_[2], ALU.add, [("yk", b, 0), ("yk", b, 2)], [("yk", b, 0)])
            back(cm_, xdst, t, y_[0], ("yk", b, 0))

    def phase_init():
        K.phase()
        zt = K.sb([128, 4096], F32)
        K.memset("pool", zt, 0.0, ["zt"])
        ztb = zt.bitcast(BF16)
        nrows = NSLOT + NDUMP
        assert nrows % 512 == 0 and (nrows // 128) * 8 <= 4096
        for r in range(0, nrows, 512):
            K.dma("sp", Xs[r:r + 512, :].rearrange("(p a) d -> p (a d)", p=128), ztb[:, 0:4 * D], ["zt"], ["Xs"], "zinit%d" % ((r // 512) % 4))
        K.dma("sp", Ws.rearrange("(p a) d -> p (a d)", p=128), zt[:, 0:(nrows // 128) * 8], ["zt"], ["Ws"], "zinit")
        K.dma("sp", Ys[NSLOT:NSLOT + NDUMP, :].rearrange("(p a) d -> p (a d)", p=128), zt[:, 0:4 * D], ["zt"], ["Ys"], "zinit")

    phase_mod()
    if do_moe:
        phase_init()
    cur = x_in
    bufs = [xa, xb_]
    nb = 0
    stages = []
    for l in layer_list:
        if do_mixer:
            stages.append(("mix", l))
        if do_moe:
            stages.append(("moe", l))
    for si, (kind, l) in enumerate(stages):
        dst = out if si == len(stages) - 1 else bufs[nb % 2]
        nb += 1
        if kind == "mix":
            if l % 2 == 0:
                phase_gla(l, cur, dst)
            else:
                phase_sgu(l, cur, dst)
        else:
            phase_moe(l, cur, dst)
        cur = dst
    cnt = S.emit()
    return nc, cnt


def prep_shared(inputs, C):
    f = lambda a: np.ascontiguousarray(np.asarray(a, dtype=np.float32))
    sh = {}
    for k_ in ("ada_w", "ada_b", "ln_g", "ln_b", "gla_w_in", "gla_wg2_f", "gla_bg_f", "gla_wg2_b", "gla_bg_b", "gla_norm_g", "gla_w_out",
               "sgu_w_in", "sgu_b_in", "sgu_ln_g", "sgu_ln_b", "sgu_w_out", "sgu_b_out", "moe_w_router", "moe_b_router", "moe_w_up",
               "moe_w_down", "moe_b_down"):
        sh[k_] = f(inputs[k_])
    sh["sgu_w_sT"] = f(np.transpose(np.asarray(inputs["sgu_w_s"]), (0, 3, 1, 2)))
    sh["sgu_b_sT"] = f(np.transpose(np.asarray(inputs["sgu_b_s"]), (0, 2, 1)))
    bu = np.asarray(inputs["moe_b_up"], dtype=np.float32)
    bg = bu[:, :, 0::2].reshape(4, NE, 8, 128)
    bl = bu[:, :, 1::2].reshape(4, NE, 8, 128)
    sh["moe_b_upT"] = f(np.transpose(np.concatenate([bg, bl], axis=2), (0, 3, 1, 2)))
    sh["consts"] = make_consts()
    sh["dumpidx"] = (NE * C + np.arange(4)[None, :] * 128 + np.arange(128)[:, None]).astype(np.float32)
    sh["ecrow"] = (np.arange(NE, dtype=np.float32) * C)[None, :]
    return sh


CAP = 1024
_cache = {}


def kernel(**inputs):
    x = np.asarray(inputs["x"], dtype=np.float32)
    c = np.asarray(inputs["c"], dtype=np.float32)
    B, T, _ = x.shape
    key = (T, CAP)
    if key not in _cache:
        _cache[key] = build_program(T, CAP)[0]
    nc = _cache[key]
    sh = prep_shared(inputs, CAP)
    in_maps = []
    for b in range(B):
        m = dict(sh)
        m["x"] = np.ascontiguousarray(x[b])
        m["cT"] = np.ascontiguousarray(c[b].reshape(8, 128).T)
        in_maps.append(m)
    res = run_bass_kernel_spmd(nc, in_maps, core_ids=list(range(B)))
    return np.stack([np.asarray(r["out"], dtype=np.float32) for r in res.results], axis=0)
```

```python
import numpy as np
from contextlib import ExitStack
import concourse.bass as bass
import concourse.mybir as mybir
from concourse.bass_utils import run_bass_kernel_spmd

F32 = mybir.dt.float32
BF16 = mybir.dt.bfloat16
I32 = mybir.dt.int32
AF = mybir.ActivationFunctionType
ALU = mybir.AluOpType
AX = mybir.AxisListType

D = 1024
DEPTH = 4
ALPHA = (2.0 * DEPTH) ** 0.25
EPS = 1e-5
NE = 32
GIN = 3104
NDUMP = 512
SIG7 = float(1.0 / (1.0 + np.exp(-1.702 * 7.0)))
DEBUG = False
PIPE_B = False
PIPE_F = True


class _Op:
    __slots__ = ("eng", "fn", "deps", "is_dma", "tag", "val", "signal", "idx")


class Sched:
    ENGS = ("pe", "dve", "act", "pool", "sp")

    def __init__(self, nc, same_engine_sync=True):
        self.nc = nc
        self.ops = []
        self.last_w = {}
        self.readers = {}
        self.same_engine_sync = same_engine_sync
        self.dma_count = {}
        self.last_dma = {}

    def op(self, eng, fn, reads=(), writes=(), dma_tag=None):
        o = _Op()
        o.eng = eng
        o.fn = fn
        o.is_dma = dma_tag is not None
        o.tag = dma_tag
        o.signal = False
        o.idx = len(self.ops)
        deps = {}
        for t in reads:
            w = self.last_w.get(t)
            if w is not None:
                deps[w.idx] = w
        for t in writes:
            w = self.last_w.get(t)
            if w is not None:
                deps[w.idx] = w
            for r in self.readers.get(t, ()):
                deps[r.idx] = r
        if o.is_dma:
            p = self.last_dma.get(dma_tag)
            if p is not None:
                deps[p.idx] = p
            self.last_dma[dma_tag] = o
        o.deps = list(deps.values())
        for t in writes:
            self.last_w[t] = o
            self.readers[t] = []
        for t in reads:
            lst = self.readers.setdefault(t, [])
            key = (o.eng, o.tag)
            lst[:] = [r for r in lst if (r.eng, r.tag) != key]
            lst.append(o)
        if o.is_dma:
            c = self.dma_count.get(dma_tag, 0) + 1
            self.dma_count[dma_tag] = c
            o.val = 16 * c
        self.ops.append(o)
        return o

    def fence(self):
        last = {}
        for o in self.ops:
            if o.fn is None:
                continue
            last[(o.eng, o.tag)] = o
        deps = list(last.values())
        for e in self.ENGS:
            o = _Op()
            o.eng = e
            o.fn = None
            o.is_dma = False
            o.tag = None
            o.signal = False
            o.idx = len(self.ops)
            o.deps = list(deps)
            self.ops.append(o)
        self.last_w = {}
        self.readers = {}

    def dma(self, eng, out, in_, reads, writes, tag, **kw):
        return self.op(eng, lambda e: e.dma_start(out=out, in_=in_, **kw), reads, writes, dma_tag=tag)

    def _needs_wait(self, o, d):
        if d.is_dma:
            return True
        if d.eng == o.eng and not o.is_dma:
            if o.eng == "pe":
                return False
            return self.same_engine_sync
        return True

    def emit(self):
        nc = self.nc
        for o in self.ops:
            for d in o.deps:
                if self._needs_wait(o, d) and not d.is_dma:
                    d.signal = True
        cnt = {e: 0 for e in self.ENGS}
        for o in self.ops:
            if not o.is_dma and o.signal:
                cnt[o.eng] += 1
                o.val = cnt[o.eng]
        streams = {e: [] for e in self.ENGS}
        for o in self.ops:
            streams[o.eng].append(o)
        with ExitStack() as es:
            esem = {e: es.enter_context(nc.semaphore("s_" + e)) for e in self.ENGS}
            dsem = {t: es.enter_context(nc.semaphore("d_" + str(t))) for t in self.dma_count}
            block = es.enter_context(nc.Block())

            def run(ename, eng):
                known = {}
                for o in streams[ename]:
                    for d in o.deps:
                        if not self._needs_wait(o, d):
                            continue
                        if d.is_dma:
                            s, v, k = dsem[d.tag], d.val, ("d", d.tag)
                        else:
                            s, v, k = esem[d.eng], d.val, ("e", d.eng)
                        if known.get(k, 0) >= v:
                            continue
                        known[k] = v
                        eng.wait_ge(s, v)
                    if o.fn is None:
                        continue
                    ins = o.fn(eng)
                    if o.is_dma:
                        ins.then_inc(dsem[o.tag], 16)
                    elif o.signal:
                        ins.then_inc(esem[ename], 1)
                if ename == "sp":
                    for t, c in self.dma_count.items():
                        eng.wait_ge(dsem[t], 16 * c)

            @block.tensor
            def _(e):
                run("pe", e)

            @block.vector
            def _(e):
                run("dve", e)

            @block.scalar
            def _(e):
                run("act", e)

            @block.gpsimd
            def _(e):
                run("pool", e)

            @block.sync
            def _(e):
                run("sp", e)
        return cnt


SB_BYTES = 211968


class KB:
    def __init__(self, nc):
        self.nc = nc
        self.S = Sched(nc)
        self.SB = nc.alloc_sbuf_tensor("SB", [128, SB_BYTES // 4], F32).ap()
        self.PS = nc.alloc_psum_tensor("PS", [128, 4096], F32).ap()
        self.off = 0
        self.base = 0
        self.pso = 0
        self.uid = 0

    def persist(self):
        self.base = self.off

    def phase(self):
        self.S.fence()
        self.off = self.base
        self.pso = 0

    def sb(self, shape, dt, np_=128):
        n = int(np.prod(shape[1:]))
        bpe = 2 if dt == BF16 else 4
        nb = (n * bpe + 63) // 64 * 64
        assert self.off + nb <= SB_BYTES, ("SBUF overflow", self.off, nb)
        a = self.SB[0:shape[0], self.off // 4:(self.off + nb) // 4]
        self.off += nb
        if dt != F32:
            a = a.bitcast(dt)
        a = a[:, 0:n]
        if len(shape) == 3:
            a = a.rearrange("p (a b) -> p a b", a=shape[1])
        return a

    def ps(self, nbanks=1):
        assert self.pso + nbanks <= 8
        a = self.PS[:, self.pso * 512:(self.pso + nbanks) * 512]
        self.pso += nbanks
        return a

    def mm(self, out, lhsT, rhs, start, stop, r, w):
        self.S.op("pe", lambda e: e.matmul(out, lhsT=lhsT, rhs=rhs, start=start, stop=stop), r, w)

    def tr(self, out, in_, ident, r, w):
        self.S.op("pe", lambda e: e.transpose(out=out, in_=in_, identity=ident), r, w)

    def tt(self, eng, out, in0, in1, op, r, w):
        self.S.op(eng, lambda e: e.tensor_tensor(out=out, in0=in0, in1=in1, op=op), r, w)

    def ts(self, eng, out, in0, s1, s2, op0, op1, r, w):
        if op1 is None:
            self.S.op(eng, lambda e: e.tensor_scalar(out=out, in0=in0, scalar1=s1, scalar2=None, op0=op0), r, w)
        else:
            self.S.op(eng, lambda e: e.tensor_scalar(out=out, in0=in0, scalar1=s1, scalar2=s2, op0=op0, op1=op1), r, w)

    def stt(self, eng, out, in0, sc, in1, op0, op1, r, w):
        self.S.op(eng, lambda e: e.scalar_tensor_tensor(out=out, in0=in0, scalar=sc, in1=in1, op0=op0, op1=op1), r, w)

    def cp(self, eng, out, in_, r, w):
        if eng == "act":
            self.S.op(eng, lambda e: e.copy(out=out, in_=in_), r, w)
        else:
            self.S.op(eng, lambda e: e.tensor_copy(out=out, in_=in_), r, w)

    def act(self, out, in_, func, r, w, bias=None, scale=None, accum=None):
        kw = {}
        if bias is not None:
            kw["bias"] = bias
        if scale is not None:
            kw["scale"] = scale
        if accum is not None:
            kw["accum_out"] = accum
        self.S.op("act", lambda e: e.activation(out=out, in_=in_, func=func, **kw), r, w)

    def memset(self, eng, ap, v, w):
        self.S.op(eng, lambda e: e.memset(ap, v), [], w)

    def dma(self, eng, out, in_, r, w, tag, **kw):
        self.S.dma(eng, out, in_, r, w, tag, **kw)


def make_consts():
    j = np.arange(128)[:, None]
    i = np.arange(128)[None, :]
    c = -1.0 / 16.0
    mats = [
        np.eye(128),
        (j <= i) * c,
        (j >= i) * c,
        (j > i) * c,
        (j < i) * c,
        (j <= i) * 1.0,
        (j >= i) * 1.0,
        np.ones((128, 128)),
        (j < i) * 1.0,
    ]
    return np.concatenate(mats, axis=1).astype(np.float32)


NCONST = 9


def build_program(T, C, layer_list=(0, 1, 2, 3), do_mixer=True, do_moe=True):
    NT = T // 128
    NSLOT = NE * C
    nc = bass.Bass("TRN2", target_bir_lowering=False)
    dt_in = lambda name, shape, dt=F32: nc.dram_tensor(name, list(shape), dt, kind="ExternalInput").ap()
    dt_int = lambda name, shape, dt=F32: nc.dram_tensor(name, list(shape), dt, kind="Internal").ap()
    x_in = dt_in("x", [T, D])
    cT_in = dt_in("cT", [128, 8])
    ada_w = dt_in("ada_w", [4, D, 6 * D])
    ada_b = dt_in("ada_b", [4, 6 * D])
    ln_g = dt_in("ln_g", [4, 2, D])
    ln_b = dt_in("ln_b", [4, 2, D])
    gla_w_in = dt_in("gla_w_in", [2, D, GIN])
    gla_wg2_f = dt_in("gla_wg2_f", [2, 16, 512])
    gla_bg_f = dt_in("gla_bg_f", [2, 512])
    gla_wg2_b = dt_in("gla_wg2_b", [2, 16, 512])
    gla_bg_b = dt_in("gla_bg_b", [2, 512])
    gla_norm_g = dt_in("gla_norm_g", [2, 256])
    gla_w_out = dt_in("gla_w_out", [2, D, D])
    sgu_w_in = dt_in("sgu_w_in", [2, D, 2 * D])
    sgu_b_in = dt_in("sgu_b_in", [2, 2 * D])
    sgu_ln_g = dt_in("sgu_ln_g", [2, D])
    sgu_ln_b = dt_in("sgu_ln_b", [2, D])
    sgu_w_sT = dt_in("sgu_w_sT", [2, 128, 8, 128])
    sgu_b_sT = dt_in("sgu_b_sT", [2, 128, 8])
    sgu_w_out = dt_in("sgu_w_out", [2, D, D])
    sgu_b_out = dt_in("sgu_b_out", [2, D])
    moe_w_router = dt_in("moe_w_router", [4, D, NE])
    moe_b_router = dt_in("moe_b_router", [4, NE])
    moe_w_up = dt_in("moe_w_up", [4, NE, D, 2 * D])
    moe_b_upT = dt_in("moe_b_upT", [4, 128, NE, 16])
    moe_w_down = dt_in("moe_w_down", [4, NE, D, D])
    moe_b_down = dt_in("moe_b_down", [4, NE, D])
    consts_in = dt_in("consts", [128, NCONST * 128])
    dumpidx_in = dt_in("dumpidx", [128, 4])
    ecrow_in = dt_in("ecrow", [1, NE])
    out = nc.dram_tensor("out", [T, D], F32, kind="ExternalOutput").ap()

    xa = dt_int("xa", [T, D])
    xb_ = dt_int("xb", [T, D])
    modd = dt_int("modd", [4, 6 * D])
    sbn = dt_int("sbn", [NT, 128, 1024], BF16)
    Xs = dt_int("Xs", [NSLOT + NDUMP, D], BF16)
    Ws = dt_int("Ws", [NSLOT + NDUMP, 8])
    Ys = dt_int("Ys", [NSLOT + NDUMP, D])

    K = KB(nc)
    S = K.S
    dbg_outs = {}

    def dbg(name, ap, tok, dt=F32):
        if not DEBUG:
            return
        shape = [ap.shape[0], int(np.prod(ap.shape[1:]))]
        d_ = nc.dram_tensor("dbg_" + name, shape, dt, kind="ExternalOutput").ap()
        src = ap if len(ap.shape) == 2 else ap.rearrange("p a b -> p (a b)")
        K.dma("sp", d_, src, [tok], [], "dbg_" + name)
        dbg_outs[name] = d_

    c32 = K.sb([128, NCONST * 128], F32)
    cb = K.sb([128, NCONST * 128], BF16)
    ones_r32 = K.sb([1, 512], F32)
    ones_rb = K.sb([1, 512], BF16)
    nhalf = K.sb([128, 4], F32)
    K.dma("sp", c32, consts_in, [], ["c32"], "c32")
    K.cp("dve", cb, c32, ["c32"], ["cb"])
    K.memset("pool", ones_r32, 1.0, ["ones_r32"])
    K.cp("dve", ones_rb, ones_r32, ["ones_r32"], ["ones_rb"])
    K.memset("pool", nhalf, -0.5, ["nhalf"])
    cm = lambda t, i: t[:, i * 128:(i + 1) * 128]
    id32, idb = cm(c32, 0), cm(cb, 0)
    CT = ["c32", "cb", "ones_r32", "ones_rb", "nhalf"]
    K.persist()

    def phase_mod():
        K.phase()
        cT = K.sb([128, 8], F32)
        sc = K.sb([128, 8], F32)
        wt = [K.sb([128, 8, 512], F32) for _ in range(2)]
        ab = K.sb([1, 6 * D], F32)
        mrow = K.sb([1, 6 * D], F32)
        pm = K.ps(1)
        K.dma("sp", cT, cT_in, [], ["cT"], "cT")
        K.act(sc, cT, AF.Silu, ["cT"], ["sc"])
        n = 0
        for l in layer_list:
            K.dma("sp", ab, ada_b[l:l + 1, :], [], ["ab"], "ab")
            for j in range(12):
                b = n % 2
                n += 1
                K.dma("sp", wt[b], ada_w[l][:, j * 512:(j + 1) * 512].rearrange("(k p) n -> p k n", p=128), [], [("wt", b)], "wt%d" % b)
                for k in range(8):
                    K.mm(pm[0:1, :], sc[:, k:k + 1], wt[b][:, k, :], k == 0, k == 7, ["sc", ("wt", b)], ["pm"])
                plus = 1.0 if (j // 2) in (1, 2, 4, 5) else 0.0
                K.stt("dve", mrow[:, j * 512:(j + 1) * 512], pm[0:1, :], plus, ab[:, j * 512:(j + 1) * 512], ALU.add, ALU.add, ["pm", "ab"], ["mrow"])
            K.dma("sp", modd[l:l + 1, :], mrow, ["mrow"], ["modd"], "mrow")

    def load_bc(dst, row, tok):
        K.dma("sp", dst, row.partition_broadcast(128), ["modd"], [tok], "bc")

    def alloc_bc(sub, l, names=("scale", "shift", "gate", "lng", "lnb"), t=None):
        t = {} if t is None else t
        o = sub * 3 * D
        srcs = {"shift": modd[l:l + 1, o:o + D], "scale": modd[l:l + 1, o + D:o + 2 * D], "gate": modd[l:l + 1, o + 2 * D:o + 3 * D],
                "lng": ln_g[l, sub:sub + 1, :], "lnb": ln_b[l, sub:sub + 1, :]}
        for nm in names:
            t[nm] = K.sb([128, D], F32)
            load_bc(t[nm], srcs[nm], "bc_" + nm)
        return t

    def alloc_work(t):
        t["xt"] = [K.sb([128, D], F32) for _ in range(3)]
        t["h32"] = K.sb([128, D], F32)
        t["z"] = K.sb([128, D], F32)
        t["xo"] = [K.sb([128, D], F32) for _ in range(2)]
        t["st"] = K.sb([128, 2, 6], F32)
        t["mv"] = K.sb([128, 8], F32)
        return t

    def alloc_common(sub, l):
        return alloc_work(alloc_bc(sub, l))

    BCT = ["bc_shift", "bc_scale", "bc_gate", "bc_lng", "bc_lnb"]

    def front(cm_, xsrc, t, hb):
        b = t % 3
        K.dma("sp", cm_["xt"][b], xsrc[t * 128:(t + 1) * 128, :], ["xsrc"], [("xt", b)], "xt%d" % b)
        K.tt("dve", cm_["h32"], cm_["xt"][b], cm_["scale"], ALU.mult, [("xt", b), "bc_scale"], ["h32"])
        if hb is not None:
            K.tt("dve", hb, cm_["h32"], cm_["shift"], ALU.add, ["h32", "bc_shift"], ["hb"])
        else:
            K.tt("dve", cm_["h32"], cm_["h32"], cm_["shift"], ALU.add, ["h32", "bc_shift"], ["h32"])

    def back(cm_, xdst, t, y_ap, ytok):
        b = t % 2
        bx = t % 3
        z, mv, st = cm_["z"], cm_["mv"], cm_["st"]
        K.tt("dve", z, y_ap, cm_["gate"], ALU.mult, [ytok, "bc_gate"], ["z"])
        K.stt("dve", z, cm_["xt"][bx], ALPHA, z, ALU.mult, ALU.add, [("xt", bx), "z"], ["z"])
        for c in range(2):
            S.op("dve", lambda e, c=c: e.bn_stats(out=st[:, c, :], in_=z[:, c * 512:(c + 1) * 512]), ["z"], ["st"])
        S.op("dve", lambda e: e.bn_aggr(out=mv[:, 0:2], in_=st.rearrange("p a b -> p (a b)")), ["st"], ["mv"])
        K.ts("dve", mv[:, 2:3], mv[:, 1:2], EPS, None, ALU.add, None, ["mv"], ["mv"])
        K.tt("pool", mv[:, 3:4], mv[:, 2:3], nhalf[:, 0:1], ALU.pow, ["mv", "nhalf"], ["mv"])
        K.stt("dve", mv[:, 4:5], mv[:, 0:1], -1.0, mv[:, 3:4], ALU.mult, ALU.mult, ["mv"], ["mv"])
        K.act(z, z, AF.Identity, ["z", "mv"], ["z"], bias=mv[:, 4:5], scale=mv[:, 3:4])
        K.tt("dve", z, z, cm_["lng"], ALU.mult, ["z", "bc_lng"], ["z"])
        K.tt("pool", cm_["xo"][b], z, cm_["lnb"], ALU.add, ["z", "bc_lnb"], [("xo", b)])
        K.dma("sp", xdst[t * 128:(t + 1) * 128, :], cm_["xo"][b], [("xo", b)], [], "xo%d" % b)

    def transpose8(src_b, dstT, pT, srctok, dsttok, ident=None, eng="act", ptok="pT"):
        ident = idb if ident is None else ident
        for half in range(2):
            for j in range(4):
                k = half * 4 + j
                K.tr(pT[:, j, :], src_b[:, k * 128:(k + 1) * 128], ident, [srctok, "cb", "c32"], [ptok])
            K.cp(eng, dstT[:, half * 4:(half + 1) * 4, :], pT, [ptok], [dsttok])

    def load_w_cast(dst, src, tok, tag):
        N = src.shape[-1]
        c0 = 0
        while c0 < N:
            c1 = min(N, c0 + 2048)
            K.dma("pool", dst[:, :, c0:c1], src[:, c0:c1].rearrange("(k p) n -> p k n", p=128), [], [tok], tag)
            c0 = c1

    def load_row_bf16(dst_b, tmp32, src_row, tok):
        K.dma("sp", tmp32, src_row, [], [tok + "_32"], "rows")
        K.cp("dve", dst_b, tmp32, [tok + "_32"], [tok])

    def phase_sgu(l, xsrc, xdst):
        i = l // 2
        K.phase()
        cm_ = alloc_common(0, l)
        win = K.sb([128, 8, 2 * D], BF16)
        wout = K.sb([128, 8, D], BF16)
        wsT = K.sb([128, 8, 128], BF16)
        ws32 = K.sb([128, 8, 128], F32)
        bsT = K.sb([128, 8], F32)
        r32 = K.sb([1, 2 * D], F32)
        r32b = K.sb([1, D], F32)
        binr = K.sb([1, 2 * D], BF16)
        boutr = K.sb([1, D], BF16)
        slg = K.sb([128, D], F32)
        slb = K.sb([128, D], F32)
        hb = K.sb([128, D], BF16)
        hT = K.sb([128, 8, 128], BF16)
        u = [K.sb([128, D], F32) for _ in range(2)]
        v = K.sb([128, D], F32)
        vb = [K.sb([128, D], BF16) for _ in range(2)]
        gb = K.sb([128, D], BF16)
        gT = K.sb([128, 8, 128], BF16)
        st2 = K.sb([128, 2, 6], F32)
        mv2 = K.sb([128, 8], F32)
        pT = K.ps(1).bitcast(BF16)[:, 0:512].rearrange("p (a b) -> p a b", a=4)
        pT2 = K.ps(1).bitcast(BF16)[:, 0:512].rearrange("p (a b) -> p a b", a=4)
        pz = K.ps(2)
        psv = K.ps(2)
        py = K.ps(2)
        load_w_cast(win, sgu_w_in[i], "win", "wload")
        load_w_cast(wout, sgu_w_out[i], "wout", "wload")
        K.dma("sp", ws32, sgu_w_sT[i], [], ["ws32"], "rows")
        K.cp("dve", wsT, ws32, ["ws32"], ["wsT"])
        K.dma("sp", bsT, sgu_b_sT[i], [], ["bsT"], "rows")
        load_row_bf16(binr, r32, sgu_b_in[i:i + 1, :], "binr")
        load_row_bf16(boutr, r32b, sgu_b_out[i:i + 1, :], "boutr")
        K.dma("sp", slg, sgu_ln_g[i:i + 1, :].partition_broadcast(128), [], ["slg"], "bc")
        K.dma("sp", slb, sgu_ln_b[i:i + 1, :].partition_broadcast(128), [], ["slb"], "bc")

        def s1(t):
            p = t % 2
            front(cm_, xsrc, t, hb)
            transpose8(hb, hT, pT, "hb", "hT", ptok="pT")
            for part in range(2):
                dst = u[p] if part == 0 else v
                for c in range(2):
                    col = part * D + c * 512
                    for k in range(8):
                        K.mm(pz[:, c * 512:(c + 1) * 512], hT[:, k, :], win[:, k, col:col + 512], k == 0, False, ["hT", "win"], ["pz"])
                    K.mm(pz[:, c * 512:(c + 1) * 512], ones_rb[0:1, 0:128], binr[0:1, col:col + 512], False, True, ["ones_rb", "binr"], ["pz"])
                K.act(dst, pz, AF.Gelu, ["pz"], [("u", p) if part == 0 else "v"])
            for c in range(2):
                S.op("dve", lambda e, c=c: e.bn_stats(out=st2[:, c, :], in_=v[:, c * 512:(c + 1) * 512]), ["v"], ["st2"])
            S.op("dve", lambda e: e.bn_aggr(out=mv2[:, 0:2], in_=st2.rearrange("p a b -> p (a b)")), ["st2"], ["mv2"])
            K.ts("dve", mv2[:, 2:3], mv2[:, 1:2], EPS, None, ALU.add, None, ["mv2"], ["mv2"])
            K.tt("pool", mv2[:, 3:4], mv2[:, 2:3], nhalf[:, 0:1], ALU.pow, ["mv2", "nhalf"], ["mv2"])
            K.stt("dve", mv2[:, 4:5], mv2[:, 0:1], -1.0, mv2[:, 3:4], ALU.mult, ALU.mult, ["mv2"], ["mv2"])
            K.act(v, v, AF.Identity, ["v", "mv2"], ["v"], bias=mv2[:, 4:5], scale=mv2[:, 3:4])
            K.tt("dve", v, v, slg, ALU.mult, ["v", "slg"], ["v"])
            K.tt("pool", vb[p], v, slb, ALU.add, ["v", "slb"], [("vb", p)])

        def s2(t):
            p = t % 2
            for g in range(8):
                K.mm(psv[:, g * 128:(g + 1) * 128], wsT[:, g, :], vb[p][:, g * 128:(g + 1) * 128], True, True, ["wsT", ("vb", p)], ["psv"])
            for g in range(8):
                K.stt("dve", gb[:, g * 128:(g + 1) * 128], psv[:, g * 128:(g + 1) * 128], bsT[:, g:g + 1], u[p][:, g * 128:(g + 1) * 128],
                      ALU.add, ALU.mult, ["psv", "bsT", ("u", p)], ["gb"])
            transpose8(gb, gT, pT2, "gb", "gT", ptok="pT2")
            for c in range(2):
                for k in range(8):
                    K.mm(py[:, c * 512:(c + 1) * 512], gT[:, k, :], wout[:, k, c * 512:(c + 1) * 512], k == 0, False, ["gT", "wout"], ["py"])
                K.mm(py[:, c * 512:(c + 1) * 512], ones_rb[0:1, 0:128], boutr[0:1, c * 512:(c + 1) * 512], False, True, ["ones_rb", "boutr"], ["py"])
            back(cm_, xdst, t, py, "py")

        s1(0)
        for t in range(NT):
            if t + 1 < NT:
                s1(t + 1)
            s2(t)

    def phase_gla(l, xsrc, xdst):
        i = l // 2
        K.phase()
        cm_ = alloc_common(0, l)
        win = K.sb([128, 8, GIN], BF16)
        wout = K.sb([128, 8, D], BF16)
        wg2 = [K.sb([16, 512], F32) for _ in range(2)]
        bg = [K.sb([1, 512], F32) for _ in range(2)]
        ng4 = K.sb([128, D], F32)
        hb = K.sb([128, D], BF16)
        hT = K.sb([128, 8, 128], BF16)
        vb = [K.sb([128, D], BF16) for _ in range(2)]
        kt32 = K.sb([128, 512], F32)
        lr = K.sb([16, 256], F32)
        rs = [K.sb([128, D], F32) for _ in range(2)]
        ex = K.sb([128, 512], F32)
        spl = [K.sb([128, 512], F32) for _ in range(2)]
        Eq = [K.sb([128, 512], F32) for _ in range(2)]
        Ek = [K.sb([128, 512], F32) for _ in range(2)]
        Ed = K.sb([128, 512], F32)
        qT = [[K.sb([128, 512], BF16) for _ in range(2)] for _ in range(2)]
        kT = [K.sb([128, 512], BF16) for _ in range(2)]
        kh = [K.sb([128, 512], BF16) for _ in range(2)]
        dec = [K.sb([128, 4], F32) for _ in range(2)]
        S32 = K.sb([128, D], F32)
        Sb = K.sb([128, D], BF16)
        Sbn = K.sb([128, D], BF16)
        sm32 = K.sb([128, 512], F32)
        smb = [K.sb([128, 512], BF16) for _ in range(2)]
        Mf4 = K.sb([128, 512], F32)
        Mb4 = K.sb([128, 512], F32)
        ss = K.sb([128, 8], F32)
        junk = K.sb([128, 256], F32)
        gb = K.sb([128, D], BF16)
        gT = K.sb([128, 8, 128], BF16)
        pT = K.ps(1).bitcast(BF16)[:, 0:512].rearrange("p (a b) -> p a b", a=4)
        pA = K.ps(2)
        pB = K.ps(2)
        pQ = K.ps(1)
        pK = K.ps(1)
        pM = K.ps(1)

        load_w_cast(win, gla_w_in[i], "win", "wload")
        load_w_cast(wout, gla_w_out[i], "wout", "wload")
        K.dma("sp", wg2[0], gla_wg2_f[i], [], ["wg2"], "rows")
        K.dma("sp", wg2[1], gla_wg2_b[i], [], ["wg2"], "rows")
        K.dma("sp", bg[0], gla_bg_f[i:i + 1, :], [], ["wg2"], "rows")
        K.dma("sp", bg[1], gla_bg_b[i:i + 1, :], [], ["wg2"], "rows")
        for hh in range(4):
            K.dma("sp", ng4[:, hh * 256:(hh + 1) * 256], gla_norm_g[i:i + 1, :].partition_broadcast(128), [], ["ng4"], "bc")
            K.cp("dve", Mf4[:, hh * 128:(hh + 1) * 128], cm(c32, 5), ["c32"], ["Mf4"])
            K.cp("dve", Mb4[:, hh * 128:(hh + 1) * 128], cm(c32, 6), ["c32"], ["Mb4"])

        def softplus_neg(dirn):
            K.mm(pM, lr[0:16, dirn * 128:(dirn + 1) * 128], wg2[dirn], True, False, ["lr", "wg2"], ["pM"])
            K.mm(pM, ones_r32[0:1, 0:128], bg[dirn], False, True, ["ones_r32", "wg2"], ["pM"])
            K.act(ex, pM, AF.Exp, ["pM"], ["ex"], scale=-1.0)
            K.ts("dve", ex, ex, 1.0, None, ALU.add, None, ["ex"], ["ex"])
            K.act(spl[dirn], ex, AF.Ln, ["ex"], [("spl", dirn)])

        def proj_common(t, p):
            front(cm_, xsrc, t, hb)
            transpose8(hb, hT, pT, "hb", "hT")
            for c in range(2):
                for k in range(8):
                    K.mm(pA[:, c * 512:(c + 1) * 512], hT[:, k, :], win[:, k, 1024 + c * 512:1024 + (c + 1) * 512], k == 0, k == 7, ["hT", "win"], ["pA"])
            K.cp("act", vb[p], pA, ["pA"], [("vb", p)])
            for k in range(8):
                K.mm(pM, hT[:, k, :], win[:, k, 512:1024], k == 0, k == 7, ["hT", "win"], ["pM"])
            K.cp("dve", kt32, pM, ["pM"], ["kt32"])
            for dirn in range(2):
                for k in range(8):
                    K.mm(pM[0:16, dirn * 128:(dirn + 1) * 128], win[:, k, 3072 + dirn * 16:3072 + (dirn + 1) * 16], hT[:, k, :], k == 0, k == 7, ["hT", "win"], ["pM"])
            K.cp("dve", lr, pM[0:16, 0:256], ["pM"], ["lr"])

        def khat(dirn, Lmat, p):
            K.mm(pM, Lmat, spl[dirn], True, True, ["c32", ("spl", dirn)], ["pM"])
            K.act(Ed, pM, AF.Exp, ["pM"], ["Ed"])
            K.tt("dve", kh[p], kt32, Ed, ALU.mult, ["kt32", "Ed"], [("kh", p)])

        def state_update(p):
            for hh in range(4):
                K.mm(pB[:, hh * 256:(hh + 1) * 256], kh[p][:, hh * 128:(hh + 1) * 128], vb[p][:, hh * 256:(hh + 1) * 256], True, True, [("kh", p), ("vb", p)], ["pB"])
            for hh in range(4):
                K.stt("dve", S32[:, hh * 256:(hh + 1) * 256], S32[:, hh * 256:(hh + 1) * 256], dec[p][:, hh:hh + 1], pB[:, hh * 256:(hh + 1) * 256],
                      ALU.mult, ALU.add, ["S32", ("dec", p), "pB"], ["S32"])
            K.cp("act", Sb, S32, ["S32"], ["Sb"])

        def s1b(t, p):
            proj_common(t, p)
            softplus_neg(1)
            for hh in range(4):
                K.mm(pQ[:, hh:hh + 1], spl[1][:, hh * 128:(hh + 1) * 128], cm(c32, 1)[:, 127:128], True, True, [("spl", 1), "c32"], ["pQ"])
            K.act(dec[p], pQ[:, 0:4], AF.Exp, ["pQ"], [("dec", p)])
            khat(1, cm(c32, 4), p)

        def s2b(t, p):
            K.dma("sp", sbn[t], Sb, ["Sb"], [("sbn", t)], "sbn")
            state_update(p)

        K.memset("pool", S32, 0.0, ["S32"])
        K.memset("pool", Sb, 0.0, ["Sb"])
        order = list(range(NT - 1, -1, -1))
        if PIPE_B:
            s1b(order[0], 0)
            for n, t in enumerate(order):
                if n + 1 < NT:
                    s1b(order[n + 1], (n + 1) % 2)
                s2b(t, n % 2)
        else:
            for n, t in enumerate(order):
                s1b(t, n % 2)
                s2b(t, n % 2)

        def s1f(t, p):
            proj_common(t, p)
            for c in range(2):
                for k in range(8):
                    K.mm(pB[:, c * 512:(c + 1) * 512], hT[:, k, :], win[:, k, 2048 + c * 512:2048 + (c + 1) * 512], k == 0, k == 7, ["hT", "win"], ["pB"])
            K.act(rs[p], pB, AF.Silu, ["pB"], [("rs", p)])
            K.tt("pool", rs[p], rs[p], ng4, ALU.mult, [("rs", p), "ng4"], [("rs", p)])
            for hh in range(4):
                for k in range(8):
                    K.mm(pQ[:, hh * 128:(hh + 1) * 128], win[:, k, hh * 128:(hh + 1) * 128], hT[:, k, :], k == 0, k == 7, ["hT", "win"], ["pQ"])
            for hh in range(4):
                for k in range(8):
                    K.mm(pK[:, hh * 128:(hh + 1) * 128], win[:, k, 512 + hh * 128:512 + (hh + 1) * 128], hT[:, k, :], k == 0, k == 7, ["hT", "win"], ["pK"])
            softplus_neg(0)
            softplus_neg(1)
            for dirn in range(2):
                U = cm(c32, 1 + dirn)
                for hh in range(4):
                    K.mm(pM[:, hh * 128:(hh + 1) * 128], spl[dirn][:, hh * 128:(hh + 1) * 128], U, True, True, [("spl", dirn), "c32"], ["pM"])
                K.act(Eq[dirn], pM, AF.Exp, ["pM"], [("Eq", dirn)])
                K.act(Ek[dirn], pM, AF.Exp, ["pM"], [("Ek", dirn)], scale=-1.0)
                K.stt("dve", qT[p][dirn], pQ, 128.0 ** -0.5, Eq[dirn], ALU.mult, ALU.mult, ["pQ", ("Eq", dirn)], [("qT", p, dirn)])
                K.tt("dve", kT[dirn], pK, Ek[dirn], ALU.mult, ["pK", ("Ek", dirn)], [("kT", dirn)])
            for hh in range(4):
                K.mm(pM[:, hh * 128:(hh + 1) * 128], kT[0][:, hh * 128:(hh + 1) * 128], qT[p][0][:, hh * 128:(hh + 1) * 128], True, True, [("kT", 0), ("qT", p, 0)], ["pM"])
            K.tt("dve", sm32, pM, Mf4, ALU.mult, ["pM", "Mf4"], ["sm32"])
            for hh in range(4):
                K.mm(pM[:, hh * 128:(hh + 1) * 128], kT[1][:, hh * 128:(hh + 1) * 128], qT[p][1][:, hh * 128:(hh + 1) * 128], True, True, [("kT", 1), ("qT", p, 1)], ["pM"])
            K.tt("dve", ex, pM, Mb4, ALU.mult, ["pM", "Mb4"], ["ex"])
            K.tt("dve", smb[p], sm32, ex, ALU.add, ["sm32", "ex"], [("smb", p)])
            for hh in range(4):
                K.cp("dve", dec[p][:, hh:hh + 1], Eq[0][:, hh * 128 + 127:hh * 128 + 128], [("Eq", 0)], [("dec", p)])
            khat(0, cm(c32, 3), p)

        def s2f(t, p):
            K.dma("sp", Sbn, sbn[t], [("sbn", t)], ["Sbn"], "sbnl")
            for hh in range(4):
                o_ = pA[:, hh * 256:(hh + 1) * 256]
                K.mm(o_, smb[p][:, hh * 128:(hh + 1) * 128], vb[p][:, hh * 256:(hh + 1) * 256], True, False, [("smb", p), ("vb", p)], ["pA"])
                K.mm(o_, qT[p][0][:, hh * 128:(hh + 1) * 128], Sb[:, hh * 256:(hh + 1) * 256], False, False, [("qT", p, 0), "Sb"], ["pA"])
                K.mm(o_, qT[p][1][:, hh * 128:(hh + 1) * 128], Sbn[:, hh * 256:(hh + 1) * 256], False, True, [("qT", p, 1), "Sbn"], ["pA"])
            for hh in range(4):
                K.act(junk, pA[:, hh * 256:(hh + 1) * 256], AF.Square, ["pA"], ["junk", "ss"], accum=ss[:, hh:hh + 1])
            K.ts("dve", ss[:, 4:8], ss[:, 0:4], 1.0 / 256.0, EPS, ALU.mult, ALU.add, ["ss"], ["ss"])
            K.tt("pool", ss[:, 0:4], ss[:, 4:8], nhalf, ALU.pow, ["ss", "nhalf"], ["ss"])
            for hh in range(4):
                K.stt("dve", gb[:, hh * 256:(hh + 1) * 256], pA[:, hh * 256:(hh + 1) * 256], ss[:, hh:hh + 1], rs[p][:, hh * 256:(hh + 1) * 256],
                      ALU.mult, ALU.mult, ["pA", "ss", ("rs", p)], ["gb"])
            state_update(p)
            transpose8(gb, gT, pT, "gb", "gT")
            for c in range(2):
                for k in range(8):
                    K.mm(pB[:, c * 512:(c + 1) * 512], gT[:, k, :], wout[:, k, c * 512:(c + 1) * 512], k == 0, k == 7, ["gT", "wout"], ["pB"])
            back(cm_, xdst, t, pB, "pB")

        K.memset("pool", S32, 0.0, ["S32"])
        K.memset("pool", Sb, 0.0, ["Sb"])
        if PIPE_F:
            s1f(0, 0)
            for t in range(NT):
                if t + 1 < NT:
                    s1f(t + 1, (t + 1) % 2)
                s2f(t, t % 2)
        else:
            for t in range(NT):
                s1f(t, t % 2)
                s2f(t, t % 2)

    def phase_moe(l, xsrc, xdst):
        K.phase()
        cm_ = {}
        wr = K.sb([128, 8, NE], F32)
        br = K.sb([1, NE], F32)
        desti = K.sb([128, NT * 4], I32)
        dumpf = K.sb([128, 4], F32)
        ecb = K.sb([128, NE], F32)
        cnt = K.sb([128, NE], F32)
        K.dma("sp", wr, moe_w_router[l].rearrange("(k p) n -> p k n", p=128), [], ["wr"], "rows")
        K.dma("sp", br, moe_b_router[l:l + 1, :], [], ["wr"], "rows")
        K.dma("sp", dumpf, dumpidx_in, [], ["dumpf"], "rows")
        K.ts("dve", dumpf, dumpf, 1.0, None, ALU.add, None, ["dumpf"], ["dumpf"])
        K.dma("sp", ecb, ecrow_in.partition_broadcast(128), [], ["ecb"], "bc")
        K.memset("pool", cnt, 0.0, ["cnt"])
        mark = K.off
        alloc_bc(1, l, ("scale", "shift"), cm_)
        alloc_work(cm_)
        RT = []
        for p in range(2):
            r_ = {"p": p}
            r_["h32"] = K.sb([128, D], F32)
            r_["hb"] = K.sb([128, D], BF16)
            r_["hT32"] = K.sb([128, 8, 128], F32)
            for nm in ("lg", "rank", "over", "dfull", "tmp", "tmp2"):
                r_[nm] = K.sb([128, NE], F32)
            r_["oh"] = [K.sb([128, NE], F32) for _ in range(4)]
            r_["maskb"] = K.sb([128, NE], BF16)
            for nm in ("top8", "sm", "destf"):
                r_[nm] = K.sb([128, 8], F32)
            r_["w4"] = K.sb([128, 4], F32)
            r_["w4x"] = K.sb([128, 4, 8], F32)
            r_["pT32"] = K.ps(1).rearrange("p (a b) -> p a b", a=4)
            r_["pL"] = K.ps(1)
            r_["pR"] = K.ps(1)
            K.memset("pool", r_["w4x"], 0.0, [("w4x", p)])
            RT.append(r_)

        def route_tile(r_, t):
            p = r_["p"]
            T_ = lambda nm: (nm, p)
            lg, top8, sm, w4, w4x, oh, maskb = r_["lg"], r_["top8"], r_["sm"], r_["w4"], r_["w4x"], r_["oh"], r_["maskb"]
            rank, over, dfull, tmp, tmp2, destf, pL, pR = r_["rank"], r_["over"], r_["dfull"], r_["tmp"], r_["tmp2"], r_["destf"], r_["pL"], r_["pR"]
            hT32 = r_["hT32"]
            for half in range(2):
                for j in range(4):
                    k = half * 4 + j
                    K.tr(r_["pT32"][:, j, :], r_["h32"][:, k * 128:(k + 1) * 128], id32, [T_("h32"), "c32"], [T_("pT32")])
                K.cp("act", hT32[:, half * 4:(half + 1) * 4, :], r_["pT32"], [T_("pT32")], [T_("hT32")])
            for k in range(8):
                K.mm(pL[:, 0:NE], hT32[:, k, :], wr[:, k, :], k == 0, False, [T_("hT32"), "wr"], [T_("pL")])
            K.mm(pL[:, 0:NE], ones_r32[0:1, 0:128], br, False, True, ["ones_r32", "wr"], [T_("pL")])
            K.cp("dve", lg, pL[:, 0:NE], [T_("pL")], [T_("lg")])
            S.op("dve", lambda e: e.max(out=top8, in_=lg), [T_("lg")], [T_("top8")])
            K.ts("dve", sm[:, 0:1], top8[:, 0:1], -1.0, None, ALU.mult, None, [T_("top8")], [T_("sm")])
            K.act(w4, top8[:, 0:4], AF.Exp, [T_("top8"), T_("sm")], [T_("w4"), T_("sm")], bias=sm[:, 0:1], accum=sm[:, 1:2])
            S.op("dve", lambda e: e.reciprocal(out=sm[:, 2:3], in_=sm[:, 1:2]), [T_("sm")], [T_("sm")])
            K.ts("dve", w4x[:, :, 0:1], w4.rearrange("p (a b) -> p a b", b=1), sm[:, 2:3], None, ALU.mult, None, [T_("w4"), T_("sm")], [T_("w4x")])
            for k in range(4):
                K.ts("dve", oh[k], lg, top8[:, k:k + 1], None, ALU.is_equal, None, [T_("lg"), T_("top8")], [T_("oh%d" % k)])
            K.ts("dve", maskb, lg, top8[:, 3:4], None, ALU.is_ge, None, [T_("lg"), T_("top8")], [T_("maskb")])
            K.mm(pR[:, 0:NE], cm(cb, 8), maskb, True, True, ["cb", T_("maskb")], [T_("pR")])
            K.mm(pR[:, NE:2 * NE], cm(cb, 7), maskb, True, True, ["cb", T_("maskb")], [T_("pR")])
            K.tt("dve", rank, pR[:, 0:NE], cnt, ALU.add, [T_("pR"), "cnt"], [T_("rank")])
            K.tt("dve", cnt, pR[:, NE:2 * NE], cnt, ALU.add, [T_("pR"), "cnt"], ["cnt"])
            K.ts("dve", over, rank, float(C), -1.0, ALU.is_ge, ALU.add, [T_("rank")], [T_("over")])
            K.stt("dve", dfull, rank, 1.0, ecb, ALU.add, ALU.add, [T_("rank"), "ecb"], [T_("dfull")])
            K.stt("dve", dfull, dfull, -1.0, over, ALU.mult, ALU.mult, [T_("dfull"), T_("over")], [T_("dfull")])
            for k in range(4):
                K.tt("dve", tmp, oh[k], dfull, ALU.mult, [T_("oh%d" % k), T_("dfull")], [T_("tmp")])
                S.op("dve", lambda e, k=k: e.reduce_sum(out=destf[:, k:k + 1], in_=tmp, axis=AX.X), [T_("tmp")], [T_("destf")])
            K.ts("dve", destf[:, 4:8], destf[:, 0:4], 0.0, None, ALU.is_equal, None, [T_("destf")], [T_("destf")])
            K.tt("dve", destf[:, 4:8], destf[:, 4:8], dumpf, ALU.mult, [T_("destf"), "dumpf"], [T_("destf")])
            K.stt("dve", destf[:, 0:4], destf[:, 0:4], -1.0, destf[:, 4:8], ALU.add, ALU.add, [T_("destf")], [T_("destf")])
            K.cp("dve", desti[:, t * 4:(t + 1) * 4], destf[:, 0:4], [T_("destf")], [("desti", t)])
            for k in range(4):
                idx = desti[:, t * 4 + k:t * 4 + k + 1]
                S.op("pool", lambda e, idx=idx: e.indirect_dma_start(out=Xs, out_offset=bass.IndirectOffsetOnAxis(ap=idx, axis=0), in_=r_["hb"], in_offset=None),
                     [T_("hb"), ("desti", t)], [], dma_tag="scatx%d_%d" % (k, p))
                S.op("pool", lambda e, idx=idx, k=k: e.indirect_dma_start(out=Ws, out_offset=bass.IndirectOffsetOnAxis(ap=idx, axis=0), in_=w4x[:, k, :], in_offset=None),
                     [T_("w4x"), ("desti", t)], [], dma_tag="scatw%d_%d" % (k, p))

        for t in range(NT):
            r_ = RT[t % 2]
            p = t % 2
            b = t % 3
            K.dma("sp", cm_["xt"][b], xsrc[t * 128:(t + 1) * 128, :], ["xsrc"], [("xt", b)], "xt%d" % b)
            K.tt("dve", r_["h32"], cm_["xt"][b], cm_["scale"], ALU.mult, [("xt", b), "bc_scale"], [("h32", p)])
            K.tt("pool", r_["hb"], r_["h32"], cm_["shift"], ALU.add, [("h32", p), "bc_shift"], [("hb", p)])
            K.tt("dve", r_["h32"], r_["h32"], cm_["shift"], ALU.add, [("h32", p), "bc_shift"], [("h32", p)])
            route_tile(r_, t)
        dbg("cnt%d" % l, cnt, "cnt")
        S.fence()
        K.off = mark
        K.pso = 0
        GS = 512 if C % 512 == 0 else 384
        NG = C // GS
        NB = GS // 128
        assert NG * GS == C
        bup = K.sb([128, NE, 16], F32)
        K.dma("sp", bup, moe_b_upT[l], [], ["bup"], "rows")
        K.ts("dve", bup[:, :, 8:16], bup[:, :, 8:16], 1.0, None, ALU.add, None, ["bup"], ["bup"])
        bup17 = K.sb([128, NE, 8], F32)
        K.ts("dve", bup17, bup[:, :, 0:8], 1.702, None, ALU.mult, None, ["bup"], ["bup17"])
        wup = [K.sb([128, 8, 2 * D], BF16) for _ in range(2)]
        wdn = [K.sb([128, 8, D], BF16) for _ in range(2)]
        NST = 3
        stage = [K.sb([128, 2 * D], F32) for _ in range(NST)]
        bd32 = [K.sb([1, D], F32) for _ in range(2)]
        bdb = [K.sb([1, D], BF16) for _ in range(2)]
        xs = [K.sb([128, D], BF16) for _ in range(2)]
        wsl = [K.sb([128, 8], F32) for _ in range(16)]
        xsT = [K.sb([128, 8, GS], BF16) for _ in range(2)]
        actT = [K.sb([128, 8, GS], BF16) for _ in range(2)]
        gt = [K.sb([128, GS], F32) for _ in range(2)]
        sg = [K.sb([128, GS], F32) for _ in range(2)]
        lA = [K.sb([128, GS], F32) for _ in range(2)]
        ys = [K.sb([128, 512], F32) for _ in range(3)]
        pTs = [K.ps(1).bitcast(BF16)[:, 0:512].rearrange("p (a b) -> p a b", a=4) for _ in range(2)]
        pG = [K.ps(1) for _ in range(2)]
        pLn = [K.ps(1) for _ in range(2)]
        pY = K.ps(2)
        cnts = {"ns": 0, "nx": 0, "nw": 0, "ny": 0, "nj": 0}

        def weight_steps(e_):
            be = e_ % 2
            steps = []
            for k in range(8):
                def f(k=k):
                    b = cnts["ns"] % NST
                    cnts["ns"] += 1
                    K.dma("sp", stage[b], moe_w_up[l, e_, k * 128:(k + 1) * 128, :], [], [("stage", b)], "stage%d" % b)
                    st_v = stage[b].rearrange("p (f two) -> p two f", two=2)
                    K.cp("act", wup[be][:, k, :].rearrange("p (two f) -> p two f", two=2), st_v, [("stage", b)], [("wup", be)])
                steps.append(f)
            for k2 in range(4):
                def f(k2=k2):
                    b = cnts["ns"] % NST
                    cnts["ns"] += 1
                    K.dma("sp", stage[b].rearrange("p (a n) -> p a n", a=2),
                          moe_w_down[l, e_, k2 * 256:(k2 + 1) * 256, :].rearrange("(a p) n -> p a n", p=128), [], [("stage", b)], "stage%d" % b)
                    K.cp("dve", wdn[be][:, 2 * k2:2 * k2 + 2, :], stage[b].rearrange("p (a n) -> p a n", a=2), [("stage", b)], [("wdn", be)])
                steps.append(f)

            def f():
                K.dma("sp", bd32[be], moe_b_down[l, e_:e_ + 1, :], [], [("bd32", be)], "bd32_%d" % be)
                K.cp("dve", bdb[be], bd32[be], [("bd32", be)], [("bdb", be)])
            steps.append(f)
            return steps

        units = [(e_, g) for e_ in range(NE) for g in range(NG)]
        wslots = {}

        def unit_A(u):
            e_, g = units[u]
            gb_ = u % 2
            base = e_ * C + g * GS
            wl = []
            for blk in range(NB):
                b = cnts["nx"] % 2
                cnts["nx"] += 1
                wi = cnts["nw"] % 16
                cnts["nw"] += 1
                wl.append(wi)
                r0 = base + blk * 128
                K.dma("sp", xs[b], Xs[r0:r0 + 128, :], ["Xs"], [("xs", b)], "xs%d" % b)
                K.dma("sp", wsl[wi], Ws[r0:r0 + 128, :], ["Ws"], [("wsl", wi)], "wsl%d" % wi)
                for half in range(2):
                    pT = pTs[half]
                    for j in range(4):
                        k = half * 4 + j
                        K.tr(pT[:, j, :], xs[b][:, k * 128:(k + 1) * 128], idb, [("xs", b), "cb"], [("pT", half)])
                    K.cp("act" if half == 0 else "dve", xsT[gb_][:, half * 4:(half + 1) * 4, blk * 128:(blk + 1) * 128], pT, [("pT", half)], [("xsT", gb_)])
            wslots[u] = wl

        def unit_U(u, pref):
            e_, g = units[u]
            be = e_ % 2
            gb_ = u % 2
            for j in range(8):
                jb = cnts["nj"] % 2
                cnts["nj"] += 1
                for k in range(8):
                    K.mm(pG[jb][:, 0:GS], wup[be][:, k, j * 128:(j + 1) * 128], xsT[gb_][:, k, :], k == 0, k == 7, [("wup", be), ("xsT", gb_)], [("pG", jb)])
                for k in range(8):
                    K.mm(pLn[jb][:, 0:GS], wup[be][:, k, D + j * 128:D + (j + 1) * 128], xsT[gb_][:, k, :], k == 0, k == 7, [("wup", be), ("xsT", gb_)], [("pLn", jb)])
                K.act(gt[jb], pG[jb][:, 0:GS], AF.Identity, [("pG", jb), "bup"], [("gt", jb)], bias=bup[:, e_, j:j + 1], scale=1.0)
                K.act(sg[jb], pG[jb][:, 0:GS], AF.Sigmoid, [("pG", jb), "bup17"], [("sg", jb)], bias=bup17[:, e_, j:j + 1], scale=1.702)
                K.act(lA[jb], pLn[jb][:, 0:GS], AF.Identity, [("pLn", jb), "bup"], [("lA", jb)], bias=bup[:, e_, 8 + j:9 + j], scale=1.0)
                K.ts("dve", lA[jb], lA[jb], -6.0, 8.0, ALU.max, ALU.min, [("lA", jb)], [("lA", jb)])
                K.stt("dve", gt[jb], gt[jb], 7.0, lA[jb], ALU.min, ALU.mult, [("gt", jb), ("lA", jb)], [("gt", jb)])
                K.stt("dve", actT[gb_][:, j, :], sg[jb], SIG7, gt[jb], ALU.min, ALU.mult, [("sg", jb), ("gt", jb)], [("actT", gb_)])
                if pref:
                    pref.pop(0)()
                    if len(pref) > 8 - j and j % 2 == 1:
                        pref.pop(0)()

        def unit_D(u):
            e_, g = units[u]
            be = e_ % 2
            gb_ = u % 2
            base = e_ * C + g * GS
            for blk in range(NB):
                wi = wslots[u][blk]
                r0 = base + blk * 128
                for c in range(2):
                    for k in range(8):
                        K.mm(pY[:, c * 512:(c + 1) * 512], actT[gb_][:, k, blk * 128:(blk + 1) * 128], wdn[be][:, k, c * 512:(c + 1) * 512], k == 0, False, [("actT", gb_), ("wdn", be)], [("pY", c)])
                    K.mm(pY[:, c * 512:(c + 1) * 512], ones_rb[0:1, 0:128], bdb[be][0:1, c * 512:(c + 1) * 512], False, True, ["ones_rb", ("bdb", be)], [("pY", c)])
                    b = cnts["ny"] % 3
                    cnts["ny"] += 1
                    K.act(ys[b], pY[:, c * 512:(c + 1) * 512], AF.Copy, [("pY", c), ("wsl", wi)], [("ys", b)], scale=wsl[wi][:, 0:1])
                    K.dma("act", Ys[r0:r0 + 128, c * 512:(c + 1) * 512], ys[b], [("ys", b)], [], "ys%d" % b)

        for f in weight_steps(0):
            f()
        unit_A(0)
        pref = []
        for u in range(len(units)):
            e_, g = units[u]
            if g == 0:
                for f in pref:
                    f()
                pref = weight_steps(e_ + 1) if e_ + 1 < NE else []
            if u + 1 < len(units):
                unit_A(u + 1)
            unit_U(u, pref)
            unit_D(u)
        for f in pref:
            f()
        S.fence()
        K.off = mark
        K.pso = 0
        alloc_bc(1, l, ("gate", "lng", "lnb"), cm_)
        alloc_work(cm_)
        yk = [[K.sb([128, D], F32) for _ in range(4)] for _ in range(2)]
        for t in range(NT):
            b = t % 2
            y_ = yk[b]
            bx = t % 3
            K.dma("sp", cm_["xt"][bx], xsrc[t * 128:(t + 1) * 128, :], ["xsrc"], [("xt", bx)], "xt%d" % bx)
            for k in range(4):
                idx = desti[:, t * 4 + k:t * 4 + k + 1]
                S.op("pool", lambda e, idx=idx, k=k, y_=y_: e.indirect_dma_start(out=y_[k], out_offset=None, in_=Ys, in_offset=bass.IndirectOffsetOnAxis(ap=idx, axis=0)),
                     ["Ys", ("desti", t)], [("yk", b, k)], dma_tag="gath%d_%d" % (k, b))
            K.tt("dve", y_[0], y_[0], y_[1], ALU.add, [("yk", b, 0), ("yk", b, 1)], [("yk", b, 0)])
            K.tt("pool", y_[2], y_[2], y_[3], ALU.add, [("yk", b, 2), ("yk", b, 3)], [("yk", b, 2)])
            K.tt("dve", y_[0], y_[0], y_[2], ALU.add, [("yk", b, 0), ("yk", b, 2)], [("yk", b, 0)])
            back(cm_, xdst, t, y_[0], ("yk", b, 0))

    def phase_init():
        K.phase()
        zt = K.sb([128, 4096], F32)
        K.memset("pool", zt, 0.0, ["zt"])
        ztb = zt.bitcast(BF16)
        nrows = NSLOT + NDUMP
        assert nrows % 512 == 0 and (nrows // 128) * 8 <= 4096
        for r in range(0, nrows, 512):
            K.dma("sp", Xs[r:r + 512, :].rearrange("(p a) d -> p (a d)", p=128), ztb[:, 0:4 * D], ["zt"], [], "zinit%d" % ((r // 512) % 4))
        K.dma("sp", Ws.rearrange("(p a) d -> p (a d)", p=128), zt[:, 0:(nrows // 128) * 8], ["zt"], ["Ws"], "zinit")
        K.dma("sp", Ys[NSLOT:NSLOT + NDUMP, :].rearrange("(p a) d -> p (a d)", p=128), zt[:, 0:4 * D], ["zt"], ["Ys"], "zinit")

    phase_mod()
    if do_moe:
        phase_init()
    cur = x_in
    bufs = [xa, xb_]
    nb = 0
    stages = []
    for l in layer_list:
        if do_mixer:
            stages.append(("mix", l))
        if do_moe:
            stages.append(("moe", l))
    for si, (kind, l) in enumerate(stages):
        dst = out if si == len(stages) - 1 else bufs[nb % 2]
        nb += 1
        if kind == "mix":
            if l % 2 == 0:
                phase_gla(l, cur, dst)
            else:
                phase_sgu(l, cur, dst)
        else:
            phase_moe(l, cur, dst)
        cur = dst
    cnt = S.emit()
    return nc, cnt


def prep_shared(inputs, C):
    f = lambda a: np.ascontiguousarray(np.asarray(a, dtype=np.float32))
    sh = {}
    for k_ in ("ada_w", "ada_b", "ln_g", "ln_b", "gla_w_in", "gla_wg2_f", "gla_bg_f", "gla_wg2_b", "gla_bg_b", "gla_norm_g", "gla_w_out",
               "sgu_w_in", "sgu_b_in", "sgu_ln_g", "sgu_ln_b", "sgu_w_out", "sgu_b_out", "moe_w_router", "moe_b_router", "moe_w_up",
               "moe_w_down", "moe_b_down"):
        sh[k_] = f(inputs[k_])
    sh["sgu_w_sT"] = f(np.transpose(np.asarray(inputs["sgu_w_s"]), (0, 3, 1, 2)))
    sh["sgu_b_sT"] = f(np.transpose(np.asarray(inputs["sgu_b_s"]), (0, 2, 1)))
    bu = np.asarray(inputs["moe_b_up"], dtype=np.float32)
    bg = bu[:, :, 0::2].reshape(4, NE, 8, 128)
    bl = bu[:, :, 1::2].reshape(4, NE, 8, 128)
    sh["moe_b_upT"] = f(np.transpose(np.concatenate([bg, bl], axis=2), (0, 3, 1, 2)))
    sh["consts"] = make_consts()
    sh["dumpidx"] = (NE * C + np.arange(4)[None, :] * 128 + np.arange(128)[:, None]).astype(np.float32)
    sh["ecrow"] = (np.arange(NE, dtype=np.float32) * C)[None, :]
    return sh


CAP = 1024
_cache = {}


def kernel(**inputs):
    x = np.asarray(inputs["x"], dtype=np.float32)
    c = np.asarray(inputs["c"], dtype=np.float32)
    B, T, _ = x.shape
    key = (T, CAP)
    if key not in _cache:
        _cache[key] = build_program(T, CAP)[0]
    nc = _cache[key]
    sh = prep_shared(inputs, CAP)
    in_maps = []
    for b in range(B):
        m = dict(sh)
        m["x"] = np.ascontiguousarray(x[b])
        m["cT"] = np.ascontiguousarray(c[b].reshape(8, 128).T)
        in_maps.append(m)
    res = run_bass_kernel_spmd(nc, in_maps, core_ids=list(range(B)))
    return np.stack([np.asarray(r["out"], dtype=np.float32) for r in res.results], axis=0)
```

```python
import numpy as np
from contextlib import ExitStack
import concourse.bass as bass
import concourse.mybir as mybir
from concourse.bass_utils import run_bass_kernel_spmd

F32 = mybir.dt.float32
BF16 = mybir.dt.bfloat16
I32 = mybir.dt.int32
AF = mybir.ActivationFunctionType
ALU = mybir.AluOpType
AX = mybir.AxisListType

D = 1024
DEPTH = 4
ALPHA = (2.0 * DEPTH) ** 0.25
EPS = 1e-5
NE = 32
GIN = 3104
NDUMP = 512
SIG7 = float(1.0 / (1.0 + np.exp(-1.702 * 7.0)))
DEBUG = False
PIPE_B = False
PIPE_F = True


class _Op:
    __slots__ = ("eng", "fn", "deps", "is_dma", "tag", "val", "signal", "idx")


class Sched:
    ENGS = ("pe", "dve", "act", "pool", "sp")

    def __init__(self, nc, same_engine_sync=True):
        self.nc = nc
        self.ops = []
        self.last_w = {}
        self.readers = {}
        self.same_engine_sync = same_engine_sync
        self.dma_count = {}
        self.last_dma = {}
        self._cap = None

    def capture(self):
        self._cap = []

    def end_capture(self):
        c, self._cap = self._cap, None
        return c

    def replay(self, lists, lag):
        idx = [0] * len(lists)
        step = 0
        while True:
            done = True
            for i, L in enumerate(lists):
                if idx[i] < len(L):
                    done = False
                    if step >= i * lag:
                        self.op(*L[idx[i]])
                        idx[i] += 1
            if done:
                break
            step += 1

    def op(self, eng, fn, reads=(), writes=(), dma_tag=None):
        if self._cap is not None:
            self._cap.append((eng, fn, tuple(reads), tuple(writes), dma_tag))
            return None
        o = _Op()
        o.eng = eng
        o.fn = fn
        o.is_dma = dma_tag is not None
        o.tag = dma_tag
        o.signal = False
        o.idx = len(self.ops)
        deps = {}
        for t in reads:
            w = self.last_w.get(t)
            if w is not None:
                deps[w.idx] = w
        for t in writes:
            w = self.last_w.get(t)
            if w is not None:
                deps[w.idx] = w
            for r in self.readers.get(t, ()):
                deps[r.idx] = r
        if o.is_dma:
            p = self.last_dma.get(dma_tag)
            if p is not None:
                deps[p.idx] = p
            self.last_dma[dma_tag] = o
        o.deps = list(deps.values())
        for t in writes:
            self.last_w[t] = o
            self.readers[t] = []
        for t in reads:
            lst = self.readers.setdefault(t, [])
            key = (o.eng, o.tag)
            lst[:] = [r for r in lst if (r.eng, r.tag) != key]
            lst.append(o)
        if o.is_dma:
            c = self.dma_count.get(dma_tag, 0) + 1
            self.dma_count[dma_tag] = c
            o.val = 16 * c
        self.ops.append(o)
        return o

    def fence(self):
        last = {}
        for o in self.ops:
            if o.fn is None:
                continue
            last[(o.eng, o.tag)] = o
        deps = list(last.values())
        for e in self.ENGS:
            o = _Op()
            o.eng = e
            o.fn = None
            o.is_dma = False
            o.tag = None
            o.signal = False
            o.idx = len(self.ops)
            o.deps = list(deps)
            self.ops.append(o)
        self.last_w = {}
        self.readers = {}

    def dma(self, eng, out, in_, reads, writes, tag, **kw):
        return self.op(eng, lambda e: e.dma_start(out=out, in_=in_, **kw), reads, writes, dma_tag=tag)

    def _needs_wait(self, o, d):
        if d.is_dma:
            return True
        if d.eng == o.eng and not o.is_dma:
            if o.eng == "pe":
                return False
            return self.same_engine_sync
        return True

    def emit(self):
        nc = self.nc
        for o in self.ops:
            for d in o.deps:
                if self._needs_wait(o, d) and not d.is_dma:
                    d.signal = True
        cnt = {e: 0 for e in self.ENGS}
        for o in self.ops:
            if not o.is_dma and o.signal:
                cnt[o.eng] += 1
                o.val = cnt[o.eng]
        streams = {e: [] for e in self.ENGS}
        for o in self.ops:
            streams[o.eng].append(o)
        with ExitStack() as es:
            esem = {e: es.enter_context(nc.semaphore("s_" + e)) for e in self.ENGS}
            dsem = {t: es.enter_context(nc.semaphore("d_" + str(t))) for t in self.dma_count}
            block = es.enter_context(nc.Block())

            def run(ename, eng):
                known = {}
                for o in streams[ename]:
                    for d in o.deps:
                        if not self._needs_wait(o, d):
                            continue
                        if d.is_dma:
                            s, v, k = dsem[d.tag], d.val, ("d", d.tag)
                        else:
                            s, v, k = esem[d.eng], d.val, ("e", d.eng)
                        if known.get(k, 0) >= v:
                            continue
                        known[k] = v
                        eng.wait_ge(s, v)
                    if o.fn is None:
                        continue
                    ins = o.fn(eng)
                    if o.is_dma:
                        ins.then_inc(dsem[o.tag], 16)
                    elif o.signal:
                        ins.then_inc(esem[ename], 1)
                if ename == "sp":
                    for t, c in self.dma_count.items():
                        eng.wait_ge(dsem[t], 16 * c)

            @block.tensor
            def _(e):
                run("pe", e)

            @block.vector
            def _(e):
                run("dve", e)

            @block.scalar
            def _(e):
                run("act", e)

            @block.gpsimd
            def _(e):
                run("pool", e)

            @block.sync
            def _(e):
                run("sp", e)
        return cnt


SB_BYTES = 211968


class KB:
    def __init__(self, nc):
        self.nc = nc
        self.S = Sched(nc)
        self.SB = nc.alloc_sbuf_tensor("SB", [128, SB_BYTES // 4], F32).ap()
        self.PS = nc.alloc_psum_tensor("PS", [128, 4096], F32).ap()
        self.off = 0
        self.base = 0
        self.pso = 0
        self.uid = 0

    def persist(self):
        self.base = self.off

    def phase(self):
        self.S.fence()
        self.off = self.base
        self.pso = 0

    def sb(self, shape, dt, np_=128):
        n = int(np.prod(shape[1:]))
        bpe = 2 if dt == BF16 else 4
        nb = (n * bpe + 63) // 64 * 64
        assert self.off + nb <= SB_BYTES, ("SBUF overflow", self.off, nb)
        a = self.SB[0:shape[0], self.off // 4:(self.off + nb) // 4]
        self.off += nb
        if dt != F32:
            a = a.bitcast(dt)
        a = a[:, 0:n]
        if len(shape) == 3:
            a = a.rearrange("p (a b) -> p a b", a=shape[1])
        return a

    def ps(self, nbanks=1):
        assert self.pso + nbanks <= 8
        a = self.PS[:, self.pso * 512:(self.pso + nbanks) * 512]
        self.pso += nbanks
        return a

    def mm(self, out, lhsT, rhs, start, stop, r, w):
        self.S.op("pe", lambda e: e.matmul(out, lhsT=lhsT, rhs=rhs, start=start, stop=stop), r, w)

    def tr(self, out, in_, ident, r, w):
        self.S.op("pe", lambda e: e.transpose(out=out, in_=in_, identity=ident), r, w)

    def tt(self, eng, out, in0, in1, op, r, w):
        self.S.op(eng, lambda e: e.tensor_tensor(out=out, in0=in0, in1=in1, op=op), r, w)

    def ts(self, eng, out, in0, s1, s2, op0, op1, r, w):
        if op1 is None:
            self.S.op(eng, lambda e: e.tensor_scalar(out=out, in0=in0, scalar1=s1, scalar2=None, op0=op0), r, w)
        else:
            self.S.op(eng, lambda e: e.tensor_scalar(out=out, in0=in0, scalar1=s1, scalar2=s2, op0=op0, op1=op1), r, w)

    def stt(self, eng, out, in0, sc, in1, op0, op1, r, w):
        self.S.op(eng, lambda e: e.scalar_tensor_tensor(out=out, in0=in0, scalar=sc, in1=in1, op0=op0, op1=op1), r, w)

    def cp(self, eng, out, in_, r, w):
        if eng == "act":
            self.S.op(eng, lambda e: e.copy(out=out, in_=in_), r, w)
        else:
            self.S.op(eng, lambda e: e.tensor_copy(out=out, in_=in_), r, w)

    def act(self, out, in_, func, r, w, bias=None, scale=None, accum=None):
        kw = {}
        if bias is not None:
            kw["bias"] = bias
        if scale is not None:
            kw["scale"] = scale
        if accum is not None:
            kw["accum_out"] = accum
        self.S.op("act", lambda e: e.activation(out=out, in_=in_, func=func, **kw), r, w)

    def memset(self, eng, ap, v, w):
        self.S.op(eng, lambda e: e.memset(ap, v), [], w)

    def dma(self, eng, out, in_, r, w, tag, **kw):
        self.S.dma(eng, out, in_, r, w, tag, **kw)


def make_consts():
    j = np.arange(128)[:, None]
    i = np.arange(128)[None, :]
    c = -1.0 / 16.0
    mats = [
        np.eye(128),
        (j <= i) * c,
        (j >= i) * c,
        (j > i) * c,
        (j < i) * c,
        (j <= i) * 1.0,
        (j >= i) * 1.0,
        np.ones((128, 128)),
        (j < i) * 1.0,
    ]
    return np.concatenate(mats, axis=1).astype(np.float32)


NCONST = 9


def build_program(T, C, layer_list=(0, 1, 2, 3), do_mixer=True, do_moe=True):
    NT = T // 128
    NSLOT = NE * C
    nc = bass.Bass("TRN2", target_bir_lowering=False)
    dt_in = lambda name, shape, dt=F32: nc.dram_tensor(name, list(shape), dt, kind="ExternalInput").ap()
    dt_int = lambda name, shape, dt=F32: nc.dram_tensor(name, list(shape), dt, kind="Internal").ap()
    x_in = dt_in("x", [T, D])
    cT_in = dt_in("cT", [128, 8])
    ada_w = dt_in("ada_w", [4, D, 6 * D])
    ada_b = dt_in("ada_b", [4, 6 * D])
    ln_g = dt_in("ln_g", [4, 2, D])
    ln_b = dt_in("ln_b", [4, 2, D])
    gla_w_in = dt_in("gla_w_in", [2, D, GIN])
    gla_wg2_f = dt_in("gla_wg2_f", [2, 16, 512])
    gla_bg_f = dt_in("gla_bg_f", [2, 512])
    gla_wg2_b = dt_in("gla_wg2_b", [2, 16, 512])
    gla_bg_b = dt_in("gla_bg_b", [2, 512])
    gla_norm_g = dt_in("gla_norm_g", [2, 256])
    gla_w_out = dt_in("gla_w_out", [2, D, D])
    sgu_w_in = dt_in("sgu_w_in", [2, D, 2 * D])
    sgu_b_in = dt_in("sgu_b_in", [2, 2 * D])
    sgu_ln_g = dt_in("sgu_ln_g", [2, D])
    sgu_ln_b = dt_in("sgu_ln_b", [2, D])
    sgu_w_sT = dt_in("sgu_w_sT", [2, 128, 8, 128])
    sgu_b_sT = dt_in("sgu_b_sT", [2, 128, 8])
    sgu_w_out = dt_in("sgu_w_out", [2, D, D])
    sgu_b_out = dt_in("sgu_b_out", [2, D])
    moe_w_router = dt_in("moe_w_router", [4, D, NE])
    moe_b_router = dt_in("moe_b_router", [4, NE])
    moe_w_up = dt_in("moe_w_up", [4, NE, D, 2 * D])
    moe_b_upT = dt_in("moe_b_upT", [4, 128, NE, 16])
    moe_w_down = dt_in("moe_w_down", [4, NE, D, D])
    moe_b_down = dt_in("moe_b_down", [4, NE, D])
    consts_in = dt_in("consts", [128, NCONST * 128])
    dumpidx_in = dt_in("dumpidx", [128, 4])
    ecrow_in = dt_in("ecrow", [1, NE])
    out = nc.dram_tensor("out", [T, D], F32, kind="ExternalOutput").ap()

    xa = dt_int("xa", [T, D])
    xb_ = dt_int("xb", [T, D])
    modd = dt_int("modd", [4, 6 * D])
    sbn = dt_int("sbn", [NT, 128, 1024], BF16)
    Xs = dt_int("Xs", [NSLOT + NDUMP, D], BF16)
    Ws = dt_int("Ws", [NSLOT + NDUMP, 8])
    Ys = dt_int("Ys", [NSLOT + NDUMP, D])

    K = KB(nc)
    S = K.S
    dbg_outs = {}

    def dbg(name, ap, tok, dt=F32):
        if not DEBUG:
            return
        shape = [ap.shape[0], int(np.prod(ap.shape[1:]))]
        d_ = nc.dram_tensor("dbg_" + name, shape, dt, kind="ExternalOutput").ap()
        src = ap if len(ap.shape) == 2 else ap.rearrange("p a b -> p (a b)")
        K.dma("sp", d_, src, [tok], [], "dbg_" + name)
        dbg_outs[name] = d_

    c32 = K.sb([128, NCONST * 128], F32)
    cb = K.sb([128, NCONST * 128], BF16)
    ones_r32 = K.sb([1, 512], F32)
    ones_rb = K.sb([1, 512], BF16)
    nhalf = K.sb([128, 4], F32)
    K.dma("sp", c32, consts_in, [], ["c32"], "c32")
    K.cp("dve", cb, c32, ["c32"], ["cb"])
    K.memset("pool", ones_r32, 1.0, ["ones_r32"])
    K.cp("dve", ones_rb, ones_r32, ["ones_r32"], ["ones_rb"])
    K.memset("pool", nhalf, -0.5, ["nhalf"])
    cm = lambda t, i: t[:, i * 128:(i + 1) * 128]
    id32, idb = cm(c32, 0), cm(cb, 0)
    CT = ["c32", "cb", "ones_r32", "ones_rb", "nhalf"]
    K.persist()

    def phase_mod():
        K.phase()
        cT = K.sb([128, 8], F32)
        sc = K.sb([128, 8], F32)
        wt = [K.sb([128, 8, 512], F32) for _ in range(2)]
        ab = K.sb([1, 6 * D], F32)
        mrow = K.sb([1, 6 * D], F32)
        pm = K.ps(1)
        K.dma("sp", cT, cT_in, [], ["cT"], "cT")
        K.act(sc, cT, AF.Silu, ["cT"], ["sc"])
        n = 0
        for l in layer_list:
            K.dma("sp", ab, ada_b[l:l + 1, :], [], ["ab"], "ab")
            for j in range(12):
                b = n % 2
                n += 1
                K.dma("sp", wt[b], ada_w[l][:, j * 512:(j + 1) * 512].rearrange("(k p) n -> p k n", p=128), [], [("wt", b)], "wt%d" % b)
                for k in range(8):
                    K.mm(pm[0:1, :], sc[:, k:k + 1], wt[b][:, k, :], k == 0, k == 7, ["sc", ("wt", b)], ["pm"])
                plus = 1.0 if (j // 2) in (1, 2, 4, 5) else 0.0
                K.stt("dve", mrow[:, j * 512:(j + 1) * 512], pm[0:1, :], plus, ab[:, j * 512:(j + 1) * 512], ALU.add, ALU.add, ["pm", "ab"], ["mrow"])
            K.dma("sp", modd[l:l + 1, :], mrow, ["mrow"], ["modd"], "mrow")

    def load_bc(dst, row, tok):
        K.dma("sp", dst, row.partition_broadcast(128), ["modd"], [tok], "bc")

    def alloc_bc(sub, l, names=("scale", "shift", "gate", "lng", "lnb"), t=None):
        t = {} if t is None else t
        o = sub * 3 * D
        srcs = {"shift": modd[l:l + 1, o:o + D], "scale": modd[l:l + 1, o + D:o + 2 * D], "gate": modd[l:l + 1, o + 2 * D:o + 3 * D],
                "lng": ln_g[l, sub:sub + 1, :], "lnb": ln_b[l, sub:sub + 1, :]}
        for nm in names:
            t[nm] = K.sb([128, D], F32)
            load_bc(t[nm], srcs[nm], "bc_" + nm)
        return t

    def alloc_work(t):
        t["xt"] = [K.sb([128, D], F32) for _ in range(3)]
        t["h32"] = K.sb([128, D], F32)
        t["z"] = [K.sb([128, D], F32) for _ in range(2)]
        t["xo"] = [K.sb([128, D], F32) for _ in range(2)]
        t["st"] = [K.sb([128, 2, 6], F32) for _ in range(2)]
        t["mv"] = [K.sb([128, 8], F32) for _ in range(2)]
        return t

    def alloc_common(sub, l):
        return alloc_work(alloc_bc(sub, l))

    BCT = ["bc_shift", "bc_scale", "bc_gate", "bc_lng", "bc_lnb"]

    def front(cm_, xsrc, t, hb):
        b = t % 3
        K.dma("sp", cm_["xt"][b], xsrc[t * 128:(t + 1) * 128, :], ["xsrc"], [("xt", b)], "xt%d" % b)
        K.tt("dve", cm_["h32"], cm_["xt"][b], cm_["scale"], ALU.mult, [("xt", b), "bc_scale"], ["h32"])
        if hb is not None:
            K.tt("dve", hb, cm_["h32"], cm_["shift"], ALU.add, ["h32", "bc_shift"], ["hb"])
        else:
            K.tt("dve", cm_["h32"], cm_["h32"], cm_["shift"], ALU.add, ["h32", "bc_shift"], ["h32"])

    def back(cm_, xdst, t, y_ap, ytok):
        b = t % 2
        bx = t % 3
        z, mv, st = cm_["z"][b], cm_["mv"][b], cm_["st"][b]
        Z, MV, ST = ("z", b), ("mv", b), ("st", b)
        K.tt("dve", z, y_ap, cm_["gate"], ALU.mult, [ytok, "bc_gate"], [Z])
        K.stt("dve", z, cm_["xt"][bx], ALPHA, z, ALU.mult, ALU.add, [("xt", bx), Z], [Z])
        for c in range(2):
            S.op("dve", lambda e, c=c: e.bn_stats(out=st[:, c, :], in_=z[:, c * 512:(c + 1) * 512]), [Z], [ST])
        S.op("dve", lambda e: e.bn_aggr(out=mv[:, 0:2], in_=st.rearrange("p a b -> p (a b)")), [ST], [MV])
        K.ts("dve", mv[:, 2:3], mv[:, 1:2], EPS, None, ALU.add, None, [MV], [MV])
        K.tt("pool", mv[:, 3:4], mv[:, 2:3], nhalf[:, 0:1], ALU.pow, [MV, "nhalf"], [MV])
        K.stt("dve", mv[:, 4:5], mv[:, 0:1], -1.0, mv[:, 3:4], ALU.mult, ALU.mult, [MV], [MV])
        K.act(z, z, AF.Identity, [Z, MV], [Z], bias=mv[:, 4:5], scale=mv[:, 3:4])
        K.tt("dve", z, z, cm_["lng"], ALU.mult, [Z, "bc_lng"], [Z])
        K.tt("pool", cm_["xo"][b], z, cm_["lnb"], ALU.add, [Z, "bc_lnb"], [("xo", b)])
        K.dma("sp", xdst[t * 128:(t + 1) * 128, :], cm_["xo"][b], [("xo", b)], [], "xo%d" % b)

    def transpose8(src_b, dstT, pT, srctok, dsttok, ident=None, eng="act", ptok="pT"):
        ident = idb if ident is None else ident
        for half in range(2):
            for j in range(4):
                k = half * 4 + j
                K.tr(pT[:, j, :], src_b[:, k * 128:(k + 1) * 128], ident, [srctok, "cb", "c32"], [ptok])
            K.cp(eng, dstT[:, half * 4:(half + 1) * 4, :], pT, [ptok], [dsttok])

    def load_w_cast(dst, src, tok, tag):
        N = src.shape[-1]
        c0 = 0
        while c0 < N:
            c1 = min(N, c0 + 2048)
            K.dma("pool", dst[:, :, c0:c1], src[:, c0:c1].rearrange("(k p) n -> p k n", p=128), [], [tok], tag)
            c0 = c1

    def load_row_bf16(dst_b, tmp32, src_row, tok):
        K.dma("sp", tmp32, src_row, [], [tok + "_32"], "rows")
        K.cp("dve", dst_b, tmp32, [tok + "_32"], [tok])

    def phase_sgu(l, xsrc, xdst):
        i = l // 2
        K.phase()
        cm_ = alloc_common(0, l)
        win = K.sb([128, 8, 2 * D], BF16)
        wout = K.sb([128, 8, D], BF16)
        wsT = K.sb([128, 8, 128], BF16)
        ws32 = K.sb([128, 8, 128], F32)
        bsT = K.sb([128, 8], F32)
        r32 = K.sb([1, 2 * D], F32)
        r32b = K.sb([1, D], F32)
        binr = K.sb([1, 2 * D], BF16)
        boutr = K.sb([1, D], BF16)
        slg = K.sb([128, D], F32)
        slb = K.sb([128, D], F32)
        hb = K.sb([128, D], BF16)
        hT = K.sb([128, 8, 128], BF16)
        u = [K.sb([128, D], F32) for _ in range(2)]
        v = K.sb([128, D], F32)
        vb = [K.sb([128, D], BF16) for _ in range(2)]
        gb = K.sb([128, D], BF16)
        gT = K.sb([128, 8, 128], BF16)
        st2 = K.sb([128, 2, 6], F32)
        mv2 = K.sb([128, 8], F32)
        pT = K.ps(1).bitcast(BF16)[:, 0:512].rearrange("p (a b) -> p a b", a=4)
        pT2 = K.ps(1).bitcast(BF16)[:, 0:512].rearrange("p (a b) -> p a b", a=4)
        pz = K.ps(2)
        psv = K.ps(2)
        py = K.ps(2)
        load_w_cast(win, sgu_w_in[i], "win", "wload")
        load_w_cast(wout, sgu_w_out[i], "wout", "wload")
        K.dma("sp", ws32, sgu_w_sT[i], [], ["ws32"], "rows")
        K.cp("dve", wsT, ws32, ["ws32"], ["wsT"])
        K.dma("sp", bsT, sgu_b_sT[i], [], ["bsT"], "rows")
        load_row_bf16(binr, r32, sgu_b_in[i:i + 1, :], "binr")
        load_row_bf16(boutr, r32b, sgu_b_out[i:i + 1, :], "boutr")
        K.dma("sp", slg, sgu_ln_g[i:i + 1, :].partition_broadcast(128), [], ["slg"], "bc")
        K.dma("sp", slb, sgu_ln_b[i:i + 1, :].partition_broadcast(128), [], ["slb"], "bc")

        def s1(t):
            p = t % 2
            front(cm_, xsrc, t, hb)
            transpose8(hb, hT, pT, "hb", "hT", ptok="pT")
            for part in range(2):
                dst = u[p] if part == 0 else v
                for c in range(2):
                    col = part * D + c * 512
                    for k in range(8):
                        K.mm(pz[:, c * 512:(c + 1) * 512], hT[:, k, :], win[:, k, col:col + 512], k == 0, False, ["hT", "win"], ["pz"])
                    K.mm(pz[:, c * 512:(c + 1) * 512], ones_rb[0:1, 0:128], binr[0:1, col:col + 512], False, True, ["ones_rb", "binr"], ["pz"])
                K.act(dst, pz, AF.Gelu, ["pz"], [("u", p) if part == 0 else "v"])
            for c in range(2):
                S.op("dve", lambda e, c=c: e.bn_stats(out=st2[:, c, :], in_=v[:, c * 512:(c + 1) * 512]), ["v"], ["st2"])
            S.op("dve", lambda e: e.bn_aggr(out=mv2[:, 0:2], in_=st2.rearrange("p a b -> p (a b)")), ["st2"], ["mv2"])
            K.ts("dve", mv2[:, 2:3], mv2[:, 1:2], EPS, None, ALU.add, None, ["mv2"], ["mv2"])
            K.tt("pool", mv2[:, 3:4], mv2[:, 2:3], nhalf[:, 0:1], ALU.pow, ["mv2", "nhalf"], ["mv2"])
            K.stt("dve", mv2[:, 4:5], mv2[:, 0:1], -1.0, mv2[:, 3:4], ALU.mult, ALU.mult, ["mv2"], ["mv2"])
            K.act(v, v, AF.Identity, ["v", "mv2"], ["v"], bias=mv2[:, 4:5], scale=mv2[:, 3:4])
            K.tt("dve", v, v, slg, ALU.mult, ["v", "slg"], ["v"])
            K.tt("pool", vb[p], v, slb, ALU.add, ["v", "slb"], [("vb", p)])

        def s2(t):
            p = t % 2
            for g in range(8):
                K.mm(psv[:, g * 128:(g + 1) * 128], wsT[:, g, :], vb[p][:, g * 128:(g + 1) * 128], True, True, ["wsT", ("vb", p)], ["psv"])
            for g in range(8):
                K.stt("dve", gb[:, g * 128:(g + 1) * 128], psv[:, g * 128:(g + 1) * 128], bsT[:, g:g + 1], u[p][:, g * 128:(g + 1) * 128],
                      ALU.add, ALU.mult, ["psv", "bsT", ("u", p)], ["gb"])
            transpose8(gb, gT, pT2, "gb", "gT", ptok="pT2")
            for c in range(2):
                for k in range(8):
                    K.mm(py[:, c * 512:(c + 1) * 512], gT[:, k, :], wout[:, k, c * 512:(c + 1) * 512], k == 0, False, ["gT", "wout"], ["py"])
                K.mm(py[:, c * 512:(c + 1) * 512], ones_rb[0:1, 0:128], boutr[0:1, c * 512:(c + 1) * 512], False, True, ["ones_rb", "boutr"], ["py"])
            back(cm_, xdst, t, py, "py")

        s1(0)
        for t in range(NT):
            lists = []
            if t + 1 < NT:
                S.capture()
                s1(t + 1)
                lists.append(S.end_capture())
            S.capture()
            s2(t)
            lists.append(S.end_capture())
            S.replay(lists, 0)

    def phase_gla(l, xsrc, xdst):
        i = l // 2
        K.phase()
        cm_ = alloc_common(0, l)
        win = K.sb([128, 8, GIN], BF16)
        wout = K.sb([128, 8, D], BF16)
        wg2 = [K.sb([16, 512], F32) for _ in range(2)]
        bg = [K.sb([1, 512], F32) for _ in range(2)]
        ng4 = K.sb([128, D], F32)
        hb = K.sb([128, D], BF16)
        hT = K.sb([128, 8, 128], BF16)
        vb = [K.sb([128, D], BF16) for _ in range(2)]
        kt32 = K.sb([128, 512], F32)
        lr = K.sb([16, 256], F32)
        rs = [K.sb([128, D], F32) for _ in range(2)]
        ex = K.sb([128, 512], F32)
        spl = [K.sb([128, 512], F32) for _ in range(2)]
        Eq = [K.sb([128, 512], F32) for _ in range(2)]
        Ek = [K.sb([128, 512], F32) for _ in range(2)]
        Ed = K.sb([128, 512], F32)
        qT = [[K.sb([128, 512], BF16) for _ in range(2)] for _ in range(2)]
        kT = [K.sb([128, 512], BF16) for _ in range(2)]
        kh = [K.sb([128, 512], BF16) for _ in range(2)]
        dec = [K.sb([128, 4], F32) for _ in range(2)]
        S32 = K.sb([128, D], F32)
        Sb = K.sb([128, D], BF16)
        Sbn = K.sb([128, D], BF16)
        sm32 = K.sb([128, 512], F32)
        smb = [K.sb([128, 512], BF16) for _ in range(2)]
        Mf4 = K.sb([128, 512], F32)
        Mb4 = K.sb([128, 512], F32)
        ss = K.sb([128, 8], F32)
        junk = K.sb([128, 256], F32)
        gb = K.sb([128, D], BF16)
        gT = K.sb([128, 8, 128], BF16)
        pT = K.ps(1).bitcast(BF16)[:, 0:512].rearrange("p (a b) -> p a b", a=4)
        pA = K.ps(2)
        pB = K.ps(2)
        pQ = K.ps(1)
        pK = K.ps(1)
        pM = K.ps(1)

        load_w_cast(win, gla_w_in[i], "win", "wload")
        load_w_cast(wout, gla_w_out[i], "wout", "wload")
        K.dma("sp", wg2[0], gla_wg2_f[i], [], ["wg2"], "rows")
        K.dma("sp", wg2[1], gla_wg2_b[i], [], ["wg2"], "rows")
        K.dma("sp", bg[0], gla_bg_f[i:i + 1, :], [], ["wg2"], "rows")
        K.dma("sp", bg[1], gla_bg_b[i:i + 1, :], [], ["wg2"], "rows")
        for hh in range(4):
            K.dma("sp", ng4[:, hh * 256:(hh + 1) * 256], gla_norm_g[i:i + 1, :].partition_broadcast(128), [], ["ng4"], "bc")
            K.cp("dve", Mf4[:, hh * 128:(hh + 1) * 128], cm(c32, 5), ["c32"], ["Mf4"])
            K.cp("dve", Mb4[:, hh * 128:(hh + 1) * 128], cm(c32, 6), ["c32"], ["Mb4"])

        def softplus_neg(dirn):
            K.mm(pM, lr[0:16, dirn * 128:(dirn + 1) * 128], wg2[dirn], True, False, ["lr", "wg2"], ["pM"])
            K.mm(pM, ones_r32[0:1, 0:128], bg[dirn], False, True, ["ones_r32", "wg2"], ["pM"])
            K.act(ex, pM, AF.Exp, ["pM"], ["ex"], scale=-1.0)
            K.ts("dve", ex, ex, 1.0, None, ALU.add, None, ["ex"], ["ex"])
            K.act(spl[dirn], ex, AF.Ln, ["ex"], [("spl", dirn)])

        def proj_common(t, p):
            front(cm_, xsrc, t, hb)
            transpose8(hb, hT, pT, "hb", "hT")
            for c in range(2):
                for k in range(8):
                    K.mm(pA[:, c * 512:(c + 1) * 512], hT[:, k, :], win[:, k, 1024 + c * 512:1024 + (c + 1) * 512], k == 0, k == 7, ["hT", "win"], ["pA"])
            K.cp("act", vb[p], pA, ["pA"], [("vb", p)])
            for k in range(8):
                K.mm(pM, hT[:, k, :], win[:, k, 512:1024], k == 0, k == 7, ["hT", "win"], ["pM"])
            K.cp("dve", kt32, pM, ["pM"], ["kt32"])
            for dirn in range(2):
                for k in range(8):
                    K.mm(pM[0:16, dirn * 128:(dirn + 1) * 128], win[:, k, 3072 + dirn * 16:3072 + (dirn + 1) * 16], hT[:, k, :], k == 0, k == 7, ["hT", "win"], ["pM"])
            K.cp("dve", lr, pM[0:16, 0:256], ["pM"], ["lr"])

        def khat(dirn, Lmat, p):
            K.mm(pM, Lmat, spl[dirn], True, True, ["c32", ("spl", dirn)], ["pM"])
            K.act(Ed, pM, AF.Exp, ["pM"], ["Ed"])
            K.tt("dve", kh[p], kt32, Ed, ALU.mult, ["kt32", "Ed"], [("kh", p)])

        def state_update(p):
            for hh in range(4):
                K.mm(pB[:, hh * 256:(hh + 1) * 256], kh[p][:, hh * 128:(hh + 1) * 128], vb[p][:, hh * 256:(hh + 1) * 256], True, True, [("kh", p), ("vb", p)], ["pB"])
            for hh in range(4):
                K.stt("dve", S32[:, hh * 256:(hh + 1) * 256], S32[:, hh * 256:(hh + 1) * 256], dec[p][:, hh:hh + 1], pB[:, hh * 256:(hh + 1) * 256],
                      ALU.mult, ALU.add, ["S32", ("dec", p), "pB"], ["S32"])
            K.cp("act", Sb, S32, ["S32"], ["Sb"])

        def s1b(t, p):
            proj_common(t, p)
            softplus_neg(1)
            for hh in range(4):
                K.mm(pQ[:, hh:hh + 1], spl[1][:, hh * 128:(hh + 1) * 128], cm(c32, 1)[:, 127:128], True, True, [("spl", 1), "c32"], ["pQ"])
            K.act(dec[p], pQ[:, 0:4], AF.Exp, ["pQ"], [("dec", p)])
            khat(1, cm(c32, 4), p)

        def s2b(t, p):
            K.dma("sp", sbn[t], Sb, ["Sb"], [("sbn", t)], "sbn")
            state_update(p)

        K.memset("pool", S32, 0.0, ["S32"])
        K.memset("pool", Sb, 0.0, ["Sb"])
        order = list(range(NT - 1, -1, -1))
        if PIPE_B:
            s1b(order[0], 0)
            for n, t in enumerate(order):
                if n + 1 < NT:
                    s1b(order[n + 1], (n + 1) % 2)
                s2b(t, n % 2)
        else:
            for n, t in enumerate(order):
                s1b(t, n % 2)
                s2b(t, n % 2)

        def s1f(t, p):
            proj_common(t, p)
            for c in range(2):
                for k in range(8):
                    K.mm(pB[:, c * 512:(c + 1) * 512], hT[:, k, :], win[:, k, 2048 + c * 512:2048 + (c + 1) * 512], k == 0, k == 7, ["hT", "win"], ["pB"])
            K.act(rs[p], pB, AF.Silu, ["pB"], [("rs", p)])
            K.tt("pool", rs[p], rs[p], ng4, ALU.mult, [("rs", p), "ng4"], [("rs", p)])
            for hh in range(4):
                for k in range(8):
                    K.mm(pQ[:, hh * 128:(hh + 1) * 128], win[:, k, hh * 128:(hh + 1) * 128], hT[:, k, :], k == 0, k == 7, ["hT", "win"], ["pQ"])
            for hh in range(4):
                for k in range(8):
                    K.mm(pK[:, hh * 128:(hh + 1) * 128], win[:, k, 512 + hh * 128:512 + (hh + 1) * 128], hT[:, k, :], k == 0, k == 7, ["hT", "win"], ["pK"])
            softplus_neg(0)
            softplus_neg(1)
            for dirn in range(2):
                U = cm(c32, 1 + dirn)
                for hh in range(4):
                    K.mm(pM[:, hh * 128:(hh + 1) * 128], spl[dirn][:, hh * 128:(hh + 1) * 128], U, True, True, [("spl", dirn), "c32"], ["pM"])
                K.act(Eq[dirn], pM, AF.Exp, ["pM"], [("Eq", dirn)])
                K.act(Ek[dirn], pM, AF.Exp, ["pM"], [("Ek", dirn)], scale=-1.0)
                K.stt("dve", qT[p][dirn], pQ, 128.0 ** -0.5, Eq[dirn], ALU.mult, ALU.mult, ["pQ", ("Eq", dirn)], [("qT", p, dirn)])
                K.tt("dve", kT[dirn], pK, Ek[dirn], ALU.mult, ["pK", ("Ek", dirn)], [("kT", dirn)])
            for hh in range(4):
                K.mm(pM[:, hh * 128:(hh + 1) * 128], kT[0][:, hh * 128:(hh + 1) * 128], qT[p][0][:, hh * 128:(hh + 1) * 128], True, True, [("kT", 0), ("qT", p, 0)], ["pM"])
            K.tt("dve", sm32, pM, Mf4, ALU.mult, ["pM", "Mf4"], ["sm32"])
            for hh in range(4):
                K.mm(pM[:, hh * 128:(hh + 1) * 128], kT[1][:, hh * 128:(hh + 1) * 128], qT[p][1][:, hh * 128:(hh + 1) * 128], True, True, [("kT", 1), ("qT", p, 1)], ["pM"])
            K.tt("dve", ex, pM, Mb4, ALU.mult, ["pM", "Mb4"], ["ex"])
            K.tt("dve", smb[p], sm32, ex, ALU.add, ["sm32", "ex"], [("smb", p)])
            for hh in range(4):
                K.cp("dve", dec[p][:, hh:hh + 1], Eq[0][:, hh * 128 + 127:hh * 128 + 128], [("Eq", 0)], [("dec", p)])
            khat(0, cm(c32, 3), p)

        def s2f(t, p):
            K.dma("sp", Sbn, sbn[t], [("sbn", t)], ["Sbn"], "sbnl")
            for hh in range(4):
                o_ = pA[:, hh * 256:(hh + 1) * 256]
                K.mm(o_, smb[p][:, hh * 128:(hh + 1) * 128], vb[p][:, hh * 256:(hh + 1) * 256], True, False, [("smb", p), ("vb", p)], ["pA"])
                K.mm(o_, qT[p][0][:, hh * 128:(hh + 1) * 128], Sb[:, hh * 256:(hh + 1) * 256], False, False, [("qT", p, 0), "Sb"], ["pA"])
                K.mm(o_, qT[p][1][:, hh * 128:(hh + 1) * 128], Sbn[:, hh * 256:(hh + 1) * 256], False, True, [("qT", p, 1), "Sbn"], ["pA"])
            for hh in range(4):
                K.act(junk, pA[:, hh * 256:(hh + 1) * 256], AF.Square, ["pA"], ["junk", "ss"], accum=ss[:, hh:hh + 1])
            K.ts("dve", ss[:, 4:8], ss[:, 0:4], 1.0 / 256.0, EPS, ALU.mult, ALU.add, ["ss"], ["ss"])
            K.tt("pool", ss[:, 0:4], ss[:, 4:8], nhalf, ALU.pow, ["ss", "nhalf"], ["ss"])
            for hh in range(4):
                K.stt("dve", gb[:, hh * 256:(hh + 1) * 256], pA[:, hh * 256:(hh + 1) * 256], ss[:, hh:hh + 1], rs[p][:, hh * 256:(hh + 1) * 256],
                      ALU.mult, ALU.mult, ["pA", "ss", ("rs", p)], ["gb"])
            state_update(p)
            transpose8(gb, gT, pT, "gb", "gT")
            for c in range(2):
                for k in range(8):
                    K.mm(pB[:, c * 512:(c + 1) * 512], gT[:, k, :], wout[:, k, c * 512:(c + 1) * 512], k == 0, k == 7, ["gT", "wout"], ["pB"])
            back(cm_, xdst, t, pB, "pB")

        K.memset("pool", S32, 0.0, ["S32"])
        K.memset("pool", Sb, 0.0, ["Sb"])
        if PIPE_F:
            s1f(0, 0)
            for t in range(NT):
                if t + 1 < NT:
                    s1f(t + 1, (t + 1) % 2)
                s2f(t, t % 2)
        else:
            for t in range(NT):
                s1f(t, t % 2)
                s2f(t, t % 2)

    def phase_moe(l, xsrc, xdst):
        K.phase()
        cm_ = {}
        wr = K.sb([128, 8, NE], F32)
        br = K.sb([1, NE], F32)
        desti = K.sb([128, NT * 4], I32)
        dumpf = K.sb([128, 4], F32)
        ecb = K.sb([128, NE], F32)
        cnt = K.sb([128, NE], F32)
        K.dma("sp", wr, moe_w_router[l].rearrange("(k p) n -> p k n", p=128), [], ["wr"], "rows")
        K.dma("sp", br, moe_b_router[l:l + 1, :], [], ["wr"], "rows")
        K.dma("sp", dumpf, dumpidx_in, [], ["dumpf"], "rows")
        K.ts("dve", dumpf, dumpf, 1.0, None, ALU.add, None, ["dumpf"], ["dumpf"])
        K.dma("sp", ecb, ecrow_in.partition_broadcast(128), [], ["ecb"], "bc")
        K.memset("pool", cnt, 0.0, ["cnt"])
        mark = K.off
        alloc_bc(1, l, ("scale", "shift"), cm_)
        alloc_work(cm_)
        RT = []
        for p in range(2):
            r_ = {"p": p}
            r_["h32"] = K.sb([128, D], F32)
            r_["hb"] = K.sb([128, D], BF16)
            r_["hT32"] = K.sb([128, 8, 128], F32)
            for nm in ("lg", "rank", "over", "dfull", "tmp", "tmp2"):
                r_[nm] = K.sb([128, NE], F32)
            r_["oh"] = [K.sb([128, NE], F32) for _ in range(4)]
            r_["maskb"] = K.sb([128, NE], BF16)
            for nm in ("top8", "sm", "destf"):
                r_[nm] = K.sb([128, 8], F32)
            r_["w4"] = K.sb([128, 4], F32)
            r_["w4x"] = K.sb([128, 4, 8], F32)
            r_["pT32"] = K.ps(1).rearrange("p (a b) -> p a b", a=4)
            r_["pL"] = K.ps(1)
            r_["pR"] = K.ps(1)
            K.memset("pool", r_["w4x"], 0.0, [("w4x", p)])
            RT.append(r_)

        def route_tile(r_, t):
            p = r_["p"]
            T_ = lambda nm: (nm, p)
            lg, top8, sm, w4, w4x, oh, maskb = r_["lg"], r_["top8"], r_["sm"], r_["w4"], r_["w4x"], r_["oh"], r_["maskb"]
            rank, over, dfull, tmp, tmp2, destf, pL, pR = r_["rank"], r_["over"], r_["dfull"], r_["tmp"], r_["tmp2"], r_["destf"], r_["pL"], r_["pR"]
            hT32 = r_["hT32"]
            for half in range(2):
                for j in range(4):
                    k = half * 4 + j
                    K.tr(r_["pT32"][:, j, :], r_["h32"][:, k * 128:(k + 1) * 128], id32, [T_("h32"), "c32"], [T_("pT32")])
                K.cp("act", hT32[:, half * 4:(half + 1) * 4, :], r_["pT32"], [T_("pT32")], [T_("hT32")])
            for k in range(8):
                K.mm(pL[:, 0:NE], hT32[:, k, :], wr[:, k, :], k == 0, False, [T_("hT32"), "wr"], [T_("pL")])
            K.mm(pL[:, 0:NE], ones_r32[0:1, 0:128], br, False, True, ["ones_r32", "wr"], [T_("pL")])
            K.cp("dve", lg, pL[:, 0:NE], [T_("pL")], [T_("lg")])
            S.op("dve", lambda e: e.max(out=top8, in_=lg), [T_("lg")], [T_("top8")])
            K.ts("dve", sm[:, 0:1], top8[:, 0:1], -1.0, None, ALU.mult, None, [T_("top8")], [T_("sm")])
            K.act(w4, top8[:, 0:4], AF.Exp, [T_("top8"), T_("sm")], [T_("w4"), T_("sm")], bias=sm[:, 0:1], accum=sm[:, 1:2])
            S.op("dve", lambda e: e.reciprocal(out=sm[:, 2:3], in_=sm[:, 1:2]), [T_("sm")], [T_("sm")])
            K.ts("dve", w4x[:, :, 0:1], w4.rearrange("p (a b) -> p a b", b=1), sm[:, 2:3], None, ALU.mult, None, [T_("w4"), T_("sm")], [T_("w4x")])
            for k in range(4):
                K.ts("dve", oh[k], lg, top8[:, k:k + 1], None, ALU.is_equal, None, [T_("lg"), T_("top8")], [T_("oh%d" % k)])
            K.ts("dve", maskb, lg, top8[:, 3:4], None, ALU.is_ge, None, [T_("lg"), T_("top8")], [T_("maskb")])
            K.mm(pR[:, 0:NE], cm(cb, 8), maskb, True, True, ["cb", T_("maskb")], [T_("pR")])
            K.mm(pR[:, NE:2 * NE], cm(cb, 7), maskb, True, True, ["cb", T_("maskb")], [T_("pR")])
            K.tt("dve", rank, pR[:, 0:NE], cnt, ALU.add, [T_("pR"), "cnt"], [T_("rank")])
            K.tt("dve", cnt, pR[:, NE:2 * NE], cnt, ALU.add, [T_("pR"), "cnt"], ["cnt"])
            K.ts("dve", over, rank, float(C), -1.0, ALU.is_ge, ALU.add, [T_("rank")], [T_("over")])
            K.stt("dve", dfull, rank, 1.0, ecb, ALU.add, ALU.add, [T_("rank"), "ecb"], [T_("dfull")])
            K.stt("dve", dfull, dfull, -1.0, over, ALU.mult, ALU.mult, [T_("dfull"), T_("over")], [T_("dfull")])
            for k in range(4):
                K.tt("dve", tmp, oh[k], dfull, ALU.mult, [T_("oh%d" % k), T_("dfull")], [T_("tmp")])
                S.op("dve", lambda e, k=k: e.reduce_sum(out=destf[:, k:k + 1], in_=tmp, axis=AX.X), [T_("tmp")], [T_("destf")])
            K.ts("dve", destf[:, 4:8], destf[:, 0:4], 0.0, None, ALU.is_equal, None, [T_("destf")], [T_("destf")])
            K.tt("dve", destf[:, 4:8], destf[:, 4:8], dumpf, ALU.mult, [T_("destf"), "dumpf"], [T_("destf")])
            K.stt("dve", destf[:, 0:4], destf[:, 0:4], -1.0, destf[:, 4:8], ALU.add, ALU.add, [T_("destf")], [T_("destf")])
            K.cp("dve", desti[:, t * 4:(t + 1) * 4], destf[:, 0:4], [T_("destf")], [("desti", t)])
            for k in range(4):
                idx = desti[:, t * 4 + k:t * 4 + k + 1]
                S.op("pool", lambda e, idx=idx: e.indirect_dma_start(out=Xs, out_offset=bass.IndirectOffsetOnAxis(ap=idx, axis=0), in_=r_["hb"], in_offset=None),
                     [T_("hb"), ("desti", t)], [], dma_tag="scatx%d_%d" % (k, p))
                S.op("pool", lambda e, idx=idx, k=k: e.indirect_dma_start(out=Ws, out_offset=bass.IndirectOffsetOnAxis(ap=idx, axis=0), in_=w4x[:, k, :], in_offset=None),
                     [T_("w4x"), ("desti", t)], [], dma_tag="scatw%d_%d" % (k, p))

        def r_tile(t):
            r_ = RT[t % 2]
            p = t % 2
            b = t % 3
            K.dma("sp", cm_["xt"][b], xsrc[t * 128:(t + 1) * 128, :], ["xsrc"], [("xt", b)], "xt%d" % b)
            K.tt("dve", r_["h32"], cm_["xt"][b], cm_["scale"], ALU.mult, [("xt", b), "bc_scale"], [("h32", p)])
            K.tt("pool", r_["hb"], r_["h32"], cm_["shift"], ALU.add, [("h32", p), "bc_shift"], [("hb", p)])
            K.tt("dve", r_["h32"], r_["h32"], cm_["shift"], ALU.add, [("h32", p), "bc_shift"], [("h32", p)])
            route_tile(r_, t)

        for t0 in range(0, NT, 2):
            lists = []
            for t in range(t0, min(NT, t0 + 2)):
                S.capture()
                r_tile(t)
                lists.append(S.end_capture())
            S.replay(lists, 14)
        dbg("cnt%d" % l, cnt, "cnt")
        S.fence()
        K.off = mark
        K.pso = 0
        GS = 512 if C % 512 == 0 else 384
        NG = C // GS
        NB = GS // 128
        assert NG * GS == C
        bup = K.sb([128, NE, 16], F32)
        K.dma("sp", bup, moe_b_upT[l], [], ["bup"], "rows")
        K.ts("dve", bup[:, :, 8:16], bup[:, :, 8:16], 1.0, None, ALU.add, None, ["bup"], ["bup"])
        bup17 = K.sb([128, NE, 8], F32)
        K.ts("dve", bup17, bup[:, :, 0:8], 1.702, None, ALU.mult, None, ["bup"], ["bup17"])
        wup = [K.sb([128, 8, 2 * D], BF16) for _ in range(2)]
        wdn = [K.sb([128, 8, D], BF16) for _ in range(2)]
        NST = 3
        stage = [K.sb([128, 2 * D], F32) for _ in range(NST)]
        bd32 = [K.sb([1, D], F32) for _ in range(2)]
        bdb = [K.sb([1, D], BF16) for _ in range(2)]
        xs = [K.sb([128, D], BF16) for _ in range(2)]
        wsl = [K.sb([128, 8], F32) for _ in range(16)]
        xsT = [K.sb([128, 8, GS], BF16) for _ in range(2)]
        actT = [K.sb([128, 8, GS], BF16) for _ in range(2)]
        gt = [K.sb([128, GS], F32) for _ in range(2)]
        sg = [K.sb([128, GS], F32) for _ in range(2)]
        lA = [K.sb([128, GS], F32) for _ in range(2)]
        ys = [K.sb([128, 512], F32) for _ in range(3)]
        pTs = [K.ps(1).bitcast(BF16)[:, 0:512].rearrange("p (a b) -> p a b", a=4) for _ in range(2)]
        pG = [K.ps(1) for _ in range(2)]
        pLn = [K.ps(1) for _ in range(2)]
        pY = K.ps(2)
        cnts = {"ns": 0, "nx": 0, "nw": 0, "ny": 0, "nj": 0}

        def weight_steps(e_):
            be = e_ % 2
            steps = []
            for k in range(8):
                def f(k=k):
                    b = cnts["ns"] % NST
                    cnts["ns"] += 1
                    K.dma("sp", stage[b], moe_w_up[l, e_, k * 128:(k + 1) * 128, :], [], [("stage", b)], "stage%d" % b)
                    st_v = stage[b].rearrange("p (f two) -> p two f", two=2)
                    K.cp("act", wup[be][:, k, :].rearrange("p (two f) -> p two f", two=2), st_v, [("stage", b)], [("wup", be)])
                steps.append(f)
            for k2 in range(4):
                def f(k2=k2):
                    b = cnts["ns"] % NST
                    cnts["ns"] += 1
                    K.dma("sp", stage[b].rearrange("p (a n) -> p a n", a=2),
                          moe_w_down[l, e_, k2 * 256:(k2 + 1) * 256, :].rearrange("(a p) n -> p a n", p=128), [], [("stage", b)], "stage%d" % b)
                    K.cp("dve", wdn[be][:, 2 * k2:2 * k2 + 2, :], stage[b].rearrange("p (a n) -> p a n", a=2), [("stage", b)], [("wdn", be)])
                steps.append(f)

            def f():
                K.dma("sp", bd32[be], moe_b_down[l, e_:e_ + 1, :], [], [("bd32", be)], "bd32_%d" % be)
                K.cp("dve", bdb[be], bd32[be], [("bd32", be)], [("bdb", be)])
            steps.append(f)
            return steps

        units = [(e_, g) for e_ in range(NE) for g in range(NG)]
        wslots = {}

        def unit_A(u):
            e_, g = units[u]
            gb_ = u % 2
            base = e_ * C + g * GS
            wl = []
            for blk in range(NB):
                b = cnts["nx"] % 2
                cnts["nx"] += 1
                wi = cnts["nw"] % 16
                cnts["nw"] += 1
                wl.append(wi)
                r0 = base + blk * 128
                K.dma("sp", xs[b], Xs[r0:r0 + 128, :], ["Xs"], [("xs", b)], "xs%d" % b)
                K.dma("sp", wsl[wi], Ws[r0:r0 + 128, :], ["Ws"], [("wsl", wi)], "wsl%d" % wi)
                for half in range(2):
                    pT = pTs[half]
                    for j in range(4):
                        k = half * 4 + j
                        K.tr(pT[:, j, :], xs[b][:, k * 128:(k + 1) * 128], idb, [("xs", b), "cb"], [("pT", half)])
                    K.cp("act" if half == 0 else "dve", xsT[gb_][:, half * 4:(half + 1) * 4, blk * 128:(blk + 1) * 128], pT, [("pT", half)], [("xsT", gb_)])
            wslots[u] = wl

        def unit_U(u, pref):
            e_, g = units[u]
            be = e_ % 2
            gb_ = u % 2
            for j in range(8):
                jb = cnts["nj"] % 2
                cnts["nj"] += 1
                for k in range(8):
                    K.mm(pG[jb][:, 0:GS], wup[be][:, k, j * 128:(j + 1) * 128], xsT[gb_][:, k, :], k == 0, k == 7, [("wup", be), ("xsT", gb_)], [("pG", jb)])
                for k in range(8):
                    K.mm(pLn[jb][:, 0:GS], wup[be][:, k, D + j * 128:D + (j + 1) * 128], xsT[gb_][:, k, :], k == 0, k == 7, [("wup", be), ("xsT", gb_)], [("pLn", jb)])
                K.act(gt[jb], pG[jb][:, 0:GS], AF.Identity, [("pG", jb), "bup"], [("gt", jb)], bias=bup[:, e_, j:j + 1], scale=1.0)
                K.act(sg[jb], pG[jb][:, 0:GS], AF.Sigmoid, [("pG", jb), "bup17"], [("sg", jb)], bias=bup17[:, e_, j:j + 1], scale=1.702)
                K.act(lA[jb], pLn[jb][:, 0:GS], AF.Identity, [("pLn", jb), "bup"], [("lA", jb)], bias=bup[:, e_, 8 + j:9 + j], scale=1.0)
                K.ts("dve", lA[jb], lA[jb], -6.0, 8.0, ALU.max, ALU.min, [("lA", jb)], [("lA", jb)])
                K.stt("dve", gt[jb], gt[jb], 7.0, lA[jb], ALU.min, ALU.mult, [("gt", jb), ("lA", jb)], [("gt", jb)])
                K.stt("dve", actT[gb_][:, j, :], sg[jb], SIG7, gt[jb], ALU.min, ALU.mult, [("sg", jb), ("gt", jb)], [("actT", gb_)])
                if pref:
                    pref.pop(0)()
                    if len(pref) > 8 - j and j % 2 == 1:
                        pref.pop(0)()

        def unit_D(u):
            e_, g = units[u]
            be = e_ % 2
            gb_ = u % 2
            base = e_ * C + g * GS
            for blk in range(NB):
                wi = wslots[u][blk]
                r0 = base + blk * 128
                for c in range(2):
                    for k in range(8):
                        K.mm(pY[:, c * 512:(c + 1) * 512], actT[gb_][:, k, blk * 128:(blk + 1) * 128], wdn[be][:, k, c * 512:(c + 1) * 512], k == 0, False, [("actT", gb_), ("wdn", be)], [("pY", c)])
                    K.mm(pY[:, c * 512:(c + 1) * 512], ones_rb[0:1, 0:128], bdb[be][0:1, c * 512:(c + 1) * 512], False, True, ["ones_rb", ("bdb", be)], [("pY", c)])
                    b = cnts["ny"] % 3
                    cnts["ny"] += 1
                    K.act(ys[b], pY[:, c * 512:(c + 1) * 512], AF.Copy, [("pY", c), ("wsl", wi)], [("ys", b)], scale=wsl[wi][:, 0:1])
                    K.dma("act", Ys[r0:r0 + 128, c * 512:(c + 1) * 512], ys[b], [("ys", b)], [], "ys%d" % b)

        for f in weight_steps(0):
            f()
        unit_A(0)
        pref = []
        for u in range(len(units)):
            e_, g = units[u]
            if g == 0:
                for f in pref:
                    f()
                pref = weight_steps(e_ + 1) if e_ + 1 < NE else []
            if u + 1 < len(units):
                unit_A(u + 1)
            unit_U(u, pref)
            unit_D(u)
        for f in pref:
            f()
        S.fence()
        K.off = mark
        K.pso = 0
        alloc_bc(1, l, ("gate", "lng", "lnb"), cm_)
        alloc_work(cm_)
        yk = [[K.sb([128, D], F32) for _ in range(4)] for _ in range(2)]
        def g_tile(t):
            b = t % 2
            y_ = yk[b]
            bx = t % 3
            K.dma("sp", cm_["xt"][bx], xsrc[t * 128:(t + 1) * 128, :], ["xsrc"], [("xt", bx)], "xt%d" % bx)
            for k in range(4):
                idx = desti[:, t * 4 + k:t * 4 + k + 1]
                S.op("pool", lambda e, idx=idx, k=k, y_=y_: e.indirect_dma_start(out=y_[k], out_offset=None, in_=Ys, in_offset=bass.IndirectOffsetOnAxis(ap=idx, axis=0)),
                     ["Ys", ("desti", t)], [("yk", b, k)], dma_tag="gath%d_%d" % (k, b))
            K.tt("dve", y_[0], y_[0], y_[1], ALU.add, [("yk", b, 0), ("yk", b, 1)], [("yk", b, 0)])
            K.tt("pool", y_[2], y_[2], y_[3], ALU.add, [("yk", b, 2), ("yk", b, 3)], [("yk", b, 2)])
            K.tt("dve", y_[0], y_[0], y_[2], ALU.add, [("yk", b, 0), ("yk", b, 2)], [("yk", b, 0)])
            back(cm_, xdst, t, y_[0], ("yk", b, 0))

        for t0 in range(0, NT, 2):
            lists = []
            for t in range(t0, min(NT, t0 + 2)):
                S.capture()
                g_tile(t)
                lists.append(S.end_capture())
            S.replay(lists, 5)

    def phase_init():
        K.phase()
        zt = K.sb([128, 4096], F32)
        K.memset("pool", zt, 0.0, ["zt"])
        ztb = zt.bitcast(BF16)
        nrows = NSLOT + NDUMP
        assert nrows % 512 == 0 and (nrows // 128) * 8 <= 4096
        for r in range(0, nrows, 512):
            K.dma("sp", Xs[r:r + 512, :].rearrange("(p a) d -> p (a d)", p=128), ztb[:, 0:4 * D], ["zt"], [], "zinit%d" % ((r // 512) % 4))
        K.dma("sp", Ws.rearrange("(p a) d -> p (a d)", p=128), zt[:, 0:(nrows // 128) * 8], ["zt"], ["Ws"], "zinit")
        K.dma("sp", Ys[NSLOT:NSLOT + NDUMP, :].rearrange("(p a) d -> p (a d)", p=128), zt[:, 0:4 * D], ["zt"], ["Ys"], "zinit")

    phase_mod()
    if do_moe:
        phase_init()
    cur = x_in
    bufs = [xa, xb_]
    nb = 0
    stages = []
    for l in layer_list:
        if do_mixer:
            stages.append(("mix", l))
        if do_moe:
            stages.append(("moe", l))
    for si, (kind, l) in enumerate(stages):
        dst = out if si == len(stages) - 1 else bufs[nb % 2]
        nb += 1
        if kind == "mix":
            if l % 2 == 0:
                phase_gla(l, cur, dst)
            else:
                phase_sgu(l, cur, dst)
        else:
            phase_moe(l, cur, dst)
        cur = dst
    cnt = S.emit()
    return nc, cnt


def prep_shared(inputs, C):
    f = lambda a: np.ascontiguousarray(np.asarray(a, dtype=np.float32))
    sh = {}
    for k_ in ("ada_w", "ada_b", "ln_g", "ln_b", "gla_w_in", "gla_wg2_f", "gla_bg_f", "gla_wg2_b", "gla_bg_b", "gla_norm_g", "gla_w_out",
               "sgu_w_in", "sgu_b_in", "sgu_ln_g", "sgu_ln_b", "sgu_w_out", "sgu_b_out", "moe_w_router", "moe_b_router", "moe_w_up",
               "moe_w_down", "moe_b_down"):
        sh[k_] = f(inputs[k_])
    sh["sgu_w_sT"] = f(np.transpose(np.asarray(inputs["sgu_w_s"]), (0, 3, 1, 2)))
    sh["sgu_b_sT"] = f(np.transpose(np.asarray(inputs["sgu_b_s"]), (0, 2, 1)))
    bu = np.asarray(inputs["moe_b_up"], dtype=np.float32)
    bg = bu[:, :, 0::2].reshape(4, NE, 8, 128)
    bl = bu[:, :, 1::2].reshape(4, NE, 8, 128)
    sh["moe_b_upT"] = f(np.transpose(np.concatenate([bg, bl], axis=2), (0, 3, 1, 2)))
    sh["consts"] = make_consts()
    sh["dumpidx"] = (NE * C + np.arange(4)[None, :] * 128 + np.arange(128)[:, None]).astype(np.float32)
    sh["ecrow"] = (np.arange(NE, dtype=np.float32) * C)[None, :]
    return sh


CAP = 1024
_cache = {}


def kernel(**inputs):
    x = np.asarray(inputs["x"], dtype=np.float32)
    c = np.asarray(inputs["c"], dtype=np.float32)
    B, T, _ = x.shape
    key = (T, CAP)
    if key not in _cache:
        _cache[key] = build_program(T, CAP)[0]
    nc = _cache[key]
    sh = prep_shared(inputs, CAP)
    in_maps = []
    for b in range(B):
        m = dict(sh)
        m["x"] = np.ascontiguousarray(x[b])
        m["cT"] = np.ascontiguousarray(c[b].reshape(8, 128).T)
        in_maps.append(m)
    res = run_bass_kernel_spmd(nc, in_maps, core_ids=list(range(B)))
    return np.stack([np.asarray(r["out"], dtype=np.float32) for r in res.results], axis=0)
```

```python
import numpy as np
from contextlib import ExitStack
import concourse.bass as bass
import concourse.mybir as mybir
from concourse.bass_utils import run_bass_kernel_spmd

F32 = mybir.dt.float32
BF16 = mybir.dt.bfloat16
I32 = mybir.dt.int32
AF = mybir.ActivationFunctionType
ALU = mybir.AluOpType
AX = mybir.AxisListType

D = 1024
DEPTH = 4
ALPHA = (2.0 * DEPTH) ** 0.25
EPS = 1e-5
NE = 32
GIN = 3104
NDUMP = 512
SIG7 = float(1.0 / (1.0 + np.exp(-1.702 * 7.0)))
DEBUG = False
PIPE_B = False
PIPE_F = True


class _Op:
    __slots__ = ("eng", "fn", "deps", "is_dma", "tag", "val", "signal", "idx")


class Sched:
    ENGS = ("pe", "dve", "act", "pool", "sp")

    def __init__(self, nc, same_engine_sync=True):
        self.nc = nc
        self.ops = []
        self.last_w = {}
        self.readers = {}
        self.same_engine_sync = same_engine_sync
        self.dma_count = {}
        self.last_dma = {}
        self._cap = None

    def capture(self):
        self._cap = []

    def end_capture(self):
        c, self._cap = self._cap, None
        return c

    def replay(self, lists, lag):
        idx = [0] * len(lists)
        step = 0
        while True:
            done = True
            for i, L in enumerate(lists):
                if idx[i] < len(L):
                    done = False
                    if step >= i * lag:
                        self.op(*L[idx[i]])
                        idx[i] += 1
            if done:
                break
            step += 1

    def op(self, eng, fn, reads=(), writes=(), dma_tag=None):
        if self._cap is not None:
            self._cap.append((eng, fn, tuple(reads), tuple(writes), dma_tag))
            return None
        o = _Op()
        o.eng = eng
        o.fn = fn
        o.is_dma = dma_tag is not None
        o.tag = dma_tag
        o.signal = False
        o.idx = len(self.ops)
        deps = {}
        for t in reads:
            w = self.last_w.get(t)
            if w is not None:
                deps[w.idx] = w
        for t in writes:
            w = self.last_w.get(t)
            if w is not None:
                deps[w.idx] = w
            for r in self.readers.get(t, ()):
                deps[r.idx] = r
        if o.is_dma:
            p = self.last_dma.get(dma_tag)
            if p is not None:
                deps[p.idx] = p
            self.last_dma[dma_tag] = o
        o.deps = list(deps.values())
        for t in writes:
            self.last_w[t] = o
            self.readers[t] = []
        for t in reads:
            lst = self.readers.setdefault(t, [])
            key = (o.eng, o.tag)
            lst[:] = [r for r in lst if (r.eng, r.tag) != key]
            lst.append(o)
        if o.is_dma:
            c = self.dma_count.get(dma_tag, 0) + 1
            self.dma_count[dma_tag] = c
            o.val = 16 * c
        self.ops.append(o)
        return o

    def fence(self):
        last = {}
        for o in self.ops:
            if o.fn is None:
                continue
            last[(o.eng, o.tag)] = o
        deps = list(last.values())
        for e in self.ENGS:
            o = _Op()
            o.eng = e
            o.fn = None
            o.is_dma = False
            o.tag = None
            o.signal = False
            o.idx = len(self.ops)
            o.deps = list(deps)
            self.ops.append(o)
        self.last_w = {}
        self.readers = {}

    def dma(self, eng, out, in_, reads, writes, tag, **kw):
        return self.op(eng, lambda e: e.dma_start(out=out, in_=in_, **kw), reads, writes, dma_tag=tag)

    def _needs_wait(self, o, d):
        if d.is_dma:
            return True
        if d.eng == o.eng and not o.is_dma:
            if o.eng == "pe":
                return False
            return self.same_engine_sync
        return True

    def emit(self):
        nc = self.nc
        for o in self.ops:
            for d in o.deps:
                if self._needs_wait(o, d) and not d.is_dma:
                    d.signal = True
        cnt = {e: 0 for e in self.ENGS}
        for o in self.ops:
            if not o.is_dma and o.signal:
                cnt[o.eng] += 1
                o.val = cnt[o.eng]
        streams = {e: [] for e in self.ENGS}
        for o in self.ops:
            streams[o.eng].append(o)
        with ExitStack() as es:
            esem = {e: es.enter_context(nc.semaphore("s_" + e)) for e in self.ENGS}
            dsem = {t: es.enter_context(nc.semaphore("d_" + str(t))) for t in self.dma_count}
            block = es.enter_context(nc.Block())

            def run(ename, eng):
                known = {}
                for o in streams[ename]:
                    for d in o.deps:
                        if not self._needs_wait(o, d):
                            continue
                        if d.is_dma:
                            s, v, k = dsem[d.tag], d.val, ("d", d.tag)
                        else:
                            s, v, k = esem[d.eng], d.val, ("e", d.eng)
                        if known.get(k, 0) >= v:
                            continue
                        known[k] = v
                        eng.wait_ge(s, v)
                    if o.fn is None:
                        continue
                    ins = o.fn(eng)
                    if o.is_dma:
                        ins.then_inc(dsem[o.tag], 16)
                    elif o.signal:
                        ins.then_inc(esem[ename], 1)
                if ename == "sp":
                    for t, c in self.dma_count.items():
                        eng.wait_ge(dsem[t], 16 * c)

            @block.tensor
            def _(e):
                run("pe", e)

            @block.vector
            def _(e):
                run("dve", e)

            @block.scalar
            def _(e):
                run("act", e)

            @block.gpsimd
            def _(e):
                run("pool", e)

            @block.sync
            def _(e):
                run("sp", e)
        return cnt


SB_BYTES = 211968


class KB:
    def __init__(self, nc):
        self.nc = nc
        self.S = Sched(nc)
        self.SB = nc.alloc_sbuf_tensor("SB", [128, SB_BYTES // 4], F32).ap()
        self.PS = nc.alloc_psum_tensor("PS", [128, 4096], F32).ap()
        self.off = 0
        self.base = 0
        self.pso = 0
        self.uid = 0

    def persist(self):
        self.base = self.off

    def phase(self):
        self.S.fence()
        self.off = self.base
        self.pso = 0

    def sb(self, shape, dt, np_=128):
        n = int(np.prod(shape[1:]))
        bpe = 2 if dt == BF16 else 4
        nb = (n * bpe + 63) // 64 * 64
        assert self.off + nb <= SB_BYTES, ("SBUF overflow", self.off, nb)
        a = self.SB[0:shape[0], self.off // 4:(self.off + nb) // 4]
        self.off += nb
        if dt != F32:
            a = a.bitcast(dt)
        a = a[:, 0:n]
        if len(shape) == 3:
            a = a.rearrange("p (a b) -> p a b", a=shape[1])
        return a

    def ps(self, nbanks=1):
        assert self.pso + nbanks <= 8
        a = self.PS[:, self.pso * 512:(self.pso + nbanks) * 512]
        self.pso += nbanks
        return a

    def mm(self, out, lhsT, rhs, start, stop, r, w):
        self.S.op("pe", lambda e: e.matmul(out, lhsT=lhsT, rhs=rhs, start=start, stop=stop), r, w)

    def tr(self, out, in_, ident, r, w):
        self.S.op("pe", lambda e: e.transpose(out=out, in_=in_, identity=ident), r, w)

    def tt(self, eng, out, in0, in1, op, r, w):
        self.S.op(eng, lambda e: e.tensor_tensor(out=out, in0=in0, in1=in1, op=op), r, w)

    def ts(self, eng, out, in0, s1, s2, op0, op1, r, w):
        if op1 is None:
            self.S.op(eng, lambda e: e.tensor_scalar(out=out, in0=in0, scalar1=s1, scalar2=None, op0=op0), r, w)
        else:
            self.S.op(eng, lambda e: e.tensor_scalar(out=out, in0=in0, scalar1=s1, scalar2=s2, op0=op0, op1=op1), r, w)

    def stt(self, eng, out, in0, sc, in1, op0, op1, r, w):
        self.S.op(eng, lambda e: e.scalar_tensor_tensor(out=out, in0=in0, scalar=sc, in1=in1, op0=op0, op1=op1), r, w)

    def cp(self, eng, out, in_, r, w):
        if eng == "act":
            self.S.op(eng, lambda e: e.copy(out=out, in_=in_), r, w)
        else:
            self.S.op(eng, lambda e: e.tensor_copy(out=out, in_=in_), r, w)

    def act(self, out, in_, func, r, w, bias=None, scale=None, accum=None):
        kw = {}
        if bias is not None:
            kw["bias"] = bias
        if scale is not None:
            kw["scale"] = scale
        if accum is not None:
            kw["accum_out"] = accum
        self.S.op("act", lambda e: e.activation(out=out, in_=in_, func=func, **kw), r, w)

    def memset(self, eng, ap, v, w):
        self.S.op(eng, lambda e: e.memset(ap, v), [], w)

    def dma(self, eng, out, in_, r, w, tag, **kw):
        self.S.dma(eng, out, in_, r, w, tag, **kw)


def make_consts():
    j = np.arange(128)[:, None]
    i = np.arange(128)[None, :]
    c = -1.0 / 16.0
    mats = [
        np.eye(128),
        (j <= i) * c,
        (j >= i) * c,
        (j > i) * c,
        (j < i) * c,
        (j <= i) * 1.0,
        (j >= i) * 1.0,
        np.ones((128, 128)),
        (j < i) * 1.0,
    ]
    return np.concatenate(mats, axis=1).astype(np.float32)


NCONST = 9


def build_program(T, C, layer_list=(0, 1, 2, 3), do_mixer=True, do_moe=True):
    NT = T // 128
    NSLOT = NE * C
    nc = bass.Bass("TRN2", target_bir_lowering=False)
    dt_in = lambda name, shape, dt=F32: nc.dram_tensor(name, list(shape), dt, kind="ExternalInput").ap()
    dt_int = lambda name, shape, dt=F32: nc.dram_tensor(name, list(shape), dt, kind="Internal").ap()
    x_in = dt_in("x", [T, D])
    cT_in = dt_in("cT", [128, 8])
    ada_w = dt_in("ada_w", [4, D, 6 * D])
    ada_b = dt_in("ada_b", [4, 6 * D])
    ln_g = dt_in("ln_g", [4, 2, D])
    ln_b = dt_in("ln_b", [4, 2, D])
    gla_w_in = dt_in("gla_w_in", [2, D, GIN])
    gla_wg2_f = dt_in("gla_wg2_f", [2, 16, 512])
    gla_bg_f = dt_in("gla_bg_f", [2, 512])
    gla_wg2_b = dt_in("gla_wg2_b", [2, 16, 512])
    gla_bg_b = dt_in("gla_bg_b", [2, 512])
    gla_norm_g = dt_in("gla_norm_g", [2, 256])
    gla_w_out = dt_in("gla_w_out", [2, D, D])
    sgu_w_in = dt_in("sgu_w_in", [2, D, 2 * D])
    sgu_b_in = dt_in("sgu_b_in", [2, 2 * D])
    sgu_ln_g = dt_in("sgu_ln_g", [2, D])
    sgu_ln_b = dt_in("sgu_ln_b", [2, D])
    sgu_w_sT = dt_in("sgu_w_sT", [2, 128, 8, 128])
    sgu_b_sT = dt_in("sgu_b_sT", [2, 128, 8])
    sgu_w_out = dt_in("sgu_w_out", [2, D, D])
    sgu_b_out = dt_in("sgu_b_out", [2, D])
    moe_w_router = dt_in("moe_w_router", [4, D, NE])
    moe_b_router = dt_in("moe_b_router", [4, NE])
    moe_w_up = dt_in("moe_w_up", [4, NE, D, 2 * D])
    moe_b_upT = dt_in("moe_b_upT", [4, 128, NE, 16])
    moe_w_down = dt_in("moe_w_down", [4, NE, D, D])
    moe_b_down = dt_in("moe_b_down", [4, NE, D])
    consts_in = dt_in("consts", [128, NCONST * 128])
    dumpidx_in = dt_in("dumpidx", [128, 4])
    ecrow_in = dt_in("ecrow", [1, NE])
    out = nc.dram_tensor("out", [T, D], F32, kind="ExternalOutput").ap()

    xa = dt_int("xa", [T, D])
    xb_ = dt_int("xb", [T, D])
    modd = dt_int("modd", [4, 6 * D])
    sbn = dt_int("sbn", [NT, 128, 1024], BF16)
    Xs = dt_int("Xs", [NSLOT + NDUMP, D], BF16)
    Ws = dt_int("Ws", [NSLOT + NDUMP, 8])
    Ys = dt_int("Ys", [NSLOT + NDUMP, D])

    K = KB(nc)
    S = K.S
    dbg_outs = {}

    def dbg(name, ap, tok, dt=F32):
        if not DEBUG:
            return
        shape = [ap.shape[0], int(np.prod(ap.shape[1:]))]
        d_ = nc.dram_tensor("dbg_" + name, shape, dt, kind="ExternalOutput").ap()
        src = ap if len(ap.shape) == 2 else ap.rearrange("p a b -> p (a b)")
        K.dma("sp", d_, src, [tok], [], "dbg_" + name)
        dbg_outs[name] = d_

    c32 = K.sb([128, NCONST * 128], F32)
    cb = K.sb([128, NCONST * 128], BF16)
    ones_r32 = K.sb([1, 512], F32)
    ones_rb = K.sb([1, 512], BF16)
    nhalf = K.sb([128, 4], F32)
    K.dma("sp", c32, consts_in, [], ["c32"], "c32")
    K.cp("dve", cb, c32, ["c32"], ["cb"])
    K.memset("pool", ones_r32, 1.0, ["ones_r32"])
    K.cp("dve", ones_rb, ones_r32, ["ones_r32"], ["ones_rb"])
    K.memset("pool", nhalf, -0.5, ["nhalf"])
    cm = lambda t, i: t[:, i * 128:(i + 1) * 128]
    id32, idb = cm(c32, 0), cm(cb, 0)
    CT = ["c32", "cb", "ones_r32", "ones_rb", "nhalf"]
    K.persist()

    def phase_mod():
        K.phase()
        cT = K.sb([128, 8], F32)
        sc = K.sb([128, 8], F32)
        wt = [K.sb([128, 8, 512], F32) for _ in range(2)]
        ab = K.sb([1, 6 * D], F32)
        mrow = K.sb([1, 6 * D], F32)
        pm = K.ps(1)
        K.dma("sp", cT, cT_in, [], ["cT"], "cT")
        K.act(sc, cT, AF.Silu, ["cT"], ["sc"])
        n = 0
        for l in layer_list:
            K.dma("sp", ab, ada_b[l:l + 1, :], [], ["ab"], "ab")
            for j in range(12):
                b = n % 2
                n += 1
                K.dma("sp", wt[b], ada_w[l][:, j * 512:(j + 1) * 512].rearrange("(k p) n -> p k n", p=128), [], [("wt", b)], "wt%d" % b)
                for k in range(8):
                    K.mm(pm[0:1, :], sc[:, k:k + 1], wt[b][:, k, :], k == 0, k == 7, ["sc", ("wt", b)], ["pm"])
                plus = 1.0 if (j // 2) in (1, 2, 4, 5) else 0.0
                K.stt("dve", mrow[:, j * 512:(j + 1) * 512], pm[0:1, :], plus, ab[:, j * 512:(j + 1) * 512], ALU.add, ALU.add, ["pm", "ab"], ["mrow"])
            K.dma("sp", modd[l:l + 1, :], mrow, ["mrow"], ["modd"], "mrow")

    def load_bc(dst, row, tok):
        K.dma("sp", dst, row.partition_broadcast(128), ["modd"], [tok], "bc")

    def alloc_bc(sub, l, names=("scale", "shift", "gate", "lng", "lnb"), t=None):
        t = {} if t is None else t
        o = sub * 3 * D
        srcs = {"shift": modd[l:l + 1, o:o + D], "scale": modd[l:l + 1, o + D:o + 2 * D], "gate": modd[l:l + 1, o + 2 * D:o + 3 * D],
                "lng": ln_g[l, sub:sub + 1, :], "lnb": ln_b[l, sub:sub + 1, :]}
        for nm in names:
            t[nm] = K.sb([128, D], F32)
            load_bc(t[nm], srcs[nm], "bc_" + nm)
        return t

    def alloc_work(t):
        t["xt"] = [K.sb([128, D], F32) for _ in range(3)]
        t["h32"] = K.sb([128, D], F32)
        t["z"] = [K.sb([128, D], F32) for _ in range(2)]
        t["xo"] = [K.sb([128, D], F32) for _ in range(2)]
        t["st"] = [K.sb([128, 2, 6], F32) for _ in range(2)]
        t["mv"] = [K.sb([128, 8], F32) for _ in range(2)]
        return t

    def alloc_common(sub, l):
        return alloc_work(alloc_bc(sub, l))

    BCT = ["bc_shift", "bc_scale", "bc_gate", "bc_lng", "bc_lnb"]

    def front(cm_, xsrc, t, hb):
        b = t % 3
        K.dma("sp", cm_["xt"][b], xsrc[t * 128:(t + 1) * 128, :], ["xsrc"], [("xt", b)], "xt%d" % b)
        K.tt("dve", cm_["h32"], cm_["xt"][b], cm_["scale"], ALU.mult, [("xt", b), "bc_scale"], ["h32"])
        if hb is not None:
            K.tt("dve", hb, cm_["h32"], cm_["shift"], ALU.add, ["h32", "bc_shift"], ["hb"])
        else:
            K.tt("dve", cm_["h32"], cm_["h32"], cm_["shift"], ALU.add, ["h32", "bc_shift"], ["h32"])

    def back(cm_, xdst, t, y_ap, ytok):
        b = t % 2
        bx = t % 3
        z, mv, st = cm_["z"][b], cm_["mv"][b], cm_["st"][b]
        Z, MV, ST = ("z", b), ("mv", b), ("st", b)
        K.tt("dve", z, y_ap, cm_["gate"], ALU.mult, [ytok, "bc_gate"], [Z])
        K.stt("dve", z, cm_["xt"][bx], ALPHA, z, ALU.mult, ALU.add, [("xt", bx), Z], [Z])
        for c in range(2):
            S.op("dve", lambda e, c=c: e.bn_stats(out=st[:, c, :], in_=z[:, c * 512:(c + 1) * 512]), [Z], [ST])
        S.op("dve", lambda e: e.bn_aggr(out=mv[:, 0:2], in_=st.rearrange("p a b -> p (a b)")), [ST], [MV])
        K.ts("dve", mv[:, 2:3], mv[:, 1:2], EPS, None, ALU.add, None, [MV], [MV])
        K.tt("pool", mv[:, 3:4], mv[:, 2:3], nhalf[:, 0:1], ALU.pow, [MV, "nhalf"], [MV])
        K.stt("dve", mv[:, 4:5], mv[:, 0:1], -1.0, mv[:, 3:4], ALU.mult, ALU.mult, [MV], [MV])
        K.act(z, z, AF.Identity, [Z, MV], [Z], bias=mv[:, 4:5], scale=mv[:, 3:4])
        K.tt("dve", z, z, cm_["lng"], ALU.mult, [Z, "bc_lng"], [Z])
        K.tt("pool", cm_["xo"][b], z, cm_["lnb"], ALU.add, [Z, "bc_lnb"], [("xo", b)])
        K.dma("sp", xdst[t * 128:(t + 1) * 128, :], cm_["xo"][b], [("xo", b)], [], "xo%d" % b)

    def transpose8(src_b, dstT, pT, srctok, dsttok, ident=None, eng="act", ptok="pT"):
        ident = idb if ident is None else ident
        for half in range(2):
            for j in range(4):
                k = half * 4 + j
                K.tr(pT[:, j, :], src_b[:, k * 128:(k + 1) * 128], ident, [srctok, "cb", "c32"], [ptok])
            K.cp(eng, dstT[:, half * 4:(half + 1) * 4, :], pT, [ptok], [dsttok])

    def load_w_cast(dst, src, tok, tag):
        N = src.shape[-1]
        c0 = 0
        while c0 < N:
            c1 = min(N, c0 + 2048)
            K.dma("pool", dst[:, :, c0:c1], src[:, c0:c1].rearrange("(k p) n -> p k n", p=128), [], [tok], tag)
            c0 = c1

    def load_row_bf16(dst_b, tmp32, src_row, tok):
        K.dma("sp", tmp32, src_row, [], [tok + "_32"], "rows")
        K.cp("dve", dst_b, tmp32, [tok + "_32"], [tok])

    def phase_sgu(l, xsrc, xdst):
        i = l // 2
        K.phase()
        cm_ = alloc_common(0, l)
        win = K.sb([128, 8, 2 * D], BF16)
        wout = K.sb([128, 8, D], BF16)
        wsT = K.sb([128, 8, 128], BF16)
        ws32 = K.sb([128, 8, 128], F32)
        bsT = K.sb([128, 8], F32)
        r32 = K.sb([1, 2 * D], F32)
        r32b = K.sb([1, D], F32)
        binr = K.sb([1, 2 * D], BF16)
        boutr = K.sb([1, D], BF16)
        slg = K.sb([128, D], F32)
        slb = K.sb([128, D], F32)
        hb = K.sb([128, D], BF16)
        hT = K.sb([128, 8, 128], BF16)
        u = [K.sb([128, D], F32) for _ in range(2)]
        v = K.sb([128, D], F32)
        vb = [K.sb([128, D], BF16) for _ in range(2)]
        gb = K.sb([128, D], BF16)
        gT = K.sb([128, 8, 128], BF16)
        st2 = K.sb([128, 2, 6], F32)
        mv2 = K.sb([128, 8], F32)
        pT = K.ps(1).bitcast(BF16)[:, 0:512].rearrange("p (a b) -> p a b", a=4)
        pT2 = K.ps(1).bitcast(BF16)[:, 0:512].rearrange("p (a b) -> p a b", a=4)
        pz = K.ps(2)
        psv = K.ps(2)
        py = K.ps(2)
        load_w_cast(win, sgu_w_in[i], "win", "wload")
        load_w_cast(wout, sgu_w_out[i], "wout", "wload")
        K.dma("sp", ws32, sgu_w_sT[i], [], ["ws32"], "rows")
        K.cp("dve", wsT, ws32, ["ws32"], ["wsT"])
        K.dma("sp", bsT, sgu_b_sT[i], [], ["bsT"], "rows")
        load_row_bf16(binr, r32, sgu_b_in[i:i + 1, :], "binr")
        load_row_bf16(boutr, r32b, sgu_b_out[i:i + 1, :], "boutr")
        K.dma("sp", slg, sgu_ln_g[i:i + 1, :].partition_broadcast(128), [], ["slg"], "bc")
        K.dma("sp", slb, sgu_ln_b[i:i + 1, :].partition_broadcast(128), [], ["slb"], "bc")

        def s1(t):
            p = t % 2
            front(cm_, xsrc, t, hb)
            transpose8(hb, hT, pT, "hb", "hT", ptok="pT")
            for part in range(2):
                dst = u[p] if part == 0 else v
                for c in range(2):
                    col = part * D + c * 512
                    for k in range(8):
                        K.mm(pz[:, c * 512:(c + 1) * 512], hT[:, k, :], win[:, k, col:col + 512], k == 0, False, ["hT", "win"], ["pz"])
                    K.mm(pz[:, c * 512:(c + 1) * 512], ones_rb[0:1, 0:128], binr[0:1, col:col + 512], False, True, ["ones_rb", "binr"], ["pz"])
                K.act(dst, pz, AF.Gelu, ["pz"], [("u", p) if part == 0 else "v"])
            for c in range(2):
                S.op("dve", lambda e, c=c: e.bn_stats(out=st2[:, c, :], in_=v[:, c * 512:(c + 1) * 512]), ["v"], ["st2"])
            S.op("dve", lambda e: e.bn_aggr(out=mv2[:, 0:2], in_=st2.rearrange("p a b -> p (a b)")), ["st2"], ["mv2"])
            K.ts("dve", mv2[:, 2:3], mv2[:, 1:2], EPS, None, ALU.add, None, ["mv2"], ["mv2"])
            K.tt("pool", mv2[:, 3:4], mv2[:, 2:3], nhalf[:, 0:1], ALU.pow, ["mv2", "nhalf"], ["mv2"])
            K.stt("dve", mv2[:, 4:5], mv2[:, 0:1], -1.0, mv2[:, 3:4], ALU.mult, ALU.mult, ["mv2"], ["mv2"])
            K.act(v, v, AF.Identity, ["v", "mv2"], ["v"], bias=mv2[:, 4:5], scale=mv2[:, 3:4])
            K.tt("dve", v, v, slg, ALU.mult, ["v", "slg"], ["v"])
            K.tt("pool", vb[p], v, slb, ALU.add, ["v", "slb"], [("vb", p)])

        def s2(t):
            p = t % 2
            for g in range(8):
                K.mm(psv[:, g * 128:(g + 1) * 128], wsT[:, g, :], vb[p][:, g * 128:(g + 1) * 128], True, True, ["wsT", ("vb", p)], ["psv"])
            for g in range(8):
                K.stt("dve", gb[:, g * 128:(g + 1) * 128], psv[:, g * 128:(g + 1) * 128], bsT[:, g:g + 1], u[p][:, g * 128:(g + 1) * 128],
                      ALU.add, ALU.mult, ["psv", "bsT", ("u", p)], ["gb"])
            transpose8(gb, gT, pT2, "gb", "gT", ptok="pT2")
            for c in range(2):
                for k in range(8):
                    K.mm(py[:, c * 512:(c + 1) * 512], gT[:, k, :], wout[:, k, c * 512:(c + 1) * 512], k == 0, False, ["gT", "wout"], ["py"])
                K.mm(py[:, c * 512:(c + 1) * 512], ones_rb[0:1, 0:128], boutr[0:1, c * 512:(c + 1) * 512], False, True, ["ones_rb", "boutr"], ["py"])
            back(cm_, xdst, t, py, "py")

        s1(0)
        for t in range(NT):
            lists = []
            if t + 1 < NT:
                S.capture()
                s1(t + 1)
                lists.append(S.end_capture())
            S.capture()
            s2(t)
            lists.append(S.end_capture())
            S.replay(lists, 0)

    def phase_gla(l, xsrc, xdst):
        i = l // 2
        K.phase()
        cm_ = alloc_common(0, l)
        win = K.sb([128, 8, GIN], BF16)
        wout = K.sb([128, 8, D], BF16)
        wg2 = [K.sb([16, 512], F32) for _ in range(2)]
        bg = [K.sb([1, 512], F32) for _ in range(2)]
        ng4 = K.sb([128, D], F32)
        hb = K.sb([128, D], BF16)
        hT = K.sb([128, 8, 128], BF16)
        vb = [K.sb([128, D], BF16) for _ in range(2)]
        kt32 = K.sb([128, 512], F32)
        lr = K.sb([16, 256], F32)
        rs = [K.sb([128, D], F32) for _ in range(2)]
        ex = K.sb([128, 512], F32)
        spl = [K.sb([128, 512], F32) for _ in range(2)]
        Eq = [K.sb([128, 512], F32) for _ in range(2)]
        Ek = [K.sb([128, 512], F32) for _ in range(2)]
        Ed = K.sb([128, 512], F32)
        qT = [[K.sb([128, 512], BF16) for _ in range(2)] for _ in range(2)]
        kT = [K.sb([128, 512], BF16) for _ in range(2)]
        kh = [K.sb([128, 512], BF16) for _ in range(2)]
        dec = [K.sb([128, 4], F32) for _ in range(2)]
        S32 = K.sb([128, D], F32)
        Sb = K.sb([128, D], BF16)
        Sbn = K.sb([128, D], BF16)
        sm32 = K.sb([128, 512], F32)
        smb = [K.sb([128, 512], BF16) for _ in range(2)]
        Mf4 = K.sb([128, 512], F32)
        Mb4 = K.sb([128, 512], F32)
        ss = K.sb([128, 8], F32)
        junk = K.sb([128, 256], F32)
        gb = K.sb([128, D], BF16)
        gT = K.sb([128, 8, 128], BF16)
        pT = K.ps(1).bitcast(BF16)[:, 0:512].rearrange("p (a b) -> p a b", a=4)
        pA = K.ps(2)
        pB = K.ps(2)
        pQ = K.ps(1)
        pK = K.ps(1)
        pM = K.ps(1)

        load_w_cast(win, gla_w_in[i], "win", "wload")
        load_w_cast(wout, gla_w_out[i], "wout", "wload")
        K.dma("sp", wg2[0], gla_wg2_f[i], [], ["wg2"], "rows")
        K.dma("sp", wg2[1], gla_wg2_b[i], [], ["wg2"], "rows")
        K.dma("sp", bg[0], gla_bg_f[i:i + 1, :], [], ["wg2"], "rows")
        K.dma("sp", bg[1], gla_bg_b[i:i + 1, :], [], ["wg2"], "rows")
        for hh in range(4):
            K.dma("sp", ng4[:, hh * 256:(hh + 1) * 256], gla_norm_g[i:i + 1, :].partition_broadcast(128), [], ["ng4"], "bc")
            K.cp("dve", Mf4[:, hh * 128:(hh + 1) * 128], cm(c32, 5), ["c32"], ["Mf4"])
            K.cp("dve", Mb4[:, hh * 128:(hh + 1) * 128], cm(c32, 6), ["c32"], ["Mb4"])

        def softplus_neg(dirn):
            K.mm(pM, lr[0:16, dirn * 128:(dirn + 1) * 128], wg2[dirn], True, False, ["lr", "wg2"], ["pM"])
            K.mm(pM, ones_r32[0:1, 0:128], bg[dirn], False, True, ["ones_r32", "wg2"], ["pM"])
            K.act(ex, pM, AF.Exp, ["pM"], ["ex"], scale=-1.0)
            K.ts("dve", ex, ex, 1.0, None, ALU.add, None, ["ex"], ["ex"])
            K.act(spl[dirn], ex, AF.Ln, ["ex"], [("spl", dirn)])

        def proj_common(t, p):
            front(cm_, xsrc, t, hb)
            transpose8(hb, hT, pT, "hb", "hT")
            for c in range(2):
                for k in range(8):
                    K.mm(pA[:, c * 512:(c + 1) * 512], hT[:, k, :], win[:, k, 1024 + c * 512:1024 + (c + 1) * 512], k == 0, k == 7, ["hT", "win"], ["pA"])
            K.cp("act", vb[p], pA, ["pA"], [("vb", p)])
            for k in range(8):
                K.mm(pM, hT[:, k, :], win[:, k, 512:1024], k == 0, k == 7, ["hT", "win"], ["pM"])
            K.cp("dve", kt32, pM, ["pM"], ["kt32"])
            for dirn in range(2):
                for k in range(8):
                    K.mm(pM[0:16, dirn * 128:(dirn + 1) * 128], win[:, k, 3072 + dirn * 16:3072 + (dirn + 1) * 16], hT[:, k, :], k == 0, k == 7, ["hT", "win"], ["pM"])
            K.cp("dve", lr, pM[0:16, 0:256], ["pM"], ["lr"])

        def khat(dirn, Lmat, p):
            K.mm(pM, Lmat, spl[dirn], True, True, ["c32", ("spl", dirn)], ["pM"])
            K.act(Ed, pM, AF.Exp, ["pM"], ["Ed"])
            K.tt("dve", kh[p], kt32, Ed, ALU.mult, ["kt32", "Ed"], [("kh", p)])

        def state_update(p):
            for hh in range(4):
                K.mm(pB[:, hh * 256:(hh + 1) * 256], kh[p][:, hh * 128:(hh + 1) * 128], vb[p][:, hh * 256:(hh + 1) * 256], True, True, [("kh", p), ("vb", p)], ["pB"])
            for hh in range(4):
                K.stt("dve", S32[:, hh * 256:(hh + 1) * 256], S32[:, hh * 256:(hh + 1) * 256], dec[p][:, hh:hh + 1], pB[:, hh * 256:(hh + 1) * 256],
                      ALU.mult, ALU.add, ["S32", ("dec", p), "pB"], ["S32"])
            K.cp("act", Sb, S32, ["S32"], ["Sb"])

        def s1b(t, p):
            proj_common(t, p)
            softplus_neg(1)
            for hh in range(4):
                K.mm(pQ[:, hh:hh + 1], spl[1][:, hh * 128:(hh + 1) * 128], cm(c32, 1)[:, 127:128], True, True, [("spl", 1), "c32"], ["pQ"])
            K.act(dec[p], pQ[:, 0:4], AF.Exp, ["pQ"], [("dec", p)])
            khat(1, cm(c32, 4), p)

        def s2b(t, p):
            K.dma("sp", sbn[t], Sb, ["Sb"], [("sbn", t)], "sbn")
            state_update(p)

        K.memset("pool", S32, 0.0, ["S32"])
        K.memset("pool", Sb, 0.0, ["Sb"])
        order = list(range(NT - 1, -1, -1))
        if PIPE_B:
            s1b(order[0], 0)
            for n, t in enumerate(order):
                if n + 1 < NT:
                    s1b(order[n + 1], (n + 1) % 2)
                s2b(t, n % 2)
        else:
            for n, t in enumerate(order):
                s1b(t, n % 2)
                s2b(t, n % 2)

        def s1f_groups(t, p):
            def g1():
                front(cm_, xsrc, t, hb)
                transpose8(hb, hT, pT, "hb", "hT")

            def g2():
                for c in range(2):
                    for k in range(8):
                        K.mm(pA[:, c * 512:(c + 1) * 512], hT[:, k, :], win[:, k, 1024 + c * 512:1024 + (c + 1) * 512], k == 0, k == 7, ["hT", "win"], ["pA"])
                K.cp("act", vb[p], pA, ["pA"], [("vb", p)])

            def g3():
                for k in range(8):
                    K.mm(pM, hT[:, k, :], win[:, k, 512:1024], k == 0, k == 7, ["hT", "win"], ["pM"])
                K.cp("dve", kt32, pM, ["pM"], ["kt32"])
                for dirn in range(2):
                    for k in range(8):
                        K.mm(pM[0:16, dirn * 128:(dirn + 1) * 128], win[:, k, 3072 + dirn * 16:3072 + (dirn + 1) * 16], hT[:, k, :], k == 0, k == 7, ["hT", "win"], ["pM"])
                K.cp("dve", lr, pM[0:16, 0:256], ["pM"], ["lr"])

            def g4():
                for c in range(2):
                    for k in range(8):
                        K.mm(pB[:, c * 512:(c + 1) * 512], hT[:, k, :], win[:, k, 2048 + c * 512:2048 + (c + 1) * 512], k == 0, k == 7, ["hT", "win"], ["pB"])
                K.act(rs[p], pB, AF.Silu, ["pB"], [("rs", p)])
                K.tt("pool", rs[p], rs[p], ng4, ALU.mult, [("rs", p), "ng4"], [("rs", p)])

            def g5():
                for hh in range(4):
                    for k in range(8):
                        K.mm(pQ[:, hh * 128:(hh + 1) * 128], win[:, k, hh * 128:(hh + 1) * 128], hT[:, k, :], k == 0, k == 7, ["hT", "win"], ["pQ"])
                for hh in range(4):
                    for k in range(8):
                        K.mm(pK[:, hh * 128:(hh + 1) * 128], win[:, k, 512 + hh * 128:512 + (hh + 1) * 128], hT[:, k, :], k == 0, k == 7, ["hT", "win"], ["pK"])
                softplus_neg(0)
                softplus_neg(1)

            def g6():
                for dirn in range(2):
                    U = cm(c32, 1 + dirn)
                    for hh in range(4):
                        K.mm(pM[:, hh * 128:(hh + 1) * 128], spl[dirn][:, hh * 128:(hh + 1) * 128], U, True, True, [("spl", dirn), "c32"], ["pM"])
                    K.act(Eq[dirn], pM, AF.Exp, ["pM"], [("Eq", dirn)])
                    K.act(Ek[dirn], pM, AF.Exp, ["pM"], [("Ek", dirn)], scale=-1.0)
                    K.stt("dve", qT[p][dirn], pQ, 128.0 ** -0.5, Eq[dirn], ALU.mult, ALU.mult, ["pQ", ("Eq", dirn)], [("qT", p, dirn)])
                    K.tt("dve", kT[dirn], pK, Ek[dirn], ALU.mult, ["pK", ("Ek", dirn)], [("kT", dirn)])

            def g7():
                for hh in range(4):
                    K.mm(pM[:, hh * 128:(hh + 1) * 128], kT[0][:, hh * 128:(hh + 1) * 128], qT[p][0][:, hh * 128:(hh + 1) * 128], True, True, [("kT", 0), ("qT", p, 0)], ["pM"])
                K.tt("dve", sm32, pM, Mf4, ALU.mult, ["pM", "Mf4"], ["sm32"])
                for hh in range(4):
                    K.mm(pM[:, hh * 128:(hh + 1) * 128], kT[1][:, hh * 128:(hh + 1) * 128], qT[p][1][:, hh * 128:(hh + 1) * 128], True, True, [("kT", 1), ("qT", p, 1)], ["pM"])
                K.tt("dve", ex, pM, Mb4, ALU.mult, ["pM", "Mb4"], ["ex"])
                K.tt("dve", smb[p], sm32, ex, ALU.add, ["sm32", "ex"], [("smb", p)])
                for hh in range(4):
                    K.cp("dve", dec[p][:, hh:hh + 1], Eq[0][:, hh * 128 + 127:hh * 128 + 128], [("Eq", 0)], [("dec", p)])
                khat(0, cm(c32, 3), p)
            return [g1, g2, g3, g4, g5, g6, g7]

        def s2f_groups(t, p):
            def h1():
                K.dma("sp", Sbn, sbn[t], [("sbn", t)], ["Sbn"], "sbnl")
                for hh in range(4):
                    o_ = pA[:, hh * 256:(hh + 1) * 256]
                    K.mm(o_, smb[p][:, hh * 128:(hh + 1) * 128], vb[p][:, hh * 256:(hh + 1) * 256], True, False, [("smb", p), ("vb", p)], ["pA"])
                    K.mm(o_, qT[p][0][:, hh * 128:(hh + 1) * 128], Sb[:, hh * 256:(hh + 1) * 256], False, False, [("qT", p, 0), "Sb"], ["pA"])
                    K.mm(o_, qT[p][1][:, hh * 128:(hh + 1) * 128], Sbn[:, hh * 256:(hh + 1) * 256], False, True, [("qT", p, 1), "Sbn"], ["pA"])
                for hh in range(4):
                    K.act(junk, pA[:, hh * 256:(hh + 1) * 256], AF.Square, ["pA"], ["junk", "ss"], accum=ss[:, hh:hh + 1])
                K.ts("dve", ss[:, 4:8], ss[:, 0:4], 1.0 / 256.0, EPS, ALU.mult, ALU.add, ["ss"], ["ss"])
                K.tt("pool", ss[:, 0:4], ss[:, 4:8], nhalf, ALU.pow, ["ss", "nhalf"], ["ss"])
                for hh in range(4):
                    K.stt("dve", gb[:, hh * 256:(hh + 1) * 256], pA[:, hh * 256:(hh + 1) * 256], ss[:, hh:hh + 1], rs[p][:, hh * 256:(hh + 1) * 256],
                          ALU.mult, ALU.mult, ["pA", "ss", ("rs", p)], ["gb"])

            def h2():
                state_update(p)

            def h3():
                transpose8(gb, gT, pT, "gb", "gT")

            def h4():
                for c in range(2):
                    for k in range(8):
                        K.mm(pB[:, c * 512:(c + 1) * 512], gT[:, k, :], wout[:, k, c * 512:(c + 1) * 512], k == 0, k == 7, ["gT", "wout"], ["pB"])
                back(cm_, xdst, t, pB, "pB")
            return [h1, h2, h3, h4]

        def s1f(t, p):
            for g in s1f_groups(t, p):
                g()

        def s2f(t, p):
            for g in s2f_groups(t, p):
                g()

        K.memset("pool", S32, 0.0, ["S32"])
        K.memset("pool", Sb, 0.0, ["Sb"])
        if PIPE_F:
            s1f(0, 0)
            for t in range(NT):
                G = s1f_groups(t + 1, (t + 1) % 2) if t + 1 < NT else []
                H = s2f_groups(t, t % 2)
                order = [("g", 0), ("g", 1), ("h", 0), ("g", 2), ("g", 3), ("h", 1), ("g", 4), ("h", 2), ("g", 5), ("h", 3), ("g", 6)]
                for kind, i_ in order:
                    if kind == "g":
                        if i_ < len(G):
                            G[i_]()
                    else:
                        H[i_]()
        else:
            for t in range(NT):
                s1f(t, t % 2)
                s2f(t, t % 2)

    def phase_moe(l, xsrc, xdst):
        K.phase()
        cm_ = {}
        wr = K.sb([128, 8, NE], F32)
        br = K.sb([1, NE], F32)
        desti = K.sb([128, NT * 4], I32)
        dumpf = K.sb([128, 4], F32)
        ecb = K.sb([128, NE], F32)
        cnt = K.sb([128, NE], F32)
        K.dma("sp", wr, moe_w_router[l].rearrange("(k p) n -> p k n", p=128), [], ["wr"], "rows")
        K.dma("sp", br, moe_b_router[l:l + 1, :], [], ["wr"], "rows")
        K.dma("sp", dumpf, dumpidx_in, [], ["dumpf"], "rows")
        K.ts("dve", dumpf, dumpf, 1.0, None, ALU.add, None, ["dumpf"], ["dumpf"])
        K.dma("sp", ecb, ecrow_in.partition_broadcast(128), [], ["ecb"], "bc")
        K.memset("pool", cnt, 0.0, ["cnt"])
        mark = K.off
        alloc_bc(1, l, ("scale", "shift"), cm_)
        alloc_work(cm_)
        RT = []
        for p in range(2):
            r_ = {"p": p}
            r_["h32"] = K.sb([128, D], F32)
            r_["hb"] = K.sb([128, D], BF16)
            r_["hT32"] = K.sb([128, 8, 128], F32)
            for nm in ("lg", "rank", "over", "dfull", "tmp", "tmp2"):
                r_[nm] = K.sb([128, NE], F32)
            r_["oh"] = [K.sb([128, NE], F32) for _ in range(4)]
            r_["maskb"] = K.sb([128, NE], BF16)
            for nm in ("top8", "sm", "destf"):
                r_[nm] = K.sb([128, 8], F32)
            r_["w4"] = K.sb([128, 4], F32)
            r_["w4x"] = K.sb([128, 4, 8], F32)
            r_["pT32"] = K.ps(1).rearrange("p (a b) -> p a b", a=4)
            r_["pL"] = K.ps(1)
            r_["pR"] = K.ps(1)
            K.memset("pool", r_["w4x"], 0.0, [("w4x", p)])
            RT.append(r_)

        def route_tile(r_, t):
            p = r_["p"]
            T_ = lambda nm: (nm, p)
            lg, top8, sm, w4, w4x, oh, maskb = r_["lg"], r_["top8"], r_["sm"], r_["w4"], r_["w4x"], r_["oh"], r_["maskb"]
            rank, over, dfull, tmp, tmp2, destf, pL, pR = r_["rank"], r_["over"], r_["dfull"], r_["tmp"], r_["tmp2"], r_["destf"], r_["pL"], r_["pR"]
            hT32 = r_["hT32"]
            for half in range(2):
                for j in range(4):
                    k = half * 4 + j
                    K.tr(r_["pT32"][:, j, :], r_["h32"][:, k * 128:(k + 1) * 128], id32, [T_("h32"), "c32"], [T_("pT32")])
                K.cp("act", hT32[:, half * 4:(half + 1) * 4, :], r_["pT32"], [T_("pT32")], [T_("hT32")])
            for k in range(8):
                K.mm(pL[:, 0:NE], hT32[:, k, :], wr[:, k, :], k == 0, False, [T_("hT32"), "wr"], [T_("pL")])
            K.mm(pL[:, 0:NE], ones_r32[0:1, 0:128], br, False, True, ["ones_r32", "wr"], [T_("pL")])
            K.cp("dve", lg, pL[:, 0:NE], [T_("pL")], [T_("lg")])
            S.op("dve", lambda e: e.max(out=top8, in_=lg), [T_("lg")], [T_("top8")])
            K.ts("dve", sm[:, 0:1], top8[:, 0:1], -1.0, None, ALU.mult, None, [T_("top8")], [T_("sm")])
            K.act(w4, top8[:, 0:4], AF.Exp, [T_("top8"), T_("sm")], [T_("w4"), T_("sm")], bias=sm[:, 0:1], accum=sm[:, 1:2])
            S.op("dve", lambda e: e.reciprocal(out=sm[:, 2:3], in_=sm[:, 1:2]), [T_("sm")], [T_("sm")])
            K.ts("dve", w4x[:, :, 0:1], w4.rearrange("p (a b) -> p a b", b=1), sm[:, 2:3], None, ALU.mult, None, [T_("w4"), T_("sm")], [T_("w4x")])
            for k in range(4):
                K.ts("dve", oh[k], lg, top8[:, k:k + 1], None, ALU.is_equal, None, [T_("lg"), T_("top8")], [T_("oh%d" % k)])
            K.ts("dve", maskb, lg, top8[:, 3:4], None, ALU.is_ge, None, [T_("lg"), T_("top8")], [T_("maskb")])
            K.mm(pR[:, 0:NE], cm(cb, 8), maskb, True, True, ["cb", T_("maskb")], [T_("pR")])
            K.mm(pR[:, NE:2 * NE], cm(cb, 7), maskb, True, True, ["cb", T_("maskb")], [T_("pR")])
            K.tt("dve", rank, pR[:, 0:NE], cnt, ALU.add, [T_("pR"), "cnt"], [T_("rank")])
            K.tt("dve", cnt, pR[:, NE:2 * NE], cnt, ALU.add, [T_("pR"), "cnt"], ["cnt"])
            K.ts("dve", over, rank, float(C), -1.0, ALU.is_ge, ALU.add, [T_("rank")], [T_("over")])
            K.stt("dve", dfull, rank, 1.0, ecb, ALU.add, ALU.add, [T_("rank"), "ecb"], [T_("dfull")])
            K.stt("dve", dfull, dfull, -1.0, over, ALU.mult, ALU.mult, [T_("dfull"), T_("over")], [T_("dfull")])
            for k in range(4):
                K.tt("dve", tmp, oh[k], dfull, ALU.mult, [T_("oh%d" % k), T_("dfull")], [T_("tmp")])
                S.op("dve", lambda e, k=k: e.reduce_sum(out=destf[:, k:k + 1], in_=tmp, axis=AX.X), [T_("tmp")], [T_("destf")])
            K.ts("dve", destf[:, 4:8], destf[:, 0:4], 0.0, None, ALU.is_equal, None, [T_("destf")], [T_("destf")])
            K.tt("dve", destf[:, 4:8], destf[:, 4:8], dumpf, ALU.mult, [T_("destf"), "dumpf"], [T_("destf")])
            K.stt("dve", destf[:, 0:4], destf[:, 0:4], -1.0, destf[:, 4:8], ALU.add, ALU.add, [T_("destf")], [T_("destf")])
            K.cp("dve", desti[:, t * 4:(t + 1) * 4], destf[:, 0:4], [T_("destf")], [("desti", t)])
            for k in range(4):
                idx = desti[:, t * 4 + k:t * 4 + k + 1]
                S.op("pool", lambda e, idx=idx: e.indirect_dma_start(out=Xs, out_offset=bass.IndirectOffsetOnAxis(ap=idx, axis=0), in_=r_["hb"], in_offset=None),
                     [T_("hb"), ("desti", t)], [], dma_tag="scatx%d_%d" % (k, p))
                S.op("pool", lambda e, idx=idx, k=k: e.indirect_dma_start(out=Ws, out_offset=bass.IndirectOffsetOnAxis(ap=idx, axis=0), in_=w4x[:, k, :], in_offset=None),
                     [T_("w4x"), ("desti", t)], [], dma_tag="scatw%d_%d" % (k, p))

        def r_tile(t):
            r_ = RT[t % 2]
            p = t % 2
            b = t % 3
            K.dma("sp", cm_["xt"][b], xsrc[t * 128:(t + 1) * 128, :], ["xsrc"], [("xt", b)], "xt%d" % b)
            K.tt("dve", r_["h32"], cm_["xt"][b], cm_["scale"], ALU.mult, [("xt", b), "bc_scale"], [("h32", p)])
            K.tt("pool", r_["hb"], r_["h32"], cm_["shift"], ALU.add, [("h32", p), "bc_shift"], [("hb", p)])
            K.tt("dve", r_["h32"], r_["h32"], cm_["shift"], ALU.add, [("h32", p), "bc_shift"], [("h32", p)])
            route_tile(r_, t)

        for t0 in range(0, NT, 2):
            lists = []
            for t in range(t0, min(NT, t0 + 2)):
                S.capture()
                r_tile(t)
                lists.append(S.end_capture())
            S.replay(lists, 14)
        dbg("cnt%d" % l, cnt, "cnt")
        S.fence()
        K.off = mark
        K.pso = 0
        GS = 512 if C % 512 == 0 else 384
        NG = C // GS
        NB = GS // 128
        assert NG * GS == C
        bup = K.sb([128, NE, 16], F32)
        K.dma("sp", bup, moe_b_upT[l], [], ["bup"], "rows")
        K.ts("dve", bup[:, :, 8:16], bup[:, :, 8:16], 1.0, None, ALU.add, None, ["bup"], ["bup"])
        bup17 = K.sb([128, NE, 8], F32)
        K.ts("dve", bup17, bup[:, :, 0:8], 1.702, None, ALU.mult, None, ["bup"], ["bup17"])
        wup = [K.sb([128, 8, 2 * D], BF16) for _ in range(2)]
        wdn = [K.sb([128, 8, D], BF16) for _ in range(2)]
        NST = 3
        stage = [K.sb([128, 2 * D], F32) for _ in range(NST)]
        bd32 = [K.sb([1, D], F32) for _ in range(2)]
        bdb = [K.sb([1, D], BF16) for _ in range(2)]
        xs = [K.sb([128, D], BF16) for _ in range(2)]
        wsl = [K.sb([128, 8], F32) for _ in range(16)]
        xsT = [K.sb([128, 8, GS], BF16) for _ in range(2)]
        actT = [K.sb([128, 8, GS], BF16) for _ in range(2)]
        gt = [K.sb([128, GS], F32) for _ in range(2)]
        sg = [K.sb([128, GS], F32) for _ in range(2)]
        lA = [K.sb([128, GS], F32) for _ in range(2)]
        ys = [K.sb([128, 512], F32) for _ in range(3)]
        pTs = [K.ps(1).bitcast(BF16)[:, 0:512].rearrange("p (a b) -> p a b", a=4) for _ in range(2)]
        pG = [K.ps(1) for _ in range(2)]
        pLn = [K.ps(1) for _ in range(2)]
        pY = K.ps(2)
        cnts = {"ns": 0, "nx": 0, "nw": 0, "ny": 0, "nj": 0}

        def weight_steps(e_):
            be = e_ % 2
            steps = []
            for k in range(8):
                def f(k=k):
                    b = cnts["ns"] % NST
                    cnts["ns"] += 1
                    K.dma("sp", stage[b], moe_w_up[l, e_, k * 128:(k + 1) * 128, :], [], [("stage", b)], "stage%d" % b)
                    st_v = stage[b].rearrange("p (f two) -> p two f", two=2)
                    K.cp("act", wup[be][:, k, :].rearrange("p (two f) -> p two f", two=2), st_v, [("stage", b)], [("wup", be)])
                steps.append(f)
            for k2 in range(4):
                def f(k2=k2):
                    b = cnts["ns"] % NST
                    cnts["ns"] += 1
                    K.dma("sp", stage[b].rearrange("p (a n) -> p a n", a=2),
                          moe_w_down[l, e_, k2 * 256:(k2 + 1) * 256, :].rearrange("(a p) n -> p a n", p=128), [], [("stage", b)], "stage%d" % b)
                    K.cp("dve", wdn[be][:, 2 * k2:2 * k2 + 2, :], stage[b].rearrange("p (a n) -> p a n", a=2), [("stage", b)], [("wdn", be)])
                steps.append(f)

            def f():
                K.dma("sp", bd32[be], moe_b_down[l, e_:e_ + 1, :], [], [("bd32", be)], "bd32_%d" % be)
                K.cp("dve", bdb[be], bd32[be], [("bd32", be)], [("bdb", be)])
            steps.append(f)
            return steps

        units = [(e_, g) for e_ in range(NE) for g in range(NG)]
        wslots = {}

        def unit_A(u):
            e_, g = units[u]
            gb_ = u % 2
            base = e_ * C + g * GS
            wl = []
            for blk in range(NB):
                b = cnts["nx"] % 2
                cnts["nx"] += 1
                wi = cnts["nw"] % 16
                cnts["nw"] += 1
                wl.append(wi)
                r0 = base + blk * 128
                K.dma("sp", xs[b], Xs[r0:r0 + 128, :], ["Xs"], [("xs", b)], "xs%d" % b)
                K.dma("sp", wsl[wi], Ws[r0:r0 + 128, :], ["Ws"], [("wsl", wi)], "wsl%d" % wi)
                for half in range(2):
                    pT = pTs[half]
                    for j in range(4):
                        k = half * 4 + j
                        K.tr(pT[:, j, :], xs[b][:, k * 128:(k + 1) * 128], idb, [("xs", b), "cb"], [("pT", half)])
                    K.cp("act" if half == 0 else "dve", xsT[gb_][:, half * 4:(half + 1) * 4, blk * 128:(blk + 1) * 128], pT, [("pT", half)], [("xsT", gb_)])
            wslots[u] = wl

        def unit_U(u, pref):
            e_, g = units[u]
            be = e_ % 2
            gb_ = u % 2
            for j in range(8):
                jb = cnts["nj"] % 2
                cnts["nj"] += 1
                for k in range(8):
                    K.mm(pG[jb][:, 0:GS], wup[be][:, k, j * 128:(j + 1) * 128], xsT[gb_][:, k, :], k == 0, k == 7, [("wup", be), ("xsT", gb_)], [("pG", jb)])
                for k in range(8):
                    K.mm(pLn[jb][:, 0:GS], wup[be][:, k, D + j * 128:D + (j + 1) * 128], xsT[gb_][:, k, :], k == 0, k == 7, [("wup", be), ("xsT", gb_)], [("pLn", jb)])
                K.act(gt[jb], pG[jb][:, 0:GS], AF.Identity, [("pG", jb), "bup"], [("gt", jb)], bias=bup[:, e_, j:j + 1], scale=1.0)
                K.act(sg[jb], pG[jb][:, 0:GS], AF.Sigmoid, [("pG", jb), "bup17"], [("sg", jb)], bias=bup17[:, e_, j:j + 1], scale=1.702)
                K.act(lA[jb], pLn[jb][:, 0:GS], AF.Identity, [("pLn", jb), "bup"], [("lA", jb)], bias=bup[:, e_, 8 + j:9 + j], scale=1.0)
                K.ts("dve", lA[jb], lA[jb], -6.0, 8.0, ALU.max, ALU.min, [("lA", jb)], [("lA", jb)])
                K.stt("dve", gt[jb], gt[jb], 7.0, lA[jb], ALU.min, ALU.mult, [("gt", jb), ("lA", jb)], [("gt", jb)])
                K.stt("dve", actT[gb_][:, j, :], sg[jb], SIG7, gt[jb], ALU.min, ALU.mult, [("sg", jb), ("gt", jb)], [("actT", gb_)])
                if pref:
                    pref.pop(0)()
                    if len(pref) > 8 - j and j % 2 == 1:
                        pref.pop(0)()

        def unit_D(u):
            e_, g = units[u]
            be = e_ % 2
            gb_ = u % 2
            base = e_ * C + g * GS
            for blk in range(NB):
                wi = wslots[u][blk]
                r0 = base + blk * 128
                for c in range(2):
                    for k in range(8):
                        K.mm(pY[:, c * 512:(c + 1) * 512], actT[gb_][:, k, blk * 128:(blk + 1) * 128], wdn[be][:, k, c * 512:(c + 1) * 512], k == 0, False, [("actT", gb_), ("wdn", be)], [("pY", c)])
                    K.mm(pY[:, c * 512:(c + 1) * 512], ones_rb[0:1, 0:128], bdb[be][0:1, c * 512:(c + 1) * 512], False, True, ["ones_rb", ("bdb", be)], [("pY", c)])
                    b = cnts["ny"] % 3
                    cnts["ny"] += 1
                    K.act(ys[b], pY[:, c * 512:(c + 1) * 512], AF.Copy, [("pY", c), ("wsl", wi)], [("ys", b)], scale=wsl[wi][:, 0:1])
                    K.dma("act", Ys[r0:r0 + 128, c * 512:(c + 1) * 512], ys[b], [("ys", b)], [], "ys%d" % b)

        for f in weight_steps(0):
            f()
        unit_A(0)
        pref = []
        for u in range(len(units)):
            e_, g = units[u]
            if g == 0:
                for f in pref:
                    f()
                pref = weight_steps(e_ + 1) if e_ + 1 < NE else []
            if u + 1 < len(units):
                unit_A(u + 1)
            unit_U(u, pref)
            unit_D(u)
        for f in pref:
            f()
        S.fence()
        K.off = mark
        K.pso = 0
        alloc_bc(1, l, ("gate", "lng", "lnb"), cm_)
        alloc_work(cm_)
        yk = [[K.sb([128, D], F32) for _ in range(4)] for _ in range(2)]
        def g_tile(t):
            b = t % 2
            y_ = yk[b]
            bx = t % 3
            K.dma("sp", cm_["xt"][bx], xsrc[t * 128:(t + 1) * 128, :], ["xsrc"], [("xt", bx)], "xt%d" % bx)
            for k in range(4):
                idx = desti[:, t * 4 + k:t * 4 + k + 1]
                S.op("pool", lambda e, idx=idx, k=k, y_=y_: e.indirect_dma_start(out=y_[k], out_offset=None, in_=Ys, in_offset=bass.IndirectOffsetOnAxis(ap=idx, axis=0)),
                     ["Ys", ("desti", t)], [("yk", b, k)], dma_tag="gath%d_%d" % (k, b))
            K.tt("dve", y_[0], y_[0], y_[1], ALU.add, [("yk", b, 0), ("yk", b, 1)], [("yk", b, 0)])
            K.tt("pool", y_[2], y_[2], y_[3], ALU.add, [("yk", b, 2), ("yk", b, 3)], [("yk", b, 2)])
            K.tt("dve", y_[0], y_[0], y_[2], ALU.add, [("yk", b, 0), ("yk", b, 2)], [("yk", b, 0)])
            back(cm_, xdst, t, y_[0], ("yk", b, 0))

        for t0 in range(0, NT, 2):
            lists = []
            for t in range(t0, min(NT, t0 + 2)):
                S.capture()
                g_tile(t)
                lists.append(S.end_capture())
            S.replay(lists, 5)

    def phase_init():
        K.phase()
        zt = K.sb([128, 4096], F32)
        K.memset("pool", zt, 0.0, ["zt"])
        ztb = zt.bitcast(BF16)
        nrows = NSLOT + NDUMP
        assert nrows % 512 == 0 and (nrows // 128) * 8 <= 4096
        for r in range(0, nrows, 512):
            K.dma("sp", Xs[r:r + 512, :].rearrange("(p a) d -> p (a d)", p=128), ztb[:, 0:4 * D], ["zt"], [], "zinit%d" % ((r // 512) % 4))
        K.dma("sp", Ws.rearrange("(p a) d -> p (a d)", p=128), zt[:, 0:(nrows // 128) * 8], ["zt"], ["Ws"], "zinit")
        K.dma("sp", Ys[NSLOT:NSLOT + NDUMP, :].rearrange("(p a) d -> p (a d)", p=128), zt[:, 0:4 * D], ["zt"], ["Ys"], "zinit")

    phase_mod()
    if do_moe:
        phase_init()
    cur = x_in
    bufs = [xa, xb_]
    nb = 0
    stages = []
    for l in layer_list:
        if do_mixer:
            stages.append(("mix", l))
        if do_moe:
            stages.append(("moe", l))
    for si, (kind, l) in enumerate(stages):
        dst = out if si == len(stages) - 1 else bufs[nb % 2]
        nb += 1
        if kind == "mix":
            if l % 2 == 0:
                phase_gla(l, cur, dst)
            else:
                phase_sgu(l, cur, dst)
        else:
            phase_moe(l, cur, dst)
        cur = dst
    cnt = S.emit()
    return nc, cnt


def prep_shared(inputs, C):
    f = lambda a: np.ascontiguousarray(np.asarray(a, dtype=np.float32))
    sh = {}
    for k_ in ("ada_w", "ada_b", "ln_g", "ln_b", "gla_w_in", "gla_wg2_f", "gla_bg_f", "gla_wg2_b", "gla_bg_b", "gla_norm_g", "gla_w_out",
               "sgu_w_in", "sgu_b_in", "sgu_ln_g", "sgu_ln_b", "sgu_w_out", "sgu_b_out", "moe_w_router", "moe_b_router", "moe_w_up",
               "moe_w_down", "moe_b_down"):
        sh[k_] = f(inputs[k_])
    sh["sgu_w_sT"] = f(np.transpose(np.asarray(inputs["sgu_w_s"]), (0, 3, 1, 2)))
    sh["sgu_b_sT"] = f(np.transpose(np.asarray(inputs["sgu_b_s"]), (0, 2, 1)))
    bu = np.asarray(inputs["moe_b_up"], dtype=np.float32)
    bg = bu[:, :, 0::2].reshape(4, NE, 8, 128)
    bl = bu[:, :, 1::2].reshape(4, NE, 8, 128)
    sh["moe_b_upT"] = f(np.transpose(np.concatenate([bg, bl], axis=2), (0, 3, 1, 2)))
    sh["consts"] = make_consts()
    sh["dumpidx"] = (NE * C + np.arange(4)[None, :] * 128 + np.arange(128)[:, None]).astype(np.float32)
    sh["ecrow"] = (np.arange(NE, dtype=np.float32) * C)[None, :]
    return sh


CAP = 1024
_cache = {}


def kernel(**inputs):
    x = np.asarray(inputs["x"], dtype=np.float32)
    c = np.asarray(inputs["c"], dtype=np.float32)
    B, T, _ = x.shape
    key = (T, CAP)
    if key not in _cache:
        _cache[key] = build_program(T, CAP)[0]
    nc = _cache[key]
    sh = prep_shared(inputs, CAP)
    in_maps = []
    for b in range(B):
        m = dict(sh)
        m["x"] = np.ascontiguousarray(x[b])
        m["cT"] = np.ascontiguousarray(c[b].reshape(8, 128).T)
        in_maps.append(m)
    res = run_bass_kernel_spmd(nc, in_maps, core_ids=list(range(B)))
    return np.stack([np.asarray(r["out"], dtype=np.float32) for r in res.results], axis=0)
```
